# Optimizing a Trainium2 kernel written in Bass

```python
import math
import jax, jax.numpy as jnp
from jax import lax
import numpy as np

D_MODEL = 1024
BATCH = 8
SEQ = 4096
DEPTH = 1

HEAD_DIM = 64
DIL_GROUPS = ((128, 1), (512, 4), (2048, 16))
DIL_HEADS = 4
DIL_WIDTH = len(DIL_GROUPS) * DIL_HEADS * HEAD_DIM
DIL_OUT = DIL_HEADS * HEAD_DIM
SWA_Q_HEADS = 16
SWA_KV_HEADS = 2
SWA_REP = SWA_Q_HEADS // SWA_KV_HEADS
SWA_WINDOW = 128
SWA_Q_WIDTH = SWA_Q_HEADS * HEAD_DIM
SWA_KV_WIDTH = SWA_KV_HEADS * HEAD_DIM
N_ALIBI_HEADS = SWA_Q_HEADS + len(DIL_GROUPS) * DIL_HEADS
ATTN_BLOCK = 128
SPLIT_SIZES = (DIL_WIDTH, DIL_WIDTH, DIL_WIDTH, SWA_Q_WIDTH, SWA_KV_WIDTH, SWA_KV_WIDTH, D_MODEL, D_MODEL)
SPLIT_POINTS = tuple(sum(SPLIT_SIZES[:i + 1]) for i in range(len(SPLIT_SIZES) - 1))
IN_COLS = sum(SPLIT_SIZES)
N_EXPERTS = 32
TOP_K = 4
D_FF = 1024
SWIGLU_LIMIT = 7.0
SWIGLU_ALPHA = 1.702
MOE_BLOCK = 128
LN_EPS = 1e-5
DEEPNORM_ALPHA = (2 * DEPTH) ** 0.25
DEEPNORM_BETA = (8 * DEPTH) ** -0.25
NEG_INF = -1e30

kernel_name = "hybrid_dilated_swa_sink_moe_deepnorm"


def layer_norm(x, g, b):
    xf = x.astype(jnp.float32)
    mu = jnp.mean(xf, axis=-1, keepdims=True)
    var = jnp.mean(jnp.square(xf - mu), axis=-1, keepdims=True)
    y = (xf - mu) * lax.rsqrt(var + LN_EPS) * g.astype(jnp.float32) + b.astype(jnp.float32)
    return y.astype(x.dtype)


def alibi_slopes():
    h = jnp.arange(1, N_ALIBI_HEADS + 1, dtype=jnp.float32)
    return jnp.exp2(-8.0 * h / N_ALIBI_HEADS)


def banded_attention(q, k, v, slopes, max_diff, dist_scale, sinks):
    n, g, r, L, hd = q.shape
    nb = -(-L // ATTN_BLOCK)
    pad = nb * ATTN_BLOCK - L
    q = jnp.pad(q, ((0, 0), (0, 0), (0, 0), (0, pad), (0, 0)))
    k = jnp.pad(k, ((0, 0), (0, 0), (ATTN_BLOCK, pad), (0, 0)))
    v = jnp.pad(v, ((0, 0), (0, 0), (ATTN_BLOCK, pad), (0, 0)))
    qb = q.reshape(n, g, r, nb, ATTN_BLOCK, hd)
    kb = k.reshape(n, g, nb + 1, ATTN_BLOCK, hd)
    vb = v.reshape(n, g, nb + 1, ATTN_BLOCK, hd)
    kw = jnp.concatenate([kb[:, :, :-1], kb[:, :, 1:]], axis=3)
    vw = jnp.concatenate([vb[:, :, :-1], vb[:, :, 1:]], axis=3)
    s = jnp.einsum('ngrbqd,ngbkd->ngrbqk', qb, kw, preferred_element_type=jnp.float32) * (hd ** -0.5)
    qi = jnp.arange(ATTN_BLOCK)[:, None]
    kj = jnp.arange(2 * ATTN_BLOCK)[None, :]
    diff = qi - kj + ATTN_BLOCK
    kpos = jnp.arange(nb)[:, None, None] * ATTN_BLOCK - ATTN_BLOCK + kj[None]
    valid = (diff >= 0) & (diff <= max_diff) & (kpos >= 0)
    bias = -slopes.astype(jnp.float32)[:, :, None, None, None] * (dist_scale * diff).astype(jnp.float32)
    s = jnp.where(valid, s + bias, NEG_INF)
    m = jnp.max(s, axis=-1)
    if sinks is not None:
        sink = sinks.astype(jnp.float32)[:, :, None, None]
        m = jnp.maximum(m, sink)
    p = jnp.exp(s - m[..., None])
    denom = jnp.sum(p, axis=-1)
    if sinks is not None:
        denom = denom + jnp.exp(sink - m)
    o = jnp.einsum('ngrbqk,ngbkd->ngrbqd', p.astype(v.dtype), vw, preferred_element_type=jnp.float32)
    o = o / denom[..., None]
    lse = m + jnp.log(denom)
    o = o.reshape(n, g, r, nb * ATTN_BLOCK, hd)[:, :, :, :L]
    lse = lse.reshape(n, g, r, nb * ATTN_BLOCK)[:, :, :, :L]
    return o.astype(q.dtype), lse


def dilated_attention(q, k, v, slopes_a):
    b, S, _ = q.shape
    q = q.reshape(b, S, len(DIL_GROUPS), DIL_HEADS, HEAD_DIM)
    k = k.reshape(b, S, len(DIL_GROUPS), DIL_HEADS, HEAD_DIM)
    v = v.reshape(b, S, len(DIL_GROUPS), DIL_HEADS, HEAD_DIM)
    outs, lses = [], []
    for gi, (window, dil) in enumerate(DIL_GROUPS):
        L = S // dil

        def to_sub(t):
            t = t[:, :, gi].reshape(b, L, dil, DIL_HEADS, HEAD_DIM)
            return t.transpose(0, 2, 3, 1, 4).reshape(b * dil, DIL_HEADS, L, HEAD_DIM)

        qs, ks, vs = to_sub(q), to_sub(k), to_sub(v)
        o, lse = banded_attention(qs[:, :, None], ks, vs, slopes_a[gi][:, None],
                                  max_diff=window // dil, dist_scale=dil, sinks=None)
        o = o[:, :, 0].reshape(b, dil, DIL_HEADS, L, HEAD_DIM).transpose(0, 3, 1, 2, 4).reshape(b, S, DIL_HEADS, HEAD_DIM)
        lse = lse[:, :, 0].reshape(b, dil, DIL_HEADS, L).transpose(0, 3, 1, 2).reshape(b, S, DIL_HEADS)
        outs.append(o)
        lses.append(lse)
    w = jax.nn.softmax(jnp.stack(lses, axis=0), axis=0)
    o = jnp.einsum('gbsh,gbshd->bshd', w, jnp.stack(outs, axis=0).astype(jnp.float32))
    return o.reshape(b, S, DIL_OUT).astype(q.dtype)


def swa_sink_attention(q, k, v, slopes_b, sinks):
    b, S, _ = q.shape
    q = q.reshape(b, S, SWA_KV_HEADS, SWA_REP, HEAD_DIM).transpose(0, 2, 3, 1, 4)
    k = k.reshape(b, S, SWA_KV_HEADS, HEAD_DIM).transpose(0, 2, 1, 3)
    v = v.reshape(b, S, SWA_KV_HEADS, HEAD_DIM).transpose(0, 2, 1, 3)
    o, _ = banded_attention(q, k, v, slopes_b, max_diff=SWA_WINDOW - 1, dist_scale=1, sinks=sinks)
    return o.transpose(0, 3, 1, 2, 4).reshape(b, S, SWA_Q_WIDTH)


def token_mixer(x, w_in, sinks, w_proj_a, w_proj_b, w_out, slopes):
    h = jnp.einsum('bsd,dc->bsc', x, w_in)
    a_q, a_k, a_v, b_q, b_k, b_v, g_a, g_b = jnp.split(h, SPLIT_POINTS, axis=-1)
    slopes_b = slopes[:SWA_Q_HEADS].reshape(SWA_KV_HEADS, SWA_REP)
    slopes_a = slopes[SWA_Q_HEADS:].reshape(len(DIL_GROUPS), DIL_HEADS)
    out_a = dilated_attention(a_q, a_k, a_v, slopes_a)
    out_b = swa_sink_attention(b_q, b_k, b_v, slopes_b, sinks.reshape(SWA_KV_HEADS, SWA_REP))
    merged = (jax.nn.sigmoid(g_a) * jnp.einsum('bsc,cd->bsd', out_a, w_proj_a)
              + jax.nn.sigmoid(g_b) * jnp.einsum('bsc,cd->bsd', out_b, w_proj_b))
    return jnp.einsum('bsd,de->bse', merged, w_out)


def moe_ffn(y, router_w, router_b, w_gate, b_gate, w_up, b_up, w_down, b_down):
    b, S, D = y.shape
    n_tok = b * S
    yf = y.reshape(n_tok, D)
    logits = (yf @ router_w + router_b).astype(jnp.float32)
    top_val, top_idx = lax.top_k(logits, TOP_K)
    gates = jax.nn.softmax(top_val, axis=-1)
    n_assign = n_tok * TOP_K
    flat_e = top_idx.reshape(-1)
    order = jnp.argsort(flat_e)
    sorted_e = flat_e[order]
    tok = order // TOP_K
    counts = jnp.zeros((N_EXPERTS,), jnp.int32).at[flat_e].add(1)
    padded = (counts + MOE_BLOCK - 1) // MOE_BLOCK * MOE_BLOCK
    p_end = jnp.cumsum(padded)
    p_start = p_end - padded
    u_start = jnp.cumsum(counts) - counts
    dest = p_start[sorted_e] + jnp.arange(n_assign, dtype=jnp.int32) - u_start[sorted_e]
    n_blocks = -(-n_assign // MOE_BLOCK) + N_EXPERTS
    rows = n_blocks * MOE_BLOCK
    xs = jnp.zeros((rows, D), y.dtype).at[dest].set(yf[tok])
    block_e = jnp.minimum(jnp.searchsorted(p_end, jnp.arange(n_blocks, dtype=jnp.int32) * MOE_BLOCK, side='right'),
                          N_EXPERTS - 1).astype(jnp.int32)

    def expert_block(args):
        xb, e = args
        gt = xb @ w_gate[e] + b_gate[e]
        up = xb @ w_up[e] + b_up[e]
        gt = jnp.minimum(gt, SWIGLU_LIMIT)
        up = jnp.clip(up, -SWIGLU_LIMIT, SWIGLU_LIMIT)
        hdn = gt * jax.nn.sigmoid(SWIGLU_ALPHA * gt) * (up + 1.0)
        return hdn @ w_down[e] + b_down[e]

    ys = lax.map(expert_block, (xs.reshape(n_blocks, MOE_BLOCK, D), block_e)).reshape(rows, D)
    contrib = ys[dest] * gates.reshape(-1)[order][:, None].astype(ys.dtype)
    out = jax.ops.segment_sum(contrib, tok, num_segments=n_tok)
    return out.reshape(b, S, D)


def setup_inputs(seed: int = 0) -> dict:
    key = jax.random.key(seed)
    ks = jax.random.split(key, 20)
    f32 = jnp.float32
    beta = DEEPNORM_BETA
    col_scale = jnp.concatenate([
        jnp.ones((2 * DIL_WIDTH,), f32), jnp.full((DIL_WIDTH,), beta, f32),
        jnp.ones((SWA_Q_WIDTH + SWA_KV_WIDTH,), f32), jnp.full((SWA_KV_WIDTH,), beta, f32),
        jnp.ones((2 * D_MODEL,), f32)])
    nrm = lambda k, shape: jax.random.normal(k, shape, f32)
    return {
        "x": nrm(ks[0], (BATCH, SEQ, D_MODEL)),
        "w_in": nrm(ks[1], (DEPTH, D_MODEL, IN_COLS)) * (D_MODEL ** -0.5) * col_scale,
        "sinks": 0.5 * nrm(ks[2], (DEPTH, SWA_Q_HEADS)),
        "w_proj_a": nrm(ks[3], (DEPTH, DIL_OUT, D_MODEL)) * (DIL_OUT ** -0.5),
        "w_proj_b": nrm(ks[4], (DEPTH, SWA_Q_WIDTH, D_MODEL)) * (SWA_Q_WIDTH ** -0.5),
        "w_out": nrm(ks[5], (DEPTH, D_MODEL, D_MODEL)) * (D_MODEL ** -0.5) * beta,
        "ln1_g": 1.0 + 0.05 * nrm(ks[6], (DEPTH, D_MODEL)),
        "ln1_b": 0.02 * nrm(ks[7], (DEPTH, D_MODEL)),
        "router_w": nrm(ks[8], (DEPTH, D_MODEL, N_EXPERTS)) * (D_MODEL ** -0.5),
        "router_b": 0.01 * nrm(ks[9], (DEPTH, N_EXPERTS)),
        "w_gate": nrm(ks[10], (DEPTH, N_EXPERTS, D_MODEL, D_FF)) * (D_MODEL ** -0.5),
        "b_gate": 0.02 * nrm(ks[11], (DEPTH, N_EXPERTS, D_FF)),
        "w_up": nrm(ks[12], (DEPTH, N_EXPERTS, D_MODEL, D_FF)) * (D_MODEL ** -0.5),
        "b_up": 0.02 * nrm(ks[13], (DEPTH, N_EXPERTS, D_FF)),
        "w_down": nrm(ks[14], (DEPTH, N_EXPERTS, D_FF, D_MODEL)) * (D_FF ** -0.5) * beta,
        "b_down": 0.02 * nrm(ks[15], (DEPTH, N_EXPERTS, D_MODEL)),
        "ln2_g": 1.0 + 0.05 * nrm(ks[16], (DEPTH, D_MODEL)),
        "ln2_b": 0.02 * nrm(ks[17], (DEPTH, D_MODEL)),
    }


def reference(x, w_in, sinks, w_proj_a, w_proj_b, w_out, ln1_g, ln1_b, router_w, router_b,
              w_gate, b_gate, w_up, b_up, w_down, b_down, ln2_g, ln2_b):
    slopes = alibi_slopes()
    for l in range(DEPTH):
        mix = token_mixer(x, w_in[l], sinks[l], w_proj_a[l], w_proj_b[l], w_out[l], slopes)
        x = layer_norm(DEEPNORM_ALPHA * x + mix, ln1_g[l], ln1_b[l])
        ffn = moe_ffn(x, router_w[l], router_b[l], w_gate[l], b_gate[l], w_up[l], b_up[l], w_down[l], b_down[l])
        x = layer_norm(DEEPNORM_ALPHA * x + ffn, ln2_g[l], ln2_b[l])
    return x
```

```python
import numpy as np
from contextlib import ExitStack
import concourse.bass as bass
import concourse.mybir as mybir
from concourse.bass_utils import run_bass_kernel_spmd

F32 = mybir.dt.float32
BF16 = mybir.dt.bfloat16
I32 = mybir.dt.int32
U32 = mybir.dt.uint32
AF = mybir.ActivationFunctionType
ALU = mybir.AluOpType
AX = mybir.AxisListType

S = 4096
D = 1024
NT = S // 128
NCORES = 8
ALPHA = 2.0 ** 0.25
LN_EPS = 1e-5
NEG = -1.0e30
NE = 32
CAP = 640
NSLOT = NE * CAP
IN_COLS = 5632
C_AQ, C_AK, C_AV, C_BQ, C_BK, C_BV, C_GA, C_GB = 0, 768, 1536, 2304, 3328, 3456, 3584, 4608
SLOPES = [2.0 ** (-8.0 * (h + 1) / 28.0) for h in range(28)]
MASKS = [(127, 1), (128, 1), (128, 4), (128, 16)]
DIL = [1, 1, 4, 16]

import os
ATT = int(os.environ.get("ATT", "9"))
NOLOAD = os.environ.get("KPROBE", "") == "noload"
COMPUTE = ("pe", "act", "dve", "pool")
QUEUES = ("sp", "pool", "act")
KDMA = 16


class Op:
    __slots__ = ("eng", "fn", "dma", "deps", "inc", "sem", "semval", "slotwait", "name", "idx")


class Sched:
    def __init__(self, nc, es):
        self.nc = nc
        self.streams = {e: [] for e in ("pe", "act", "dve", "pool", "sp")}
        self.last_w = {}
        self.readers = {}
        self.csem = {e: es.enter_context(nc.semaphore("s_" + e)) for e in COMPUTE}
        self.dsem = {q: [es.enter_context(nc.semaphore("d_%s%d" % (q, i))) for i in range(KDMA)]
                     for q in QUEUES}
        self.dma_n = {q: 0 for q in QUEUES}
        self.dma_last = {}
        self.last_c = {}

    def add(self, eng, fn, reads=(), writes=(), dma=False, name=None):
        op = Op()
        op.eng, op.fn, op.dma, op.inc, op.name = eng, fn, dma, False, name
        op.sem = None
        op.semval = 0
        op.slotwait = None
        deps = set()
        for r in reads:
            w = self.last_w.get(r)
            if w is not None:
                deps.add(w)
        for k in writes:
            w = self.last_w.get(k)
            if w is not None:
                deps.add(w)
            for rd in self.readers.get(k, ()):
                deps.add(rd)
        if eng == "pe":
            deps = {d for d in deps if not (d.eng == "pe" and not d.dma)}
        latest = {}
        red = set()
        for d in deps:
            if d.dma:
                red.add(d)
            elif d.eng not in latest or latest[d.eng].idx < d.idx:
                latest[d.eng] = d
        red.update(latest.values())
        op.deps = red
        op.idx = len(self.streams[eng])
        for r in reads:
            self.readers.setdefault(r, []).append(op)
        for k in writes:
            self.last_w[k] = op
            self.readers[k] = []
        if dma:
            n = self.dma_n[eng]
            self.dma_n[eng] = n + 1
            op.sem = self.dsem[eng][n % KDMA]
            op.semval = 16 * (n // KDMA + 1)
            if n >= KDMA:
                op.slotwait = (op.sem, 16 * (n // KDMA))
            self.dma_last[(eng, n % KDMA)] = op
        else:
            self.last_c[eng] = op
        self.streams[eng].append(op)
        return op

    def barrier(self):
        snap = set(self.last_c.values()) | set(self.dma_last.values())
        for e in self.streams:
            op = Op()
            op.eng, op.fn, op.dma, op.inc, op.name = e, None, False, False, "barrier"
            op.sem, op.semval, op.slotwait = None, 0, None
            op.deps = set(snap)
            op.idx = len(self.streams[e])
            self.streams[e].append(op)
        self.last_w = {}
        self.readers = {}

    def emit(self, es):
        nc = self.nc
        for st in self.streams.values():
            for op in st:
                for d in op.deps:
                    d.inc = True
        for e, st in self.streams.items():
            cnt = 0
            for op in st:
                if (not op.dma) and op.inc and op.fn is not None:
                    cnt += 1
                    op.sem = self.csem[e]
                    op.semval = cnt
        block = es.enter_context(nc.Block())

        def body(ename):
            def run(e):
                seen = {}
                for op in self.streams[ename]:
                    waits = {}
                    if op.slotwait is not None:
                        waits[id(op.slotwait[0])] = op.slotwait
                    for d in op.deps:
                        if d.sem is None:
                            continue
                        k = id(d.sem)
                        if k not in waits or waits[k][1] < d.semval:
                            waits[k] = (d.sem, d.semval)
                    for k, (s, v) in waits.items():
                        if seen.get(k, 0) < v:
                            e.wait_ge(s, v)
                            seen[k] = v
                    if op.fn is None:
                        continue
                    inst = op.fn(e)
                    if op.dma:
                        inst.then_inc(op.sem, 16)
                    elif op.inc:
                        inst.then_inc(op.sem, 1)
                for (q, i), dop in self.dma_last.items():
                    if q == ename and seen.get(id(dop.sem), 0) < dop.semval:
                        e.wait_ge(dop.sem, dop.semval)
            return run

        block.tensor(body("pe"))
        block.scalar(body("act"))
        block.vector(body("dve"))
        block.gpsimd(body("pool"))
        block.sync(body("sp"))


def host_consts():
    ident = np.eye(128, dtype=np.float32)
    nd = np.zeros((128, 4, 2, 128), dtype=np.float32)
    k = np.arange(128)[:, None]
    q = np.arange(128)[None, :]
    for m, (maxd, scale) in enumerate(MASKS):
        for kb in range(2):
            diff = q - k + (128 if kb == 0 else 0)
            valid = (diff >= 0) & (diff <= maxd)
            nd[:, m, kb, :] = np.where(valid, -8.0 * scale * diff, NEG)
    return ident, nd.reshape(128, 4 * 256)


def build(stage="full"):
    nc = bass.Bass("TRN2", target_bir_lowering=False)
    x = nc.dram_tensor("x", [S, D], F32, kind="ExternalInput").ap()
    w_in = nc.dram_tensor("w_in", [D, IN_COLS], F32, kind="ExternalInput").ap()
    ident_d = nc.dram_tensor("ident", [128, 128], F32, kind="ExternalInput").ap()
    negd_d = nc.dram_tensor("negd", [128, 1024], F32, kind="ExternalInput").ap()
    sinkc_d = nc.dram_tensor("sinkc", [128, 8], F32, kind="ExternalInput").ap()
    out = nc.dram_tensor("out", [S, D], F32, kind="ExternalOutput").ap()
    oT_d = nc.dram_tensor("oT_scratch", [10, 128, S], BF16, kind="ExternalOutput").ap()
    w_in_v = w_in.rearrange("(dc p) c -> p dc c", p=128)
    wpa_d = nc.dram_tensor("w_proj_a", [256, D], F32, kind="ExternalInput").ap()
    wpb_d = nc.dram_tensor("w_proj_b", [D, D], F32, kind="ExternalInput").ap()
    wo_d = nc.dram_tensor("w_out", [D, D], F32, kind="ExternalInput").ap()
    lnp_d = nc.dram_tensor("lnp", [128, 4 * D], F32, kind="ExternalInput").ap()
    rw_d = nc.dram_tensor("router_w", [D, 32], F32, kind="ExternalInput").ap()
    rb_d = nc.dram_tensor("rb", [128, 32], F32, kind="ExternalInput").ap()
    cst_d = nc.dram_tensor("cst", [128, 320], F32, kind="ExternalInput").ap()
    wgate_d = nc.dram_tensor("w_gate", [32, D, D], F32, kind="ExternalInput").ap()
    wup_d = nc.dram_tensor("w_up", [32, D, D], F32, kind="ExternalInput").ap()
    wdown_d = nc.dram_tensor("w_down", [32, D, D], F32, kind="ExternalInput").ap()
    bgu_d = nc.dram_tensor("bgu", [128, 512], F32, kind="ExternalInput").ap()
    bdn_d = nc.dram_tensor("b_down", [32, D], F32, kind="ExternalInput").ap()
    y32_d = nc.dram_tensor("y32_scratch", [S, D], F32, kind="ExternalOutput").ap()
    xs_d = nc.dram_tensor("xs_scratch", [NSLOT, D], BF16, kind="ExternalOutput").ap()
    ys_d = nc.dram_tensor("ys_scratch", [NSLOT, D], BF16, kind="ExternalOutput").ap()
    es = ExitStack()
    with es:
        sc = Sched(nc, es)

        def sb(name, shape, dt, st=es):
            return st.enter_context(nc.sbuf_tensor("sb_" + name, shape, dt))

        def ps(name, shape, dt, st=es):
            return st.enter_context(nc.psum_tensor("ps_" + name, shape, dt))

        ident = sb("ident", [128, 128], F32)
        identb = sb("identb", [128, 128], BF16)
        negd = sb("negd", [128, 4, 256], F32)
        onesb = sb("onesb", [128, 64], BF16)
        esink = sb("esink", [128, 8], F32)
        idx_all = sb("idx_all", [128, NT * 4], I32)
        gate_all = sb("gate_all", [128, NT * 4], F32)
        cst = sb("cst", [128, 320], F32)
        utri, ones_f, iota_e, ebase = cst[:, 0:128], cst[:, 128:256], cst[:, 256:288], cst[:, 288:320]
        zfill = sb("zfill", [128, 2048], BF16)
        sc.add("dve", lambda e: e.memset(zfill[:], 0.0), writes=["zfill"])
        pX = ExitStack()
        xT = sb("xT", [128, 8, S], BF16, pX)

        sc.add("sp", lambda e: e.dma_start(out=ident[:], in_=ident_d), writes=["ident"], dma=True)
        sc.add("sp", lambda e: e.dma_start(out=negd[:].rearrange("p m c -> p (m c)"), in_=negd_d),
               writes=["negd"], dma=True)
        sc.add("sp", lambda e: e.dma_start(out=esink[:], in_=sinkc_d), writes=["esink"], dma=True)
        sc.add("dve", lambda e: e.tensor_copy(out=identb[:], in_=ident[:]), reads=["ident"], writes=["identb"])
        sc.add("dve", lambda e: e.memset(onesb[:], 1.0), writes=["onesb"])
        sc.add("act", lambda e: e.activation(out=esink[:], in_=esink[:], func=AF.Exp),
               reads=["esink"], writes=["esink"])

        with ExitStack() as p0:
            xin = [sb("xin%d" % i, [128, D], F32, p0) for i in range(2)]
            pst = [ps("pst%d" % i, [128, 512], F32, p0) for i in range(4)]
            for i in range(NT):
                b = i % 2
                sc.add("sp", lambda e, i=i, b=b: e.dma_start(out=xin[b][:], in_=x[i * 128:(i + 1) * 128, :]),
                       writes=[("xin", b)], dma=True)
                for h in range(2):
                    pb = (2 * i + h) % 4
                    for c4 in range(4):
                        c = h * 4 + c4
                        sc.add("pe", lambda e, b=b, c=c, c4=c4, pb=pb: e.transpose(
                            out=pst[pb][:, c4 * 128:(c4 + 1) * 128], in_=xin[b][:, c * 128:(c + 1) * 128],
                            identity=ident[:]),
                            reads=[("xin", b), "ident"], writes=[("pst", pb)])
                    if h == 0:
                        fn = lambda e, i=i, h=h, pb=pb: e.copy(
                            out=xT[:, h * 4:(h + 1) * 4, i * 128:(i + 1) * 128],
                            in_=pst[pb][:].rearrange("p (c t) -> p c t", c=4))
                    else:
                        fn = lambda e, i=i, h=h, pb=pb: e.tensor_copy(
                            out=xT[:, h * 4:(h + 1) * 4, i * 128:(i + 1) * 128],
                            in_=pst[pb][:].rearrange("p (c t) -> p c t", c=4))
                    sc.add("act" if h == 0 else "dve", fn, reads=[("pst", pb)], writes=[("xT", i)])
            sc.barrier()

        with ExitStack() as p1:
            QT = [sb("QT%d" % i, [128, S], BF16, p1) for i in range(2)]
            KTd = sb("KTd", [128, S], BF16, p1)
            Vd = sb("Vd", [128, NT, 128], BF16, p1)
            KTs = [sb("KTs%d" % i, [128, S], BF16, p1) for i in range(2)]
            Vs = sb("Vs", [128, NT, 128], BF16, p1)
            accN = sb("accN", [128, S], F32, p1)
            accD = sb("accD", [128, S], F32, p1)
            wq = [sb("wq%d" % i, [128, 8, 128], BF16, p1) for i in range(2)]
            wk = [sb("wk%d" % i, [128, 8, 128], BF16, p1) for i in range(2)]
            wv = [sb("wv%d" % i, [128, 8, 128], BF16, p1) for i in range(2)]
            tmp = [sb("tmp%d" % i, [128, 512], F32, p1) for i in range(3)]
            PT = [sb("PT%d" % i, [128, 512], BF16, p1) for i in range(3)]
            bias2 = [sb("bias2_%d" % i, [128, 2, 256], F32, p1) for i in range(2)]
            stg = [sb("stg%d" % i, [128, 512], BF16, p1) for i in range(2)]
            t1 = [sb("t1_%d" % i, [128, 512], F32, p1) for i in range(2)]
            pSall = ps("pSall", [128, 6, 512], F32, p1)
            pND = [ps("pND%d" % i, [128, 512], F32, p1) for i in range(2)]
            pj = pND
            cnt = {"pj": 0, "ev": 0, "blk": 0, "stg": 0, "zf": 0}

            def load_w(dst, key, c0, ncols=128, d0=0):
                sc.add("pool", lambda e: e.dma_start(out=dst[:, :, d0:d0 + ncols], in_=w_in_v[:, :, c0:c0 + ncols]),
                       writes=[key], dma=True)

            def evac(fn_act, fn_dve, reads, writes):
                k = cnt["ev"]
                cnt["ev"] += 1
                if k % 2 == 0:
                    sc.add("act", fn_act, reads=reads, writes=writes)
                else:
                    sc.add("dve", fn_dve, reads=reads, writes=writes)

            def proj_fm(w, wkey, dst, dkey, r):
                for tg in range(8):
                    b = cnt["pj"] % 2
                    cnt["pj"] += 1
                    for dc in range(8):
                        sc.add("pe", lambda e, b=b, dc=dc, tg=tg: e.matmul(
                            out=pj[b][:], lhsT=w[:, dc, :], rhs=xT[:, dc, tg * 512:(tg + 1) * 512],
                            start=(dc == 0), stop=(dc == 7)),
                            reads=[wkey], writes=[("pND", b)])
                    if r == 1:
                        o_ap = lambda tg=tg: dst[:, tg * 512:(tg + 1) * 512]
                        i_ap = lambda b=b: pj[b][:]
                    else:
                        n = 512 // r
                        o_ap = lambda tg=tg, n=n: dst[:].rearrange("p (r m) -> p r m", r=r)[:, :, tg * n:(tg + 1) * n]
                        i_ap = lambda b=b: pj[b][:].rearrange("p (m r) -> p r m", r=r)
                    evac(lambda e, o_ap=o_ap, i_ap=i_ap: e.copy(out=o_ap(), in_=i_ap()),
                         lambda e, o_ap=o_ap, i_ap=i_ap: e.tensor_copy(out=o_ap(), in_=i_ap()),
                         reads=[("pND", b)], writes=[(dkey, tg)])

            def proj_tm(w, wkey, dst, dkey, r):
                nbs = NT // r
                for b4 in range(NT // 4):
                    b = cnt["pj"] % 2
                    cnt["pj"] += 1
                    for k4 in range(4):
                        qb = b4 * 4 + k4
                        res, j = qb // nbs, qb % nbs
                        t0 = res + r * 128 * j
                        for dc in range(8):
                            sc.add("pe", lambda e, b=b, dc=dc, k4=k4, t0=t0: e.matmul(
                                out=pj[b][:, k4 * 128:(k4 + 1) * 128],
                                lhsT=xT[:, dc, t0:t0 + 127 * r + 1:r], rhs=w[:, dc, :],
                                start=(dc == 0), stop=(dc == 7)),
                                reads=[wkey], writes=[("pND", b)])
                    evac(lambda e, b=b, b4=b4: e.copy(out=dst[:, b4 * 4:(b4 + 1) * 4, :],
                                                     in_=pj[b][:].rearrange("p (k c) -> p k c", k=4)),
                         lambda e, b=b, b4=b4: e.tensor_copy(out=dst[:, b4 * 4:(b4 + 1) * 4, :],
                                                            in_=pj[b][:].rearrange("p (k c) -> p k c", k=4)),
                         reads=[("pND", b)], writes=[(dkey, b4)])

            def attention(g, Qb, qkeys, Kb, kkeys, Vb, vkey, vcol, slopes2, on_done, par):
                r = DIL[g]
                nbs = NT // r
                for hh in range(2):
                    sc.add("dve", lambda e, hh=hh: e.tensor_scalar(
                        out=bias2[par][:, hh, :], in0=negd[:, g, :], scalar1=float(slopes2[hh]), scalar2=None, op0=ALU.mult),
                        writes=[("bias2", par)])

                def scores(qb):
                    j = qb % nbs
                    sb_ = qb % 3
                    kbs = (0, 1) if j > 0 else (1,)
                    c0 = 0 if j > 0 else 128
                    for hh in range(2):
                        for kb in kbs:
                            kblk = qb - 1 + kb
                            sc.add("pe", lambda e, hh=hh, kb=kb, kblk=kblk: e.matmul(
                                out=pSall[:, 2 * sb_ + hh, kb * 128:(kb + 1) * 128],
                                lhsT=Kb[hh * 64:(hh + 1) * 64, kblk * 128:(kblk + 1) * 128],
                                rhs=Qb[hh * 64:(hh + 1) * 64, qb * 128:(qb + 1) * 128],
                                start=True, stop=True),
                                reads=list(qkeys) + list(kkeys), writes=[("pS", sb_, hh)])
                    t3 = lambda ap: ap[:].rearrange("p (h c) -> p h c", h=2)[:, :, c0:256]
                    sc.add("dve", lambda e: e.tensor_tensor(
                        out=t3(tmp[sb_]), in0=pSall[:, 2 * sb_:2 * sb_ + 2, c0:256], in1=bias2[par][:, :, c0:256], op=ALU.add),
                        reads=[("pS", sb_, 0), ("pS", sb_, 1), ("bias2", par)], writes=[("tmp", sb_)])
                    sc.add("act", lambda e: e.activation(out=t3(PT[sb_]), in_=t3(tmp[sb_]), func=AF.Exp, scale=0.125),
                           reads=[("tmp", sb_)], writes=[("PT", sb_)])

                def pv(qb):
                    j = qb % nbs
                    sb_ = qb % 3
                    kbs = (0, 1) if j > 0 else (1,)
                    b2i, k2 = qb // 2, qb % 2
                    pb = b2i % 2
                    for hh in range(2):
                        for (col0, isnum) in ((k2 * 128, True), (256 + k2 * 128, False)):
                            for ki, kb in enumerate(kbs):
                                kblk = qb - 1 + kb
                                lh = (lambda kblk=kblk, hh=hh: Vb[:, kblk, vcol[hh]:vcol[hh] + 64]) if isnum \
                                    else (lambda: onesb[:, 0:64])
                                rd = [("PT", sb_)] + ([(vkey, kblk // 4)] if isnum else [])
                                sc.add("pe", lambda e, hh=hh, kb=kb, ki=ki, col0=col0, lh=lh: e.matmul(
                                    out=pND[pb][hh * 64:(hh + 1) * 64, col0:col0 + 128],
                                    lhsT=lh(),
                                    rhs=PT[sb_][:, (hh * 2 + kb) * 128:(hh * 2 + kb + 1) * 128],
                                    start=(ki == 0), stop=(ki == len(kbs) - 1)),
                                    reads=rd, writes=[("pND", pb)])
                    if k2 == 1:
                        on_done(b2i, pb)

                for qb in range(NT + 2):
                    if qb < NT:
                        scores(qb)
                    if qb >= 2:
                        pv(qb - 2)

            ZCH = (NSLOT // 128) * D // 2048
            xs_flat = xs_d.rearrange("(p r) d -> p (r d)", p=128)

            def store_stage(chunk, t0, sbuf_i):
                sc.add("sp", lambda e: e.dma_start(out=oT_d[chunk, :, t0:t0 + 512], in_=stg[sbuf_i][:]),
                       reads=[("stg", sbuf_i)], dma=True)
                zi = cnt["zf"]
                if zi < ZCH:
                    cnt["zf"] += 1
                    sc.add("sp", lambda e: e.dma_start(out=xs_flat[:, zi * 2048:(zi + 1) * 2048], in_=zfill[:]),
                           reads=["zfill"], dma=True)

            for kv in range(2):
                load_w(wk[kv], ("wk", kv), C_BK + kv * 64, 64, 0)
                load_w(wk[kv], ("wk", kv), C_BK + kv * 64, 64, 64)
            load_w(wv[0], ("wv", 0), C_BV, 128)
            for kv in range(2):
                proj_fm(wk[kv], ("wk", kv), KTs[kv], ("KTs", kv), 1)
            proj_tm(wv[0], ("wv", 0), Vs, "Vs", 1)

            if stage == "p1a":
                dbg = sb("dbg", [128, S], F32, p1)
                for ci, (src, keys) in enumerate([(KTs[0][:], [(("KTs", 0), tg) for tg in range(8)]),
                                                  (KTs[1][:], [(("KTs", 1), tg) for tg in range(8)]),
                                                  (Vs[:].rearrange("p b c -> p (b c)"), [("Vs", b4) for b4 in range(8)])]):
                    sc.add("dve", lambda e, src=src: e.tensor_copy(out=dbg[:], in_=src), reads=keys, writes=["dbg"])
                    sc.add("sp", lambda e, ci=ci: e.dma_start(
                        out=out.rearrange("(r q) d -> r (q d)", q=4)[ci * 128:(ci + 1) * 128, :], in_=dbg[:]),
                        reads=["dbg"], dma=True)
            jobs = [(0, p) for p in range(8)] + [(g, pp) for pp in range(2) for g in (1, 2, 3)]
            if stage == "p1a":
                jobs = []
            if stage == "swa0":
                jobs = [(0, 0), (0, 5)]
            if stage == "dil0":
                jobs = [(1, 0), (2, 0), (3, 0)]
            for ji, (g, p) in enumerate(jobs):
                par = ji % 2
                r = DIL[g]
                if g == 0:
                    load_w(wq[par], ("wq", par), C_BQ + p * 128)
                    proj_fm(wq[par], ("wq", par), QT[par], ("Q", par), 1)
                    kv = p // 4
                    sl = (SLOPES[2 * p], SLOPES[2 * p + 1])

                    def done(b2i, pb, p=p):
                        b4, half = b2i // 2, b2i % 2
                        si = b4 % 2
                        hs = slice(half * 256, (half + 1) * 256)
                        sc.add("dve", lambda e: e.tensor_scalar(
                            out=t1[si][:, hs], in0=pND[pb][:, 256:512], scalar1=esink[:, p:p + 1], scalar2=None, op0=ALU.add),
                            reads=[("pND", pb)], writes=[("t1", si, half)])
                        sc.add("dve", lambda e: e.reciprocal(out=t1[si][:, hs], in_=t1[si][:, hs]),
                               reads=[("t1", si, half)], writes=[("t1", si, half)])
                        sc.add("dve", lambda e: e.tensor_tensor(
                            out=stg[si][:, hs], in0=pND[pb][:, 0:256], in1=t1[si][:, hs], op=ALU.mult),
                            reads=[("pND", pb), ("t1", si, half)], writes=[("stg", si)])
                        if half == 1:
                            store_stage(p, b4 * 512, si)

                    attention(0, QT[par], [(("Q", par), tg) for tg in range(8)],
                              KTs[kv], [(("KTs", kv), tg) for tg in range(8)],
                              Vs, "Vs", (kv * 64, kv * 64), sl, done, par)
                else:
                    c0 = (g - 1) * 256 + p * 128
                    load_w(wq[par], ("wq", par), C_AQ + c0)
                    load_w(wk[par], ("wk", par), C_AK + c0)
                    load_w(wv[par], ("wv", par), C_AV + c0)
                    proj_fm(wq[par], ("wq", par), QT[par], ("Q", par), r)
                    proj_fm(wk[par], ("wk", par), KTd, "KTd", r)
                    proj_tm(wv[par], ("wv", par), Vd, "Vd", r)
                    sl = (SLOPES[16 + (g - 1) * 4 + 2 * p], SLOPES[16 + (g - 1) * 4 + 2 * p + 1])
                    nbs = NT // r

                    def done(b2i, pb, g=g, r=r, nbs=nbs):
                        qb0 = b2i * 2
                        res, j0 = qb0 // nbs, qb0 % nbs
                        n, t0 = 256, res + r * 128 * j0
                        halves = sorted({(t0 + r * i) // 2048 for i in (0, n - 1)})
                        keys = [("acc", h) for h in halves]
                        for acc, pc in ((accN, 0), (accD, 256)):
                            if g == 1:
                                sc.add("act", lambda e, acc=acc, pc=pc: e.copy(
                                    out=acc[:, t0:t0 + (n - 1) * r + 1:r], in_=pND[pb][:, pc:pc + n]),
                                    reads=[("pND", pb)], writes=keys)
                            else:
                                sc.add("dve", lambda e, acc=acc, pc=pc: e.tensor_tensor(
                                    out=acc[:, t0:t0 + (n - 1) * r + 1:r], in0=pND[pb][:, pc:pc + n],
                                    in1=acc[:, t0:t0 + (n - 1) * r + 1:r], op=ALU.add),
                                    reads=[("pND", pb)] + keys, writes=keys)

                    attention(g, QT[par], [(("Q", par), tg) for tg in range(8)],
                              KTd, [("KTd", tg) for tg in range(8)],
                              Vd, "Vd", (0, 64), sl, done, par)
                    if g == 3:
                        for t8 in range(8):
                            si = t8 % 2
                            keys = [("acc", t8 // 4)]
                            sc.add("dve", lambda e, t8=t8, si=si: e.reciprocal(
                                out=t1[si][:], in_=accD[:, t8 * 512:(t8 + 1) * 512]),
                                reads=keys, writes=[("t1", si, 0), ("t1", si, 1)])
                            sc.add("dve", lambda e, t8=t8, si=si: e.tensor_tensor(
                                out=stg[si][:], in0=accN[:, t8 * 512:(t8 + 1) * 512], in1=t1[si][:], op=ALU.mult),
                                reads=keys + [("t1", si, 0), ("t1", si, 1)], writes=[("stg", si)])
                            store_stage(8 + p, t8 * 512, si)
            sc.barrier()


        if stage == "p01":
            pX.close()
            sc.emit(es)
            return nc
        if stage in ("swa0", "dil0", "attn"):
            with ExitStack() as pd:
                dbgb = sb("dbgb", [128, S], BF16, pd)
                dbg = sb("dbg", [128, S], F32, pd)
                chunks = {"swa0": [0, 5], "dil0": [8], "attn": list(range(8))}[stage]
                for ci, c in enumerate(chunks):
                    sc.add("sp", lambda e, c=c: e.dma_start(out=dbgb[:], in_=oT_d[c, :, :]), writes=["dbgb"], dma=True)
                    sc.add("dve", lambda e: e.tensor_copy(out=dbg[:], in_=dbgb[:]), reads=["dbgb"], writes=["dbg"])
                    sc.add("sp", lambda e, ci=ci: e.dma_start(
                        out=out.rearrange("(r q) d -> r (q d)", q=4)[ci * 128:(ci + 1) * 128, :], in_=dbg[:]),
                        reads=["dbg"], dma=True)
            sc.emit(es)
            return nc

        _regs = {}

        def bcreg(e):
            if "bc" not in _regs:
                r = e.alloc_register("bc")
                e.reg_mov(r, NSLOT - 1)
                _regs["bc"] = r
            return _regs["bc"]

        def ln_stats(p, zt, zkey, stats, mv):
            for h in range(2):
                sc.add("dve", lambda e, h=h: e.bn_stats(out=stats[:, h * 6:(h + 1) * 6], in_=zt[:, h * 512:(h + 1) * 512]),
                       reads=[zkey], writes=[("stats", p)])
            sc.add("dve", lambda e: e.bn_aggr(out=mv[:, 0:2], in_=stats[:, 0:12]), reads=[("stats", p)], writes=[("mv", p)])
            sc.add("dve", lambda e: e.tensor_scalar(out=mv[:, 2:3], in0=mv[:, 1:2], scalar1=LN_EPS, scalar2=None, op0=ALU.add),
                   reads=[("mv", p)], writes=[("mv", p)])
            sc.add("act", lambda e: e.activation(out=mv[:, 2:3], in_=mv[:, 2:3], func=AF.Sqrt),
                   reads=[("mv", p)], writes=[("mv", p)])
            sc.add("dve", lambda e: e.reciprocal(out=mv[:, 3:4], in_=mv[:, 2:3]), reads=[("mv", p)], writes=[("mv", p)])
            sc.add("dve", lambda e: e.scalar_tensor_tensor(out=mv[:, 4:5], in0=mv[:, 0:1], scalar=-1.0, in1=mv[:, 3:4],
                                                          op0=ALU.mult, op1=ALU.mult),
                   reads=[("mv", p)], writes=[("mv", p)])

        def ln_apply(p, zt, zkey, gam, bet, dst, dkey, mv, tmpn, tkeys=None, gain_eng="dve"):
            tk = list(tkeys) if tkeys is not None else [("tmpn", p)]
            sc.add("act", lambda e: e.activation(out=tmpn[:], in_=zt[:], func=AF.Identity, scale=mv[:, 3:4], bias=mv[:, 4:5]),
                   reads=[zkey, ("mv", p)], writes=tk)
            sc.add(gain_eng, lambda e: e.tensor_tensor(out=tmpn[:], in0=tmpn[:], in1=gam, op=ALU.mult),
                   reads=tk, writes=tk)
            sc.add("pool", lambda e: e.tensor_tensor(out=dst, in0=tmpn[:], in1=bet, op=ALU.add),
                   reads=tk, writes=[dkey])

        def layer_norm(p, zt, zkey, gam, bet, dst, dkey, stats, mv, tmpn, gain_eng="dve"):
            ln_stats(p, zt, zkey, stats, mv)
            ln_apply(p, zt, zkey, gam, bet, dst, dkey, mv, tmpn, gain_eng=gain_eng)


        with ExitStack() as p2:
            wg = sb("wg", [128, 8, 2048], BF16, p2)
            wpa = sb("wpa", [128, 2, 1024], BF16, p2)
            wpb = sb("wpb", [128, 8, 1024], BF16, p2)
            wo = sb("wo", [128, 8, 1024], BF16, p2)
            rw = sb("rw", [128, 8, 32], F32, p2)
            rb = sb("rb", [128, 32], F32, p2)
            ln1 = sb("ln1", [128, 2, 1024], F32, p2)
            oTt = [sb("oTt%d" % i, [128, 10, 128], BF16, p2) for i in range(2)]
            xres = sb("xres", [128, D], F32, p2)
            sg = sb("sg", [128, 2048], F32, p2)
            m1 = sb("m1", [128, D], F32, p2)
            m2 = sb("m2", [128, D], F32, p2)
            mT = sb("mT", [128, 8, 128], BF16, p2)
            zt = sb("zt", [128, D], F32, p2)
            y32 = sb("y32", [128, D], F32, p2)
            ybf = [sb("ybf%d" % i, [128, D], BF16, p2) for i in range(2)]
            yT = sb("yT", [128, 8, 128], F32, p2)
            stats = sb("stats", [128, 12], F32, p2)
            mv = sb("mv", [128, 8], F32, p2)
            lg = sb("lg", [128, 32], F32, p2)
            top8 = sb("top8", [128, 8], F32, p2)
            idx8 = sb("idx8", [128, 8], U32, p2)
            idxf = sb("idxf", [128, 8], F32, p2)
            negmax = sb("negmax", [128, 1], F32, p2)
            e4 = sb("e4", [128, 4], F32, p2)
            s4 = sb("s4", [128, 2], F32, p2)
            mask = sb("mask", [128, 32], F32, p2)
            slotf = sb("slotf", [128, 32], F32, p2)
            ovf = sb("ovf", [128, 32], F32, p2)
            CNT = sb("CNT", [128, 32], F32, p2)
            oh = sb("oh", [128, 32], F32, p2)
            j32 = sb("j32", [128, 32], F32, p2)
            slotk = sb("slotk", [128, 4], F32, p2)
            okk = sb("okk", [128, 4], F32, p2)
            B = [ps("B%d" % i, [128, 512], F32, p2) for i in range(8)]

            sc.add("sp", lambda e: e.dma_start(out=cst[:], in_=cst_d), writes=["cst"], dma=True)
            sc.add("sp", lambda e: e.dma_start(out=rw[:], in_=rw_d.rearrange("(dc p) e -> p dc e", p=128)), writes=["rw"], dma=True)
            sc.add("sp", lambda e: e.dma_start(out=rb[:], in_=rb_d), writes=["rb"], dma=True)
            sc.add("sp", lambda e: e.dma_start(out=ln1[:].rearrange("p a d -> p (a d)"), in_=lnp_d[:, 0:2048]), writes=["ln1"], dma=True)
            for n4 in range(4):
                sc.add("pool", lambda e, n4=n4: e.dma_start(out=wg[:, :, n4 * 512:(n4 + 1) * 512],
                                                            in_=w_in_v[:, :, C_GA + n4 * 512:C_GA + (n4 + 1) * 512]),
                       writes=["wg"], dma=True)
            sc.add("pool", lambda e: e.dma_start(out=wpa[:], in_=wpa_d.rearrange("(c p) d -> p c d", p=128)), writes=["wpa"], dma=True)
            for hf in range(2):
                sc.add("pool", lambda e, hf=hf: e.dma_start(out=wpb[:, :, hf * 512:(hf + 1) * 512],
                                                            in_=wpb_d.rearrange("(c p) d -> p c d", p=128)[:, :, hf * 512:(hf + 1) * 512]),
                       writes=["wpb"], dma=True)
                sc.add("pool", lambda e, hf=hf: e.dma_start(out=wo[:, :, hf * 512:(hf + 1) * 512],
                                                            in_=wo_d.rearrange("(c p) d -> p c d", p=128)[:, :, hf * 512:(hf + 1) * 512]),
                       writes=["wo"], dma=True)
            sc.add("dve", lambda e: e.memset(CNT[:], 0.0), writes=["CNT"])
            sc.barrier()

            zts = [zt, sb("zt1", [128, D], F32, p2)]
            y32s = [y32, sb("y32b", [128, D], F32, p2)]
            Fb = B[0:5]
            CT = B[5:7]
            CL = B[7]

            def A_loads(i):
                b = i % 2
                tsl = slice(i * 128, (i + 1) * 128)
                sc.add("sp", lambda e: e.dma_start(out=oTt[b][:], in_=oT_d[:, :, tsl].rearrange("c p t -> p c t")),
                       writes=[("oTt", b)], dma=True)
                sc.add("sp", lambda e: e.dma_start(out=xres[:], in_=x[tsl, :]), writes=["xres"], dma=True)

            def A_gates(i, half):
                tsl = slice(i * 128, (i + 1) * 128)
                for n2 in range(2):
                    n4 = half * 2 + n2
                    for dc in range(8):
                        sc.add("pe", lambda e, n2=n2, n4=n4, dc=dc: e.matmul(
                            out=Fb[n2][:], lhsT=xT[:, dc, tsl], rhs=wg[:, dc, n4 * 512:(n4 + 1) * 512],
                            start=(dc == 0), stop=(dc == 7)), writes=[("F", n2)])
                    sc.add("act", lambda e, n2=n2, n4=n4: e.activation(out=sg[:, n4 * 512:(n4 + 1) * 512], in_=Fb[n2][:],
                                                                        func=AF.Sigmoid),
                           reads=[("F", n2)], writes=[("sg", n4)])

            def A_proj(i, which):
                b = i % 2
                nck, w_, c0 = (2, wpa, 8) if which == 0 else (8, wpb, 0)
                for hf in range(2):
                    for c in range(nck):
                        sc.add("pe", lambda e, hf=hf, c=c: e.matmul(
                            out=Fb[2 + hf][:], lhsT=oTt[b][:, c0 + c, :], rhs=w_[:, c, hf * 512:(hf + 1) * 512],
                            start=(c == 0), stop=(c == nck - 1)), reads=[("oTt", b)], writes=[("F", 2 + hf)])

            def A_m(i, which):
                for hf in range(2):
                    hs = slice(hf * 512, (hf + 1) * 512)
                    if which == 0:
                        sc.add("dve", lambda e, hf=hf, hs=hs: e.tensor_tensor(
                            out=m1[:, hs], in0=Fb[2 + hf][:], in1=sg[:, hs], op=ALU.mult),
                            reads=[("F", 2 + hf), ("sg", hf)], writes=[("m1", hf)])
                    else:
                        sc.add("dve", lambda e, hf=hf, hs=hs: e.tensor_tensor(
                            out=m2[:, hs], in0=Fb[2 + hf][:], in1=sg[:, 1024 + hf * 512:1024 + (hf + 1) * 512], op=ALU.mult),
                            reads=[("F", 2 + hf), ("sg", 2 + hf)], writes=[("m2", hf)])
                        sc.add("pool", lambda e, hs=hs: e.tensor_tensor(out=m1[:, hs], in0=m1[:, hs], in1=m2[:, hs], op=ALU.add),
                               reads=[("m1", hf), ("m2", hf)], writes=[("m1", hf)])

            def A_tail(i):
                zb = i % 2
                tb = (4, 0)
                for hf in range(2):
                    for c4 in range(4):
                        c = hf * 4 + c4
                        sc.add("pe", lambda e, hf=hf, c=c, c4=c4: e.transpose(
                            out=Fb[tb[hf]][:, c4 * 128:(c4 + 1) * 128], in_=m1[:, c * 128:(c + 1) * 128], identity=ident[:]),
                            reads=[("m1", hf)], writes=[("F", tb[hf])])
                    fa = lambda e, hf=hf: e.copy(out=mT[:, hf * 4:(hf + 1) * 4, :],
                                                 in_=Fb[tb[hf]][:].rearrange("p (c t) -> p c t", c=4))
                    fd = lambda e, hf=hf: e.tensor_copy(out=mT[:, hf * 4:(hf + 1) * 4, :],
                                                        in_=Fb[tb[hf]][:].rearrange("p (c t) -> p c t", c=4))
                    sc.add("act" if hf == 0 else "dve", fa if hf == 0 else fd, reads=[("F", tb[hf])], writes=[("mT", hf)])
                for hf in range(2):
                    for c in range(8):
                        sc.add("pe", lambda e, hf=hf, c=c: e.matmul(
                            out=Fb[2 + hf][:], lhsT=mT[:, c, :], rhs=wo[:, c, hf * 512:(hf + 1) * 512],
                            start=(c == 0), stop=(c == 7)), reads=[("mT", 0), ("mT", 1)], writes=[("F", 2 + hf)])
                    sc.add("dve", lambda e, hf=hf: e.scalar_tensor_tensor(
                        out=zts[zb][:, hf * 512:(hf + 1) * 512], in0=xres[:, hf * 512:(hf + 1) * 512], scalar=float(ALPHA),
                        in1=Fb[2 + hf][:], op0=ALU.mult, op1=ALU.add),
                        reads=["xres", ("F", 2 + hf)], writes=[("zt", zb)])

            def B_stats(i):
                zb = i % 2
                ln_stats("a", zts[zb], ("zt", zb), stats, mv)

            def B_norm(i):
                zb = i % 2
                tsl = slice(i * 128, (i + 1) * 128)
                ln_apply("a", zts[zb], ("zt", zb), ln1[:, 0, :], ln1[:, 1, :], y32s[zb][:], ("y32", zb), mv, m2,
                         tkeys=[("m2", 0), ("m2", 1)])
                sc.add("sp", lambda e: e.dma_start(out=y32_d[tsl, :], in_=y32s[zb][:]), reads=[("y32", zb)], dma=True)
                sc.add("act", lambda e: e.copy(out=ybf[zb][:], in_=y32s[zb][:]), reads=[("y32", zb)], writes=[("ybf", zb)])

            def C_routerT(i):
                yb = i % 2
                for hf in range(2):
                    for c4 in range(4):
                        c = hf * 4 + c4
                        sc.add("pe", lambda e, hf=hf, c=c, c4=c4: e.transpose(
                            out=CT[hf][:, c4 * 128:(c4 + 1) * 128], in_=y32s[yb][:, c * 128:(c + 1) * 128], identity=ident[:]),
                            reads=[("y32", yb)], writes=[("CT", hf)])
                    fa = lambda e, hf=hf: e.copy(out=yT[:, hf * 4:(hf + 1) * 4, :], in_=CT[hf][:].rearrange("p (c t) -> p c t", c=4))
                    fd = lambda e, hf=hf: e.tensor_copy(out=yT[:, hf * 4:(hf + 1) * 4, :], in_=CT[hf][:].rearrange("p (c t) -> p c t", c=4))
                    sc.add("act" if hf == 0 else "dve", fa if hf == 0 else fd, reads=[("CT", hf)], writes=[("yT", hf)])

            def C_logits(i):
                for dc in range(8):
                    sc.add("pe", lambda e, dc=dc: e.matmul(out=CL[:, 0:32], lhsT=yT[:, dc, :], rhs=rw[:, dc, :],
                                                           start=(dc == 0), stop=(dc == 7)),
                           reads=[("yT", 0), ("yT", 1)], writes=["CL"])
                sc.add("dve", lambda e: e.tensor_tensor(out=lg[:], in0=CL[:, 0:32], in1=rb[:], op=ALU.add),
                       reads=["CL"], writes=["lg"])
                sc.add("dve", lambda e: e.max(out=top8[:], in_=lg[:]), reads=["lg"], writes=["top8"])
                sc.add("dve", lambda e: e.max_index(out=idx8[:], in_max=top8[:], in_values=lg[:]),
                       reads=["lg", "top8"], writes=["idx8"])
                sc.add("dve", lambda e: e.tensor_copy(out=idxf[:], in_=idx8[:]), reads=["idx8"], writes=["idxf"])
                sc.add("dve", lambda e: e.tensor_scalar(out=negmax[:], in0=top8[:, 0:1], scalar1=-1.0, scalar2=None, op0=ALU.mult),
                       reads=["top8"], writes=["negmax"])
                sc.add("act", lambda e: e.activation(out=e4[:], in_=top8[:, 0:4], func=AF.Exp, bias=negmax[:, 0:1], scale=1.0,
                                                     accum_out=s4[:, 0:1]),
                       reads=["top8", "negmax"], writes=["e4", "s4"])
                sc.add("dve", lambda e: e.tensor_scalar(out=mask[:], in0=lg[:], scalar1=top8[:, 3:4], scalar2=None, op0=ALU.is_ge),
                       reads=["lg", "top8"], writes=["mask"])

            def C_pos(i):
                b = i % 2
                sc.add("pe", lambda e: e.matmul(out=CL[:, 32:64], lhsT=utri, rhs=mask[:], start=True, stop=True),
                       reads=["mask"], writes=["CL"])
                sc.add("pe", lambda e: e.matmul(out=CL[:, 64:96], lhsT=ones_f, rhs=mask[:], start=True, stop=True),
                       reads=["mask"], writes=["CL"])
                sc.add("dve", lambda e: e.reciprocal(out=s4[:, 1:2], in_=s4[:, 0:1]), reads=["s4"], writes=["s4"])
                sc.add("dve", lambda e: e.tensor_tensor(out=slotf[:], in0=CL[:, 32:64], in1=CNT[:], op=ALU.add),
                       reads=["CL", "CNT"], writes=["slotf"])
                sc.add("dve", lambda e: e.tensor_tensor(out=CNT[:], in0=CL[:, 64:96], in1=CNT[:], op=ALU.add),
                       reads=["CL", "CNT", "slotf"], writes=["CNT"])
                sc.add("dve", lambda e: e.tensor_scalar(out=ovf[:], in0=slotf[:], scalar1=float(CAP), scalar2=1.0e6,
                                                       op0=ALU.is_ge, op1=ALU.mult),
                       reads=["slotf"], writes=["ovf"])
                sc.add("dve", lambda e: e.tensor_tensor(out=slotf[:], in0=slotf[:], in1=ebase, op=ALU.add),
                       reads=["slotf"], writes=["slotf"])
                sc.add("dve", lambda e: e.tensor_tensor(out=slotf[:], in0=slotf[:], in1=ovf[:], op=ALU.add),
                       reads=["slotf", "ovf"], writes=["slotf"])
                for k in range(4):
                    sc.add("dve", lambda e, k=k: e.tensor_scalar(out=oh[:], in0=iota_e, scalar1=idxf[:, k:k + 1], scalar2=None,
                                                                op0=ALU.is_equal),
                           reads=["idxf"], writes=["oh"])
                    sc.add("dve", lambda e, k=k: e.tensor_tensor(out=j32[:], in0=oh[:], in1=slotf[:], op=ALU.mult),
                           reads=["oh", "slotf"], writes=["j32"])
                    sc.add("dve", lambda e, k=k: e.reduce_sum(out=slotk[:, k:k + 1], in_=j32[:], axis=AX.X),
                           reads=["j32"], writes=["slotk"])
                sc.add("dve", lambda e: e.tensor_scalar(out=okk[:], in0=slotk[:], scalar1=float(NSLOT), scalar2=None, op0=ALU.is_lt),
                       reads=["slotk"], writes=["okk"])
                sc.add("dve", lambda e: e.tensor_scalar(out=e4[:], in0=e4[:], scalar1=s4[:, 1:2], scalar2=None, op0=ALU.mult),
                       reads=["e4", "s4"], writes=["e4"])
                sc.add("dve", lambda e: e.tensor_tensor(out=gate_all[:, i * 4:(i + 1) * 4], in0=e4[:], in1=okk[:], op=ALU.mult),
                       reads=["e4", "okk"], writes=[("gate", i)])
                sc.add("dve", lambda e: e.tensor_copy(out=idx_all[:, i * 4:(i + 1) * 4], in_=slotk[:]),
                       reads=["slotk"], writes=[("idx", i)])
                for k in range(4):
                    sc.add("pool", lambda e, k=k: e.indirect_dma_start(
                        out=xs_d[:, :], out_offset=bass.IndirectOffsetOnAxis(ap=idx_all[:, i * 4 + k:i * 4 + k + 1], axis=0),
                        in_=ybf[b][:, :], in_offset=None, bounds_check=bcreg(e), oob_is_err=False),
                        reads=[("idx", i), ("ybf", b)], dma=True)

            for s_ in range(NT + 2):
                a, bt, ct = s_, s_ - 1, s_ - 2
                hasA, hasB, hasC = a < NT, 0 <= bt < NT, 0 <= ct < NT
                if hasA:
                    A_loads(a)
                if hasB:
                    B_stats(bt)
                if hasA:
                    A_gates(a, 0)
                    A_proj(a, 0)
                if hasC:
                    C_routerT(ct)
                if hasB:
                    B_norm(bt)
                if hasA:
                    A_m(a, 0)
                    A_gates(a, 1)
                if hasC:
                    C_logits(ct)
                if hasA:
                    A_proj(a, 1)
                    A_m(a, 1)
                    A_tail(a)
                if hasC:
                    C_pos(ct)
            sc.barrier()
        pX.close()

        if stage == "y":
            sc.emit(es)
            return nc


        NB = CAP // 128
        SG = CAP // 2
        with ExitStack() as p4:
            wgt = [sb("wgt%d" % i, [128, 8, D], BF16, p4) for i in range(2)]
            wup = [sb("wup%d" % i, [128, 8, D], BF16, p4) for i in range(2)]
            wdn = [sb("wdn%d" % i, [128, 8, D], BF16, p4) for i in range(2)]
            bdr = [sb("bdr%d" % i, [1, D], BF16, p4) for i in range(2)]
            bgu = sb("bgu", [128, 512], F32, p4)
            onesr = sb("onesr", [1, 128], BF16, p4)
            NXS = 5
            xs = [sb("xs%d" % i, [128, D], BF16, p4) for i in range(NXS)]
            xsT = [sb("xsT%d" % i, [128, 8, CAP], BF16, p4) for i in range(2)]
            hT = [sb("hT%d" % i, [128, 8, CAP], BF16, p4) for i in range(2)]
            gtt = [sb("gtt%d" % i, [128, SG], F32, p4) for i in range(2)]
            sgm = [sb("sgm%d" % i, [128, SG], F32, p4) for i in range(2)]
            ubt = [sb("ubt%d" % i, [128, SG], F32, p4) for i in range(2)]
            ysb = [sb("ysb%d" % i, [128, D], BF16, p4) for i in range(2)]
            pT = [ps("pT%d" % i, [128, 1024], BF16, p4) for i in range(2)]
            pG = [ps("pG%d" % i, [128, 512], F32, p4) for i in range(2)]
            pU = [ps("pU%d" % i, [128, 512], F32, p4) for i in range(2)]
            pY = [ps("pY%d" % i, [128, 512], F32, p4) for i in range(2)]
            sc.add("sp", lambda e: e.dma_start(out=bgu[:], in_=bgu_d), writes=["bgu"], dma=True)
            sc.add("dve", lambda e: e.memset(onesr[:], 1.0), writes=["onesr"])
            c4 = {"xs": 0, "xl": 0, "g": 0, "y": 0, "ev": 0}

            wdst = sb("wdst", [128, 8, D], F32, p4)

            def load_expert(e_):
                par = e_ % 2
                for (dst, src, nm) in ((wgt, wgate_d, "wgt"), (wup, wup_d, "wup")):
                    for hf in range(2):
                        sc.add("pool", lambda e, dst=dst, src=src, hf=hf: e.dma_start(
                            out=dst[par][:, :, hf * 512:(hf + 1) * 512],
                            in_=src[e_].rearrange("(dc p) f -> p dc f", p=128)[:, :, hf * 512:(hf + 1) * 512]),
                            writes=[(nm, par)], dma=True)
                sc.add("pool", lambda e: e.dma_start(out=bdr[par][:], in_=bdn_d[e_:e_ + 1, :]), writes=[("bdr", par)], dma=True)
                for hf in range(2):
                    sc.add("sp", lambda e, hf=hf: e.dma_start(
                        out=wdst[:, :, hf * 512:(hf + 1) * 512],
                        in_=wdown_d[e_].rearrange("(dc p) f -> p dc f", p=128)[:, :, hf * 512:(hf + 1) * 512]),
                        writes=[("wdst", hf)], dma=True)

            def cast_dn(e_):
                par = e_ % 2
                for dc in range(8):
                    sc.add("act", lambda e, dc=dc: e.copy(out=wdn[par][:, dc, :], in_=wdst[:, dc, :]),
                           reads=[("wdst", 0), ("wdst", 1)], writes=[("wdn", par)])

            n_exp = NE if stage != "moe1" else 2
            load_expert(0)
            cast_dn(0)
            def load_xs(e_):
                for blk in range(NB):
                    xb = c4["xl"] % NXS
                    c4["xl"] += 1
                    r0 = e_ * CAP + blk * 128
                    sc.add("sp", lambda e, xb=xb, r0=r0: e.dma_start(out=xs[xb][:], in_=xs_d[r0:r0 + 128, :]),
                           writes=[("xs", xb)], dma=True)

            def stage_T(e_):
                par = e_ % 2
                for blk in range(NB):
                    xb = c4["xs"] % NXS
                    tb = c4["xs"] % 2
                    c4["xs"] += 1
                    for dc in range(8):
                        sc.add("pe", lambda e, xb=xb, tb=tb, dc=dc: e.transpose(
                            out=pT[tb][:, dc * 128:(dc + 1) * 128], in_=xs[xb][:, dc * 128:(dc + 1) * 128], identity=identb[:]),
                            reads=[("xs", xb)], writes=[("pT", tb)])
                    sc.add("act", lambda e, blk=blk, tb=tb: e.copy(out=xsT[par][:, :, blk * 128:(blk + 1) * 128],
                                                                 in_=pT[tb][:].rearrange("p (c t) -> p c t", c=8)),
                           reads=[("pT", tb)], writes=[("xsT", par, blk)])

            def stage_GU(e_):
                par = e_ % 2
                xkeys = [("xsT", par, blk) for blk in range(NB)]
                groups = [(sgi, fc) for sgi in range(2) for fc in range(8)]

                def A(n):
                    sgi, fc = groups[n]
                    ss = slice(sgi * SG, (sgi + 1) * SG)
                    gb = n % 2
                    for (pp_, w_, nm, wn) in ((pG, wgt, "pG", "wgt"), (pU, wup, "pU", "wup")):
                        for dc in range(8):
                            sc.add("pe", lambda e, pp_=pp_, w_=w_, dc=dc: e.matmul(
                                out=pp_[gb][:, 0:SG], lhsT=w_[par][:, dc, fc * 128:(fc + 1) * 128], rhs=xsT[par][:, dc, ss],
                                start=(dc == 0), stop=(dc == 7)),
                                reads=xkeys + [(wn, par)], writes=[(nm, gb)])
                    col = e_ * 8 + fc
                    sc.add("dve", lambda e: e.tensor_scalar(
                        out=gtt[gb][:], in0=pG[gb][:, 0:SG], scalar1=bgu[:, col:col + 1], scalar2=7.0,
                        op0=ALU.add, op1=ALU.min), reads=[("pG", gb), "bgu"], writes=[("gtt", gb)])
                    sc.add("act", lambda e: e.activation(out=sgm[gb][:], in_=gtt[gb][:], func=AF.Sigmoid, scale=1.702),
                           reads=[("gtt", gb)], writes=[("sgm", gb)])
                    sc.add("act", lambda e: e.activation(
                        out=ubt[gb][:], in_=pU[gb][:, 0:SG], func=AF.Identity, bias=bgu[:, 256 + col:256 + col + 1], scale=1.0),
                        reads=[("pU", gb), "bgu"], writes=[("ubt", gb)])

                def B(n):
                    sgi, fc = groups[n]
                    ss = slice(sgi * SG, (sgi + 1) * SG)
                    gb = n % 2
                    sc.add("pool", lambda e: e.tensor_scalar(out=ubt[gb][:], in0=ubt[gb][:], scalar1=7.0, scalar2=-7.0,
                                                            op0=ALU.min, op1=ALU.max),
                           reads=[("ubt", gb)], writes=[("ubt", gb)])
                    sc.add("pool", lambda e: e.tensor_tensor(out=gtt[gb][:], in0=gtt[gb][:], in1=sgm[gb][:], op=ALU.mult),
                           reads=[("gtt", gb), ("sgm", gb)], writes=[("gtt", gb)])
                    sc.add("dve", lambda e: e.scalar_tensor_tensor(
                        out=hT[par][:, fc, ss], in0=ubt[gb][:], scalar=1.0, in1=gtt[gb][:], op0=ALU.add, op1=ALU.mult),
                        reads=[("ubt", gb), ("gtt", gb)], writes=[("hT", par, sgi, fc)])

                for n in range(len(groups) + 1):
                    if n < len(groups):
                        A(n)
                    if n >= 1:
                        B(n - 1)

            def stage_DN(e_):
                par = e_ % 2
                hkeys = [("hT", par, sgi, fc) for sgi in range(2) for fc in range(8)]
                for blk in range(NB):
                    yb = c4["y"] % 2
                    c4["y"] += 1
                    for hf in range(2):
                        for fc in range(8):
                            sc.add("pe", lambda e, hf=hf, fc=fc, blk=blk: e.matmul(
                                out=pY[hf][:], lhsT=hT[par][:, fc, blk * 128:(blk + 1) * 128],
                                rhs=wdn[par][:, fc, hf * 512:(hf + 1) * 512], start=(fc == 0), stop=False),
                                reads=hkeys + [("wdn", par)], writes=[("pY", hf)])
                        sc.add("pe", lambda e, hf=hf: e.matmul(
                            out=pY[hf][:], lhsT=onesr[0:1, :], rhs=bdr[par][0:1, hf * 512:(hf + 1) * 512], start=False, stop=True),
                            reads=["onesr", ("bdr", par)], writes=[("pY", hf)])
                        fa = lambda e, hf=hf, yb=yb: e.copy(out=ysb[yb][:, hf * 512:(hf + 1) * 512], in_=pY[hf][:])
                        fd = lambda e, hf=hf, yb=yb: e.tensor_copy(out=ysb[yb][:, hf * 512:(hf + 1) * 512], in_=pY[hf][:])
                        sc.add("act" if hf == 0 else "dve", fa if hf == 0 else fd, reads=[("pY", hf)], writes=[("ysb", yb, hf)])
                    r0 = e_ * CAP + blk * 128
                    sc.add("sp", lambda e, yb=yb, r0=r0: e.dma_start(out=ys_d[r0:r0 + 128, :], in_=ysb[yb][:]),
                           reads=[("ysb", yb, 0), ("ysb", yb, 1)], dma=True)

            load_xs(0)
            stage_T(0)
            for e_ in range(n_exp):
                if e_ + 1 < n_exp and not (NOLOAD and e_ + 1 >= 2):
                    load_expert(e_ + 1)
                if e_ + 1 < n_exp:
                    load_xs(e_ + 1)
                stage_GU(e_)
                if e_ + 1 < n_exp:
                    stage_T(e_ + 1)
                stage_DN(e_)
                if e_ + 1 < n_exp and not (NOLOAD and e_ + 1 >= 2):
                    cast_dn(e_ + 1)
            sc.barrier()

        with ExitStack() as p5:
            ln2 = sb("ln2", [128, 2, 1024], F32, p5)
            G = [[sb("G%d_%d" % (k, i), [128, D], BF16, p5) for i in range(2)] for k in range(4)]
            yr = [sb("yr%d" % i, [128, D], F32, p5) for i in range(2)]
            acc = sb("acc5", [128, D], F32, p5)
            z2 = sb("z2", [128, D], F32, p5)
            tn = sb("tn", [128, D], F32, p5)
            ot = [sb("ot%d" % i, [128, D], F32, p5) for i in range(2)]
            stats2 = sb("stats2", [128, 12], F32, p5)
            mv2 = sb("mv2", [128, 8], F32, p5)
            sc.add("sp", lambda e: e.dma_start(out=ln2[:].rearrange("p a d -> p (a d)"), in_=lnp_d[:, 2048:4096]), writes=["ln2"], dma=True)
            for k in range(4):
                for i in range(2):
                    sc.add("pool", lambda e, k=k, i=i: e.memset(G[k][i][:], 0.0), writes=[("G", k, i)])
            def loads5(i):
                b = i % 2
                tsl = slice(i * 128, (i + 1) * 128)
                sc.add("sp", lambda e: e.dma_start(out=yr[b][:], in_=y32_d[tsl, :]), writes=[("yr", b)], dma=True)
                for k in range(4):
                    sc.add("pool", lambda e, k=k: e.indirect_dma_start(
                        out=G[k][b][:, :], out_offset=None, in_=ys_d[:, :],
                        in_offset=bass.IndirectOffsetOnAxis(ap=idx_all[:, i * 4 + k:i * 4 + k + 1], axis=0),
                        bounds_check=bcreg(e), oob_is_err=False),
                        writes=[("G", k, b)], dma=True)

            def combine5(i):
                b = i % 2
                tsl = slice(i * 128, (i + 1) * 128)
                sc.add("act", lambda e: e.activation(out=z2[:], in_=yr[b][:], func=AF.Identity, scale=float(ALPHA)),
                       reads=[("yr", b)], writes=["z2"])
                for k in range(4):
                    sc.add("dve", lambda e, k=k: e.scalar_tensor_tensor(
                        out=z2[:], in0=G[k][b][:], scalar=gate_all[:, i * 4 + k:i * 4 + k + 1], in1=z2[:],
                        op0=ALU.mult, op1=ALU.add),
                        reads=[("G", k, b), "z2"], writes=["z2"])
                layer_norm("b", z2, "z2", ln2[:, 0, :], ln2[:, 1, :], ot[b][:], ("ot", b), stats2, mv2, tn, gain_eng="pool")
                sc.add("sp", lambda e: e.dma_start(out=out[tsl, :], in_=ot[b][:]), reads=[("ot", b)], dma=True)

            loads5(0)
            for i in range(NT):
                if i + 1 < NT:
                    loads5(i + 1)
                combine5(i)

        sc.emit(es)
    return nc


_NC_CACHE = {}


def kernel(**inputs):
    stage = inputs.pop("_stage", os.environ.get("KSTAGE", "full"))
    if stage not in _NC_CACHE:
        _NC_CACHE[stage] = build(stage)
    nc = _NC_CACHE[stage]
    f = lambda k: np.ascontiguousarray(np.asarray(inputs[k], dtype=np.float32)[0])
    x = np.ascontiguousarray(inputs["x"], dtype=np.float32)
    ident, negd = host_consts()
    sinks = f("sinks").reshape(16)
    sinkc = np.zeros((128, 8), dtype=np.float32)
    for p in range(8):
        sinkc[:64, p] = sinks[2 * p]
        sinkc[64:, p] = sinks[2 * p + 1]
    lnp = np.concatenate([np.broadcast_to(f(k)[None, :], (128, D)) for k in ("ln1_g", "ln1_b", "ln2_g", "ln2_b")], axis=1)
    rb = np.broadcast_to(f("router_b")[None, :], (128, 32))
    bgu = np.concatenate([f("b_gate").reshape(32, 8, 128).transpose(2, 0, 1).reshape(128, 256),
                          f("b_up").reshape(32, 8, 128).transpose(2, 0, 1).reshape(128, 256)], axis=1)
    cst = np.zeros((128, 320), dtype=np.float32)
    cst[:, 0:128] = np.triu(np.ones((128, 128), dtype=np.float32), 1)
    cst[:, 128:256] = 1.0
    cst[:, 256:288] = np.arange(32, dtype=np.float32)[None, :]
    cst[:, 288:320] = (np.arange(32, dtype=np.float32) * CAP)[None, :]
    shared = {"ident": ident, "negd": negd, "sinkc": sinkc, "w_in": f("w_in"),
              "w_proj_a": f("w_proj_a"), "w_proj_b": f("w_proj_b"), "w_out": f("w_out"),
              "lnp": np.ascontiguousarray(lnp), "router_w": f("router_w"), "rb": np.ascontiguousarray(rb),
              "cst": cst, "w_gate": f("w_gate"), "w_up": f("w_up"), "w_down": f("w_down"),
              "bgu": np.ascontiguousarray(bgu), "b_down": f("b_down")}
    names = set(_IN_NAMES.get(stage, shared.keys()))
    in_maps = []
    for c in range(NCORES):
        m = {k: v for k, v in shared.items() if k in names}
        m["x"] = x[c]
        in_maps.append(m)
    res = run_bass_kernel_spmd(nc, in_maps, core_ids=list(range(NCORES)))
    if "KSTAGE" in os.environ and stage != "full":
        return np.zeros((NCORES, S, D), dtype=np.float32)
    if stage != "full":
        return res
    return np.stack([np.asarray(r["out"]) for r in res.results], axis=0)


_IN_NAMES = {}
```

```python
import numpy as np
from contextlib import ExitStack
import concourse.bass as bass
import concourse.mybir as mybir
from concourse.bass_utils import run_bass_kernel_spmd

F32 = mybir.dt.float32
BF16 = mybir.dt.bfloat16
I32 = mybir.dt.int32
U32 = mybir.dt.uint32
AF = mybir.ActivationFunctionType
ALU = mybir.AluOpType
AX = mybir.AxisListType

S = 4096
D = 1024
NT = S // 128
NCORES = 8
ALPHA = 2.0 ** 0.25
LN_EPS = 1e-5
NEG = -1.0e30
NE = 32
CAP = 640
NSLOT = NE * CAP
IN_COLS = 5632
C_AQ, C_AK, C_AV, C_BQ, C_BK, C_BV, C_GA, C_GB = 0, 768, 1536, 2304, 3328, 3456, 3584, 4608
SLOPES = [2.0 ** (-8.0 * (h + 1) / 28.0) for h in range(28)]
MASKS = [(127, 1), (128, 1), (128, 4), (128, 16)]
DIL = [1, 1, 4, 16]

import os
ATT = int(os.environ.get("ATT", "9"))
NOLOAD = os.environ.get("KPROBE", "") == "noload"
COMPUTE = ("pe", "act", "dve", "pool")
QUEUES = ("sp", "pool", "act")
KDMA = 16


class Op:
    __slots__ = ("eng", "fn", "dma", "deps", "inc", "sem", "semval", "slotwait", "name", "idx")


class Sched:
    def __init__(self, nc, es):
        self.nc = nc
        self.streams = {e: [] for e in ("pe", "act", "dve", "pool", "sp")}
        self.last_w = {}
        self.readers = {}
        self.csem = {e: es.enter_context(nc.semaphore("s_" + e)) for e in COMPUTE}
        self.dsem = {q: [es.enter_context(nc.semaphore("d_%s%d" % (q, i))) for i in range(KDMA)]
                     for q in QUEUES}
        self.dma_n = {q: 0 for q in QUEUES}
        self.dma_last = {}
        self.last_c = {}

    def add(self, eng, fn, reads=(), writes=(), dma=False, name=None):
        op = Op()
        op.eng, op.fn, op.dma, op.inc, op.name = eng, fn, dma, False, name
        op.sem = None
        op.semval = 0
        op.slotwait = None
        deps = set()
        for r in reads:
            w = self.last_w.get(r)
            if w is not None:
                deps.add(w)
        for k in writes:
            w = self.last_w.get(k)
            if w is not None:
                deps.add(w)
            for rd in self.readers.get(k, ()):
                deps.add(rd)
        if eng == "pe":
            deps = {d for d in deps if not (d.eng == "pe" and not d.dma)}
        latest = {}
        red = set()
        for d in deps:
            if d.dma:
                red.add(d)
            elif d.eng not in latest or latest[d.eng].idx < d.idx:
                latest[d.eng] = d
        red.update(latest.values())
        op.deps = red
        op.idx = len(self.streams[eng])
        for r in reads:
            self.readers.setdefault(r, []).append(op)
        for k in writes:
            self.last_w[k] = op
            self.readers[k] = []
        if dma:
            n = self.dma_n[eng]
            self.dma_n[eng] = n + 1
            op.sem = self.dsem[eng][n % KDMA]
            op.semval = 16 * (n // KDMA + 1)
            if n >= KDMA:
                op.slotwait = (op.sem, 16 * (n // KDMA))
            self.dma_last[(eng, n % KDMA)] = op
        else:
            self.last_c[eng] = op
        self.streams[eng].append(op)
        return op

    def barrier(self):
        snap = set(self.last_c.values()) | set(self.dma_last.values())
        for e in self.streams:
            op = Op()
            op.eng, op.fn, op.dma, op.inc, op.name = e, None, False, False, "barrier"
            op.sem, op.semval, op.slotwait = None, 0, None
            op.deps = set(snap)
            op.idx = len(self.streams[e])
            self.streams[e].append(op)
        self.last_w = {}
        self.readers = {}

    def emit(self, es):
        nc = self.nc
        for st in self.streams.values():
            for op in st:
                for d in op.deps:
                    d.inc = True
        for e, st in self.streams.items():
            cnt = 0
            for op in st:
                if (not op.dma) and op.inc and op.fn is not None:
                    cnt += 1
                    op.sem = self.csem[e]
                    op.semval = cnt
        block = es.enter_context(nc.Block())

        def body(ename):
            def run(e):
                seen = {}
                for op in self.streams[ename]:
                    waits = {}
                    if op.slotwait is not None:
                        waits[id(op.slotwait[0])] = op.slotwait
                    for d in op.deps:
                        if d.sem is None:
                            continue
                        k = id(d.sem)
                        if k not in waits or waits[k][1] < d.semval:
                            waits[k] = (d.sem, d.semval)
                    for k, (s, v) in waits.items():
                        if seen.get(k, 0) < v:
                            e.wait_ge(s, v)
                            seen[k] = v
                    if op.fn is None:
                        continue
                    inst = op.fn(e)
                    if op.dma:
                        inst.then_inc(op.sem, 16)
                    elif op.inc:
                        inst.then_inc(op.sem, 1)
                for (q, i), dop in self.dma_last.items():
                    if q == ename and seen.get(id(dop.sem), 0) < dop.semval:
                        e.wait_ge(dop.sem, dop.semval)
            return run

        block.tensor(body("pe"))
        block.scalar(body("act"))
        block.vector(body("dve"))
        block.gpsimd(body("pool"))
        block.sync(body("sp"))


def host_consts():
    ident = np.eye(128, dtype=np.float32)
    nd = np.zeros((128, 4, 2, 128), dtype=np.float32)
    k = np.arange(128)[:, None]
    q = np.arange(128)[None, :]
    for m, (maxd, scale) in enumerate(MASKS):
        for kb in range(2):
            diff = q - k + (128 if kb == 0 else 0)
            valid = (diff >= 0) & (diff <= maxd)
            nd[:, m, kb, :] = np.where(valid, -8.0 * scale * diff, NEG)
    return ident, nd.reshape(128, 4 * 256)


def build(stage="full"):
    nc = bass.Bass("TRN2", target_bir_lowering=False)
    x = nc.dram_tensor("x", [S, D], F32, kind="ExternalInput").ap()
    w_in = nc.dram_tensor("w_in", [D, IN_COLS], F32, kind="ExternalInput").ap()
    ident_d = nc.dram_tensor("ident", [128, 128], F32, kind="ExternalInput").ap()
    negd_d = nc.dram_tensor("negd", [128, 1024], F32, kind="ExternalInput").ap()
    sinkc_d = nc.dram_tensor("sinkc", [128, 8], F32, kind="ExternalInput").ap()
    out = nc.dram_tensor("out", [S, D], F32, kind="ExternalOutput").ap()
    oT_d = nc.dram_tensor("oT_scratch", [10, 128, S], BF16, kind="ExternalOutput").ap()
    w_in_v = w_in.rearrange("(dc p) c -> p dc c", p=128)
    wpa_d = nc.dram_tensor("w_proj_a", [256, D], F32, kind="ExternalInput").ap()
    wpb_d = nc.dram_tensor("w_proj_b", [D, D], F32, kind="ExternalInput").ap()
    wo_d = nc.dram_tensor("w_out", [D, D], F32, kind="ExternalInput").ap()
    lnp_d = nc.dram_tensor("lnp", [128, 4 * D], F32, kind="ExternalInput").ap()
    rw_d = nc.dram_tensor("router_w", [D, 32], F32, kind="ExternalInput").ap()
    rb_d = nc.dram_tensor("rb", [128, 32], F32, kind="ExternalInput").ap()
    cst_d = nc.dram_tensor("cst", [128, 320], F32, kind="ExternalInput").ap()
    wgate_d = nc.dram_tensor("w_gate", [32, D, D], F32, kind="ExternalInput").ap()
    wup_d = nc.dram_tensor("w_up", [32, D, D], F32, kind="ExternalInput").ap()
    wdown_d = nc.dram_tensor("w_down", [32, D, D], F32, kind="ExternalInput").ap()
    bgu_d = nc.dram_tensor("bgu", [128, 512], F32, kind="ExternalInput").ap()
    bdn_d = nc.dram_tensor("b_down", [32, D], F32, kind="ExternalInput").ap()
    y32_d = nc.dram_tensor("y32_scratch", [S, D], F32, kind="ExternalOutput").ap()
    xs_d = nc.dram_tensor("xs_scratch", [NSLOT, D], BF16, kind="ExternalOutput").ap()
    ys_d = nc.dram_tensor("ys_scratch", [NSLOT, D], BF16, kind="ExternalOutput").ap()
    es = ExitStack()
    with es:
        sc = Sched(nc, es)

        def sb(name, shape, dt, st=es):
            return st.enter_context(nc.sbuf_tensor("sb_" + name, shape, dt))

        def ps(name, shape, dt, st=es):
            return st.enter_context(nc.psum_tensor("ps_" + name, shape, dt))

        ident = sb("ident", [128, 128], F32)
        identb = sb("identb", [128, 128], BF16)
        negd = sb("negd", [128, 4, 256], F32)
        onesb = sb("onesb", [128, 64], BF16)
        esink = sb("esink", [128, 8], F32)
        idx_all = sb("idx_all", [128, NT * 4], I32)
        gate_all = sb("gate_all", [128, NT * 4], F32)
        cst = sb("cst", [128, 320], F32)
        utri, ones_f, iota_e, ebase = cst[:, 0:128], cst[:, 128:256], cst[:, 256:288], cst[:, 288:320]
        zfill = sb("zfill", [128, 2048], BF16)
        sc.add("dve", lambda e: e.memset(zfill[:], 0.0), writes=["zfill"])
        pX = ExitStack()
        xT = sb("xT", [128, 8, S], BF16, pX)

        sc.add("sp", lambda e: e.dma_start(out=ident[:], in_=ident_d), writes=["ident"], dma=True)
        sc.add("sp", lambda e: e.dma_start(out=negd[:].rearrange("p m c -> p (m c)"), in_=negd_d),
               writes=["negd"], dma=True)
        sc.add("sp", lambda e: e.dma_start(out=esink[:], in_=sinkc_d), writes=["esink"], dma=True)
        sc.add("dve", lambda e: e.tensor_copy(out=identb[:], in_=ident[:]), reads=["ident"], writes=["identb"])
        sc.add("dve", lambda e: e.memset(onesb[:], 1.0), writes=["onesb"])
        sc.add("act", lambda e: e.activation(out=esink[:], in_=esink[:], func=AF.Exp),
               reads=["esink"], writes=["esink"])

        with ExitStack() as p0:
            xin = [sb("xin%d" % i, [128, D], F32, p0) for i in range(2)]
            pst = [ps("pst%d" % i, [128, 512], F32, p0) for i in range(4)]
            for i in range(NT):
                b = i % 2
                sc.add("sp", lambda e, i=i, b=b: e.dma_start(out=xin[b][:], in_=x[i * 128:(i + 1) * 128, :]),
                       writes=[("xin", b)], dma=True)
                for h in range(2):
                    pb = (2 * i + h) % 4
                    for c4 in range(4):
                        c = h * 4 + c4
                        sc.add("pe", lambda e, b=b, c=c, c4=c4, pb=pb: e.transpose(
                            out=pst[pb][:, c4 * 128:(c4 + 1) * 128], in_=xin[b][:, c * 128:(c + 1) * 128],
                            identity=ident[:]),
                            reads=[("xin", b), "ident"], writes=[("pst", pb)])
                    if h == 0:
                        fn = lambda e, i=i, h=h, pb=pb: e.copy(
                            out=xT[:, h * 4:(h + 1) * 4, i * 128:(i + 1) * 128],
                            in_=pst[pb][:].rearrange("p (c t) -> p c t", c=4))
                    else:
                        fn = lambda e, i=i, h=h, pb=pb: e.tensor_copy(
                            out=xT[:, h * 4:(h + 1) * 4, i * 128:(i + 1) * 128],
                            in_=pst[pb][:].rearrange("p (c t) -> p c t", c=4))
                    sc.add("act" if h == 0 else "dve", fn, reads=[("pst", pb)], writes=[("xT", i)])
            sc.barrier()

        with ExitStack() as p1:
            QT = [sb("QT%d" % i, [128, S], BF16, p1) for i in range(2)]
            KTd = sb("KTd", [128, S], BF16, p1)
            Vd = sb("Vd", [128, NT, 128], BF16, p1)
            KTs = [sb("KTs%d" % i, [128, S], BF16, p1) for i in range(2)]
            Vs = sb("Vs", [128, NT, 128], BF16, p1)
            accN = sb("accN", [128, S], F32, p1)
            accD = sb("accD", [128, S], F32, p1)
            wq = [sb("wq%d" % i, [128, 8, 128], BF16, p1) for i in range(2)]
            wk = [sb("wk%d" % i, [128, 8, 128], BF16, p1) for i in range(2)]
            wv = [sb("wv%d" % i, [128, 8, 128], BF16, p1) for i in range(2)]
            tmp = [sb("tmp%d" % i, [128, 512], F32, p1) for i in range(3)]
            PT = [sb("PT%d" % i, [128, 512], BF16, p1) for i in range(3)]
            bias2 = [sb("bias2_%d" % i, [128, 2, 256], F32, p1) for i in range(2)]
            stg = [sb("stg%d" % i, [128, 512], BF16, p1) for i in range(2)]
            t1 = [sb("t1_%d" % i, [128, 512], F32, p1) for i in range(2)]
            pSall = ps("pSall", [128, 6, 512], F32, p1)
            pND = [ps("pND%d" % i, [128, 512], F32, p1) for i in range(2)]
            pj = pND
            cnt = {"pj": 0, "ev": 0, "blk": 0, "stg": 0, "zf": 0}

            def load_w(dst, key, c0, ncols=128, d0=0):
                sc.add("pool", lambda e: e.dma_start(out=dst[:, :, d0:d0 + ncols], in_=w_in_v[:, :, c0:c0 + ncols]),
                       writes=[key], dma=True)

            def evac(fn_act, fn_dve, reads, writes):
                k = cnt["ev"]
                cnt["ev"] += 1
                if k % 2 == 0:
                    sc.add("act", fn_act, reads=reads, writes=writes)
                else:
                    sc.add("dve", fn_dve, reads=reads, writes=writes)

            def proj_fm(w, wkey, dst, dkey, r):
                for tg in range(8):
                    b = cnt["pj"] % 2
                    cnt["pj"] += 1
                    for dc in range(8):
                        sc.add("pe", lambda e, b=b, dc=dc, tg=tg: e.matmul(
                            out=pj[b][:], lhsT=w[:, dc, :], rhs=xT[:, dc, tg * 512:(tg + 1) * 512],
                            start=(dc == 0), stop=(dc == 7)),
                            reads=[wkey], writes=[("pND", b)])
                    if r == 1:
                        o_ap = lambda tg=tg: dst[:, tg * 512:(tg + 1) * 512]
                        i_ap = lambda b=b: pj[b][:]
                    else:
                        n = 512 // r
                        o_ap = lambda tg=tg, n=n: dst[:].rearrange("p (r m) -> p r m", r=r)[:, :, tg * n:(tg + 1) * n]
                        i_ap = lambda b=b: pj[b][:].rearrange("p (m r) -> p r m", r=r)
                    evac(lambda e, o_ap=o_ap, i_ap=i_ap: e.copy(out=o_ap(), in_=i_ap()),
                         lambda e, o_ap=o_ap, i_ap=i_ap: e.tensor_copy(out=o_ap(), in_=i_ap()),
                         reads=[("pND", b)], writes=[(dkey, tg)])

            def proj_tm(w, wkey, dst, dkey, r):
                nbs = NT // r
                for b4 in range(NT // 4):
                    b = cnt["pj"] % 2
                    cnt["pj"] += 1
                    for k4 in range(4):
                        qb = b4 * 4 + k4
                        res, j = qb // nbs, qb % nbs
                        t0 = res + r * 128 * j
                        for dc in range(8):
                            sc.add("pe", lambda e, b=b, dc=dc, k4=k4, t0=t0: e.matmul(
                                out=pj[b][:, k4 * 128:(k4 + 1) * 128],
                                lhsT=xT[:, dc, t0:t0 + 127 * r + 1:r], rhs=w[:, dc, :],
                                start=(dc == 0), stop=(dc == 7)),
                                reads=[wkey], writes=[("pND", b)])
                    evac(lambda e, b=b, b4=b4: e.copy(out=dst[:, b4 * 4:(b4 + 1) * 4, :],
                                                     in_=pj[b][:].rearrange("p (k c) -> p k c", k=4)),
                         lambda e, b=b, b4=b4: e.tensor_copy(out=dst[:, b4 * 4:(b4 + 1) * 4, :],
                                                            in_=pj[b][:].rearrange("p (k c) -> p k c", k=4)),
                         reads=[("pND", b)], writes=[(dkey, b4)])

            def attention(g, Qb, qkeys, Kb, kkeys, Vb, vkey, vcol, slopes2, on_done, par):
                r = DIL[g]
                nbs = NT // r
                for hh in range(2):
                    sc.add("dve", lambda e, hh=hh: e.tensor_scalar(
                        out=bias2[par][:, hh, :], in0=negd[:, g, :], scalar1=float(slopes2[hh]), scalar2=None, op0=ALU.mult),
                        writes=[("bias2", par)])

                def scores(qb):
                    j = qb % nbs
                    sb_ = qb % 3
                    kbs = (0, 1) if j > 0 else (1,)
                    c0 = 0 if j > 0 else 128
                    for hh in range(2):
                        for kb in kbs:
                            kblk = qb - 1 + kb
                            sc.add("pe", lambda e, hh=hh, kb=kb, kblk=kblk: e.matmul(
                                out=pSall[:, 2 * sb_ + hh, kb * 128:(kb + 1) * 128],
                                lhsT=Kb[hh * 64:(hh + 1) * 64, kblk * 128:(kblk + 1) * 128],
                                rhs=Qb[hh * 64:(hh + 1) * 64, qb * 128:(qb + 1) * 128],
                                start=True, stop=True),
                                reads=list(qkeys) + list(kkeys), writes=[("pS", sb_, hh)])
                    t3 = lambda ap: ap[:].rearrange("p (h c) -> p h c", h=2)[:, :, c0:256]
                    sc.add("dve", lambda e: e.tensor_tensor(
                        out=t3(tmp[sb_]), in0=pSall[:, 2 * sb_:2 * sb_ + 2, c0:256], in1=bias2[par][:, :, c0:256], op=ALU.add),
                        reads=[("pS", sb_, 0), ("pS", sb_, 1), ("bias2", par)], writes=[("tmp", sb_)])
                    sc.add("act", lambda e: e.activation(out=t3(PT[sb_]), in_=t3(tmp[sb_]), func=AF.Exp, scale=0.125),
                           reads=[("tmp", sb_)], writes=[("PT", sb_)])

                def pv(qb):
                    j = qb % nbs
                    sb_ = qb % 3
                    kbs = (0, 1) if j > 0 else (1,)
                    b2i, k2 = qb // 2, qb % 2
                    pb = b2i % 2
                    for hh in range(2):
                        for (col0, isnum) in ((k2 * 128, True), (256 + k2 * 128, False)):
                            for ki, kb in enumerate(kbs):
                                kblk = qb - 1 + kb
                                lh = (lambda kblk=kblk, hh=hh: Vb[:, kblk, vcol[hh]:vcol[hh] + 64]) if isnum \
                                    else (lambda: onesb[:, 0:64])
                                rd = [("PT", sb_)] + ([(vkey, kblk // 4)] if isnum else [])
                                sc.add("pe", lambda e, hh=hh, kb=kb, ki=ki, col0=col0, lh=lh: e.matmul(
                                    out=pND[pb][hh * 64:(hh + 1) * 64, col0:col0 + 128],
                                    lhsT=lh(),
                                    rhs=PT[sb_][:, (hh * 2 + kb) * 128:(hh * 2 + kb + 1) * 128],
                                    start=(ki == 0), stop=(ki == len(kbs) - 1)),
                                    reads=rd, writes=[("pND", pb)])
                    if k2 == 1:
                        on_done(b2i, pb)

                for qb in range(NT + 2):
                    if qb < NT:
                        scores(qb)
                    if qb >= 2:
                        pv(qb - 2)

            ZCH = (NSLOT // 128) * D // 2048
            xs_flat = xs_d.rearrange("(p r) d -> p (r d)", p=128)

            def store_stage(chunk, t0, sbuf_i):
                sc.add("sp", lambda e: e.dma_start(out=oT_d[chunk, :, t0:t0 + 512], in_=stg[sbuf_i][:]),
                       reads=[("stg", sbuf_i)], dma=True)
                zi = cnt["zf"]
                if zi < ZCH:
                    cnt["zf"] += 1
                    sc.add("sp", lambda e: e.dma_start(out=xs_flat[:, zi * 2048:(zi + 1) * 2048], in_=zfill[:]),
                           reads=["zfill"], dma=True)

            for kv in range(2):
                load_w(wk[kv], ("wk", kv), C_BK + kv * 64, 64, 0)
                load_w(wk[kv], ("wk", kv), C_BK + kv * 64, 64, 64)
            load_w(wv[0], ("wv", 0), C_BV, 128)
            for kv in range(2):
                proj_fm(wk[kv], ("wk", kv), KTs[kv], ("KTs", kv), 1)
            proj_tm(wv[0], ("wv", 0), Vs, "Vs", 1)

            if stage == "p1a":
                dbg = sb("dbg", [128, S], F32, p1)
                for ci, (src, keys) in enumerate([(KTs[0][:], [(("KTs", 0), tg) for tg in range(8)]),
                                                  (KTs[1][:], [(("KTs", 1), tg) for tg in range(8)]),
                                                  (Vs[:].rearrange("p b c -> p (b c)"), [("Vs", b4) for b4 in range(8)])]):
                    sc.add("dve", lambda e, src=src: e.tensor_copy(out=dbg[:], in_=src), reads=keys, writes=["dbg"])
                    sc.add("sp", lambda e, ci=ci: e.dma_start(
                        out=out.rearrange("(r q) d -> r (q d)", q=4)[ci * 128:(ci + 1) * 128, :], in_=dbg[:]),
                        reads=["dbg"], dma=True)
            jobs = [(0, p) for p in range(8)] + [(g, pp) for pp in range(2) for g in (1, 2, 3)]
            if stage == "p1a":
                jobs = []
            if stage == "swa0":
                jobs = [(0, 0), (0, 5)]
            if stage == "dil0":
                jobs = [(1, 0), (2, 0), (3, 0)]
            for ji, (g, p) in enumerate(jobs):
                par = ji % 2
                r = DIL[g]
                if g == 0:
                    load_w(wq[par], ("wq", par), C_BQ + p * 128)
                    proj_fm(wq[par], ("wq", par), QT[par], ("Q", par), 1)
                    kv = p // 4
                    sl = (SLOPES[2 * p], SLOPES[2 * p + 1])

                    def done(b2i, pb, p=p):
                        b4, half = b2i // 2, b2i % 2
                        si = b4 % 2
                        hs = slice(half * 256, (half + 1) * 256)
                        sc.add("dve", lambda e: e.tensor_scalar(
                            out=t1[si][:, hs], in0=pND[pb][:, 256:512], scalar1=esink[:, p:p + 1], scalar2=None, op0=ALU.add),
                            reads=[("pND", pb)], writes=[("t1", si, half)])
                        sc.add("dve", lambda e: e.reciprocal(out=t1[si][:, hs], in_=t1[si][:, hs]),
                               reads=[("t1", si, half)], writes=[("t1", si, half)])
                        sc.add("dve", lambda e: e.tensor_tensor(
                            out=stg[si][:, hs], in0=pND[pb][:, 0:256], in1=t1[si][:, hs], op=ALU.mult),
                            reads=[("pND", pb), ("t1", si, half)], writes=[("stg", si)])
                        if half == 1:
                            store_stage(p, b4 * 512, si)

                    attention(0, QT[par], [(("Q", par), tg) for tg in range(8)],
                              KTs[kv], [(("KTs", kv), tg) for tg in range(8)],
                              Vs, "Vs", (kv * 64, kv * 64), sl, done, par)
                else:
                    c0 = (g - 1) * 256 + p * 128
                    load_w(wq[par], ("wq", par), C_AQ + c0)
                    load_w(wk[par], ("wk", par), C_AK + c0)
                    load_w(wv[par], ("wv", par), C_AV + c0)
                    proj_fm(wq[par], ("wq", par), QT[par], ("Q", par), r)
                    proj_fm(wk[par], ("wk", par), KTd, "KTd", r)
                    proj_tm(wv[par], ("wv", par), Vd, "Vd", r)
                    sl = (SLOPES[16 + (g - 1) * 4 + 2 * p], SLOPES[16 + (g - 1) * 4 + 2 * p + 1])
                    nbs = NT // r

                    def done(b2i, pb, g=g, r=r, nbs=nbs):
                        qb0 = b2i * 2
                        res, j0 = qb0 // nbs, qb0 % nbs
                        n, t0 = 256, res + r * 128 * j0
                        halves = sorted({(t0 + r * i) // 2048 for i in (0, n - 1)})
                        keys = [("acc", h) for h in halves]
                        for acc, pc in ((accN, 0), (accD, 256)):
                            if g == 1:
                                sc.add("act", lambda e, acc=acc, pc=pc: e.copy(
                                    out=acc[:, t0:t0 + (n - 1) * r + 1:r], in_=pND[pb][:, pc:pc + n]),
                                    reads=[("pND", pb)], writes=keys)
                            else:
                                sc.add("dve", lambda e, acc=acc, pc=pc: e.tensor_tensor(
                                    out=acc[:, t0:t0 + (n - 1) * r + 1:r], in0=pND[pb][:, pc:pc + n],
                                    in1=acc[:, t0:t0 + (n - 1) * r + 1:r], op=ALU.add),
                                    reads=[("pND", pb)] + keys, writes=keys)

                    attention(g, QT[par], [(("Q", par), tg) for tg in range(8)],
                              KTd, [("KTd", tg) for tg in range(8)],
                              Vd, "Vd", (0, 64), sl, done, par)
                    if g == 3:
                        for t8 in range(8):
                            si = t8 % 2
                            keys = [("acc", t8 // 4)]
                            sc.add("dve", lambda e, t8=t8, si=si: e.reciprocal(
                                out=t1[si][:], in_=accD[:, t8 * 512:(t8 + 1) * 512]),
                                reads=keys, writes=[("t1", si, 0), ("t1", si, 1)])
                            sc.add("dve", lambda e, t8=t8, si=si: e.tensor_tensor(
                                out=stg[si][:], in0=accN[:, t8 * 512:(t8 + 1) * 512], in1=t1[si][:], op=ALU.mult),
                                reads=keys + [("t1", si, 0), ("t1", si, 1)], writes=[("stg", si)])
                            store_stage(8 + p, t8 * 512, si)
            sc.barrier()


        if stage == "p01":
            pX.close()
            sc.emit(es)
            return nc
        if stage in ("swa0", "dil0", "attn"):
            with ExitStack() as pd:
                dbgb = sb("dbgb", [128, S], BF16, pd)
                dbg = sb("dbg", [128, S], F32, pd)
                chunks = {"swa0": [0, 5], "dil0": [8], "attn": list(range(8))}[stage]
                for ci, c in enumerate(chunks):
                    sc.add("sp", lambda e, c=c: e.dma_start(out=dbgb[:], in_=oT_d[c, :, :]), writes=["dbgb"], dma=True)
                    sc.add("dve", lambda e: e.tensor_copy(out=dbg[:], in_=dbgb[:]), reads=["dbgb"], writes=["dbg"])
                    sc.add("sp", lambda e, ci=ci: e.dma_start(
                        out=out.rearrange("(r q) d -> r (q d)", q=4)[ci * 128:(ci + 1) * 128, :], in_=dbg[:]),
                        reads=["dbg"], dma=True)
            sc.emit(es)
            return nc

        _regs = {}

        def bcreg(e):
            if "bc" not in _regs:
                r = e.alloc_register("bc")
                e.reg_mov(r, NSLOT - 1)
                _regs["bc"] = r
            return _regs["bc"]

        def ln_stats(p, zt, zkey, stats, mv):
            for h in range(2):
                sc.add("dve", lambda e, h=h: e.bn_stats(out=stats[:, h * 6:(h + 1) * 6], in_=zt[:, h * 512:(h + 1) * 512]),
                       reads=[zkey], writes=[("stats", p)])
            sc.add("dve", lambda e: e.bn_aggr(out=mv[:, 0:2], in_=stats[:, 0:12]), reads=[("stats", p)], writes=[("mv", p)])
            sc.add("dve", lambda e: e.tensor_scalar(out=mv[:, 2:3], in0=mv[:, 1:2], scalar1=LN_EPS, scalar2=None, op0=ALU.add),
                   reads=[("mv", p)], writes=[("mv", p)])
            sc.add("act", lambda e: e.activation(out=mv[:, 2:3], in_=mv[:, 2:3], func=AF.Sqrt),
                   reads=[("mv", p)], writes=[("mv", p)])
            sc.add("dve", lambda e: e.reciprocal(out=mv[:, 3:4], in_=mv[:, 2:3]), reads=[("mv", p)], writes=[("mv", p)])
            sc.add("dve", lambda e: e.scalar_tensor_tensor(out=mv[:, 4:5], in0=mv[:, 0:1], scalar=-1.0, in1=mv[:, 3:4],
                                                          op0=ALU.mult, op1=ALU.mult),
                   reads=[("mv", p)], writes=[("mv", p)])

        def ln_apply(p, zt, zkey, gam, bet, dst, dkey, mv, tmpn, tkeys=None):
            tk = list(tkeys) if tkeys is not None else [("tmpn", p)]
            sc.add("act", lambda e: e.activation(out=tmpn[:], in_=zt[:], func=AF.Identity, scale=mv[:, 3:4], bias=mv[:, 4:5]),
                   reads=[zkey, ("mv", p)], writes=tk)
            sc.add("dve", lambda e: e.tensor_tensor(out=tmpn[:], in0=tmpn[:], in1=gam, op=ALU.mult),
                   reads=tk, writes=tk)
            sc.add("pool", lambda e: e.tensor_tensor(out=dst, in0=tmpn[:], in1=bet, op=ALU.add),
                   reads=tk, writes=[dkey])

        def layer_norm(p, zt, zkey, gam, bet, dst, dkey, stats, mv, tmpn):
            ln_stats(p, zt, zkey, stats, mv)
            ln_apply(p, zt, zkey, gam, bet, dst, dkey, mv, tmpn)


        with ExitStack() as p2:
            wg = sb("wg", [128, 8, 2048], BF16, p2)
            wpa = sb("wpa", [128, 2, 1024], BF16, p2)
            wpb = sb("wpb", [128, 8, 1024], BF16, p2)
            wo = sb("wo", [128, 8, 1024], BF16, p2)
            rw = sb("rw", [128, 8, 32], F32, p2)
            rb = sb("rb", [128, 32], F32, p2)
            ln1 = sb("ln1", [128, 2, 1024], F32, p2)
            oTt = [sb("oTt%d" % i, [128, 10, 128], BF16, p2) for i in range(2)]
            xres = sb("xres", [128, D], F32, p2)
            sg = sb("sg", [128, 2048], F32, p2)
            m1 = sb("m1", [128, D], F32, p2)
            m2 = sb("m2", [128, D], F32, p2)
            mT = sb("mT", [128, 8, 128], BF16, p2)
            zt = sb("zt", [128, D], F32, p2)
            y32 = sb("y32", [128, D], F32, p2)
            ybf = [sb("ybf%d" % i, [128, D], BF16, p2) for i in range(2)]
            yT = sb("yT", [128, 8, 128], F32, p2)
            stats = sb("stats", [128, 12], F32, p2)
            mv = sb("mv", [128, 8], F32, p2)
            lg = sb("lg", [128, 32], F32, p2)
            top8 = sb("top8", [128, 8], F32, p2)
            idx8 = sb("idx8", [128, 8], U32, p2)
            idxf = sb("idxf", [128, 8], F32, p2)
            negmax = sb("negmax", [128, 1], F32, p2)
            e4 = sb("e4", [128, 4], F32, p2)
            s4 = sb("s4", [128, 2], F32, p2)
            mask = sb("mask", [128, 32], F32, p2)
            slotf = sb("slotf", [128, 32], F32, p2)
            ovf = sb("ovf", [128, 32], F32, p2)
            CNT = sb("CNT", [128, 32], F32, p2)
            oh = sb("oh", [128, 32], F32, p2)
            j32 = sb("j32", [128, 32], F32, p2)
            slotk = sb("slotk", [128, 4], F32, p2)
            okk = sb("okk", [128, 4], F32, p2)
            B = [ps("B%d" % i, [128, 512], F32, p2) for i in range(8)]

            sc.add("sp", lambda e: e.dma_start(out=cst[:], in_=cst_d), writes=["cst"], dma=True)
            sc.add("sp", lambda e: e.dma_start(out=rw[:], in_=rw_d.rearrange("(dc p) e -> p dc e", p=128)), writes=["rw"], dma=True)
            sc.add("sp", lambda e: e.dma_start(out=rb[:], in_=rb_d), writes=["rb"], dma=True)
            sc.add("sp", lambda e: e.dma_start(out=ln1[:].rearrange("p a d -> p (a d)"), in_=lnp_d[:, 0:2048]), writes=["ln1"], dma=True)
            for n4 in range(4):
                sc.add("pool", lambda e, n4=n4: e.dma_start(out=wg[:, :, n4 * 512:(n4 + 1) * 512],
                                                            in_=w_in_v[:, :, C_GA + n4 * 512:C_GA + (n4 + 1) * 512]),
                       writes=["wg"], dma=True)
            sc.add("pool", lambda e: e.dma_start(out=wpa[:], in_=wpa_d.rearrange("(c p) d -> p c d", p=128)), writes=["wpa"], dma=True)
            for hf in range(2):
                sc.add("pool", lambda e, hf=hf: e.dma_start(out=wpb[:, :, hf * 512:(hf + 1) * 512],
                                                            in_=wpb_d.rearrange("(c p) d -> p c d", p=128)[:, :, hf * 512:(hf + 1) * 512]),
                       writes=["wpb"], dma=True)
                sc.add("pool", lambda e, hf=hf: e.dma_start(out=wo[:, :, hf * 512:(hf + 1) * 512],
                                                            in_=wo_d.rearrange("(c p) d -> p c d", p=128)[:, :, hf * 512:(hf + 1) * 512]),
                       writes=["wo"], dma=True)
            sc.add("dve", lambda e: e.memset(CNT[:], 0.0), writes=["CNT"])
            sc.barrier()

            zts = [zt, sb("zt1", [128, D], F32, p2)]
            y32s = [y32, sb("y32b", [128, D], F32, p2)]
            Fb = B[0:5]
            CT = B[5:7]
            CL = B[7]

            def A_loads(i):
                b = i % 2
                tsl = slice(i * 128, (i + 1) * 128)
                sc.add("sp", lambda e: e.dma_start(out=oTt[b][:], in_=oT_d[:, :, tsl].rearrange("c p t -> p c t")),
                       writes=[("oTt", b)], dma=True)
                sc.add("sp", lambda e: e.dma_start(out=xres[:], in_=x[tsl, :]), writes=["xres"], dma=True)

            def A_gates(i, half):
                tsl = slice(i * 128, (i + 1) * 128)
                for n2 in range(2):
                    n4 = half * 2 + n2
                    for dc in range(8):
                        sc.add("pe", lambda e, n2=n2, n4=n4, dc=dc: e.matmul(
                            out=Fb[n2][:], lhsT=xT[:, dc, tsl], rhs=wg[:, dc, n4 * 512:(n4 + 1) * 512],
                            start=(dc == 0), stop=(dc == 7)), writes=[("F", n2)])
                    sc.add("act", lambda e, n2=n2, n4=n4: e.activation(out=sg[:, n4 * 512:(n4 + 1) * 512], in_=Fb[n2][:],
                                                                        func=AF.Sigmoid),
                           reads=[("F", n2)], writes=[("sg", n4)])

            def A_proj(i, which):
                b = i % 2
                nck, w_, c0 = (2, wpa, 8) if which == 0 else (8, wpb, 0)
                for hf in range(2):
                    for c in range(nck):
                        sc.add("pe", lambda e, hf=hf, c=c: e.matmul(
                            out=Fb[2 + hf][:], lhsT=oTt[b][:, c0 + c, :], rhs=w_[:, c, hf * 512:(hf + 1) * 512],
                            start=(c == 0), stop=(c == nck - 1)), reads=[("oTt", b)], writes=[("F", 2 + hf)])

            def A_m(i, which):
                for hf in range(2):
                    hs = slice(hf * 512, (hf + 1) * 512)
                    if which == 0:
                        sc.add("dve", lambda e, hf=hf, hs=hs: e.tensor_tensor(
                            out=m1[:, hs], in0=Fb[2 + hf][:], in1=sg[:, hs], op=ALU.mult),
                            reads=[("F", 2 + hf), ("sg", hf)], writes=[("m1", hf)])
                    else:
                        sc.add("dve", lambda e, hf=hf, hs=hs: e.tensor_tensor(
                            out=m2[:, hs], in0=Fb[2 + hf][:], in1=sg[:, 1024 + hf * 512:1024 + (hf + 1) * 512], op=ALU.mult),
                            reads=[("F", 2 + hf), ("sg", 2 + hf)], writes=[("m2", hf)])
                        sc.add("pool", lambda e, hs=hs: e.tensor_tensor(out=m1[:, hs], in0=m1[:, hs], in1=m2[:, hs], op=ALU.add),
                               reads=[("m1", hf), ("m2", hf)], writes=[("m1", hf)])

            def A_tail(i):
                zb = i % 2
                tb = (4, 0)
                for hf in range(2):
                    for c4 in range(4):
                        c = hf * 4 + c4
                        sc.add("pe", lambda e, hf=hf, c=c, c4=c4: e.transpose(
                            out=Fb[tb[hf]][:, c4 * 128:(c4 + 1) * 128], in_=m1[:, c * 128:(c + 1) * 128], identity=ident[:]),
                            reads=[("m1", hf)], writes=[("F", tb[hf])])
                    fa = lambda e, hf=hf: e.copy(out=mT[:, hf * 4:(hf + 1) * 4, :],
                                                 in_=Fb[tb[hf]][:].rearrange("p (c t) -> p c t", c=4))
                    fd = lambda e, hf=hf: e.tensor_copy(out=mT[:, hf * 4:(hf + 1) * 4, :],
                                                        in_=Fb[tb[hf]][:].rearrange("p (c t) -> p c t", c=4))
                    sc.add("act" if hf == 0 else "dve", fa if hf == 0 else fd, reads=[("F", tb[hf])], writes=[("mT", hf)])
                for hf in range(2):
                    for c in range(8):
                        sc.add("pe", lambda e, hf=hf, c=c: e.matmul(
                            out=Fb[2 + hf][:], lhsT=mT[:, c, :], rhs=wo[:, c, hf * 512:(hf + 1) * 512],
                            start=(c == 0), stop=(c == 7)), reads=[("mT", 0), ("mT", 1)], writes=[("F", 2 + hf)])
                    sc.add("dve", lambda e, hf=hf: e.scalar_tensor_tensor(
                        out=zts[zb][:, hf * 512:(hf + 1) * 512], in0=xres[:, hf * 512:(hf + 1) * 512], scalar=float(ALPHA),
                        in1=Fb[2 + hf][:], op0=ALU.mult, op1=ALU.add),
                        reads=["xres", ("F", 2 + hf)], writes=[("zt", zb)])

            def B_stats(i):
                zb = i % 2
                ln_stats("a", zts[zb], ("zt", zb), stats, mv)

            def B_norm(i):
                zb = i % 2
                tsl = slice(i * 128, (i + 1) * 128)
                ln_apply("a", zts[zb], ("zt", zb), ln1[:, 0, :], ln1[:, 1, :], y32s[zb][:], ("y32", zb), mv, m2,
                         tkeys=[("m2", 0), ("m2", 1)])
                sc.add("sp", lambda e: e.dma_start(out=y32_d[tsl, :], in_=y32s[zb][:]), reads=[("y32", zb)], dma=True)
                sc.add("act", lambda e: e.copy(out=ybf[zb][:], in_=y32s[zb][:]), reads=[("y32", zb)], writes=[("ybf", zb)])

            def C_routerT(i):
                yb = i % 2
                for hf in range(2):
                    for c4 in range(4):
                        c = hf * 4 + c4
                        sc.add("pe", lambda e, hf=hf, c=c, c4=c4: e.transpose(
                            out=CT[hf][:, c4 * 128:(c4 + 1) * 128], in_=y32s[yb][:, c * 128:(c + 1) * 128], identity=ident[:]),
                            reads=[("y32", yb)], writes=[("CT", hf)])
                    fa = lambda e, hf=hf: e.copy(out=yT[:, hf * 4:(hf + 1) * 4, :], in_=CT[hf][:].rearrange("p (c t) -> p c t", c=4))
                    fd = lambda e, hf=hf: e.tensor_copy(out=yT[:, hf * 4:(hf + 1) * 4, :], in_=CT[hf][:].rearrange("p (c t) -> p c t", c=4))
                    sc.add("act" if hf == 0 else "dve", fa if hf == 0 else fd, reads=[("CT", hf)], writes=[("yT", hf)])

            def C_logits(i):
                for dc in range(8):
                    sc.add("pe", lambda e, dc=dc: e.matmul(out=CL[:, 0:32], lhsT=yT[:, dc, :], rhs=rw[:, dc, :],
                                                           start=(dc == 0), stop=(dc == 7)),
                           reads=[("yT", 0), ("yT", 1)], writes=["CL"])
                sc.add("dve", lambda e: e.tensor_tensor(out=lg[:], in0=CL[:, 0:32], in1=rb[:], op=ALU.add),
                       reads=["CL"], writes=["lg"])
                sc.add("dve", lambda e: e.max(out=top8[:], in_=lg[:]), reads=["lg"], writes=["top8"])
                sc.add("dve", lambda e: e.max_index(out=idx8[:], in_max=top8[:], in_values=lg[:]),
                       reads=["lg", "top8"], writes=["idx8"])
                sc.add("dve", lambda e: e.tensor_copy(out=idxf[:], in_=idx8[:]), reads=["idx8"], writes=["idxf"])
                sc.add("dve", lambda e: e.tensor_scalar(out=negmax[:], in0=top8[:, 0:1], scalar1=-1.0, scalar2=None, op0=ALU.mult),
                       reads=["top8"], writes=["negmax"])
                sc.add("act", lambda e: e.activation(out=e4[:], in_=top8[:, 0:4], func=AF.Exp, bias=negmax[:, 0:1], scale=1.0,
                                                     accum_out=s4[:, 0:1]),
                       reads=["top8", "negmax"], writes=["e4", "s4"])
                sc.add("dve", lambda e: e.tensor_scalar(out=mask[:], in0=lg[:], scalar1=top8[:, 3:4], scalar2=None, op0=ALU.is_ge),
                       reads=["lg", "top8"], writes=["mask"])

            def C_pos(i):
                b = i % 2
                sc.add("pe", lambda e: e.matmul(out=CL[:, 32:64], lhsT=utri, rhs=mask[:], start=True, stop=True),
                       reads=["mask"], writes=["CL"])
                sc.add("pe", lambda e: e.matmul(out=CL[:, 64:96], lhsT=ones_f, rhs=mask[:], start=True, stop=True),
                       reads=["mask"], writes=["CL"])
                sc.add("dve", lambda e: e.reciprocal(out=s4[:, 1:2], in_=s4[:, 0:1]), reads=["s4"], writes=["s4"])
                sc.add("dve", lambda e: e.tensor_tensor(out=slotf[:], in0=CL[:, 32:64], in1=CNT[:], op=ALU.add),
                       reads=["CL", "CNT"], writes=["slotf"])
                sc.add("dve", lambda e: e.tensor_tensor(out=CNT[:], in0=CL[:, 64:96], in1=CNT[:], op=ALU.add),
                       reads=["CL", "CNT", "slotf"], writes=["CNT"])
                sc.add("dve", lambda e: e.tensor_scalar(out=ovf[:], in0=slotf[:], scalar1=float(CAP), scalar2=1.0e6,
                                                       op0=ALU.is_ge, op1=ALU.mult),
                       reads=["slotf"], writes=["ovf"])
                sc.add("dve", lambda e: e.tensor_tensor(out=slotf[:], in0=slotf[:], in1=ebase, op=ALU.add),
                       reads=["slotf"], writes=["slotf"])
                sc.add("dve", lambda e: e.tensor_tensor(out=slotf[:], in0=slotf[:], in1=ovf[:], op=ALU.add),
                       reads=["slotf", "ovf"], writes=["slotf"])
                for k in range(4):
                    sc.add("dve", lambda e, k=k: e.tensor_scalar(out=oh[:], in0=iota_e, scalar1=idxf[:, k:k + 1], scalar2=None,
                                                                op0=ALU.is_equal),
                           reads=["idxf"], writes=["oh"])
                    sc.add("dve", lambda e, k=k: e.tensor_tensor(out=j32[:], in0=oh[:], in1=slotf[:], op=ALU.mult),
                           reads=["oh", "slotf"], writes=["j32"])
                    sc.add("dve", lambda e, k=k: e.reduce_sum(out=slotk[:, k:k + 1], in_=j32[:], axis=AX.X),
                           reads=["j32"], writes=["slotk"])
                sc.add("dve", lambda e: e.tensor_scalar(out=okk[:], in0=slotk[:], scalar1=float(NSLOT), scalar2=None, op0=ALU.is_lt),
                       reads=["slotk"], writes=["okk"])
                sc.add("dve", lambda e: e.tensor_scalar(out=e4[:], in0=e4[:], scalar1=s4[:, 1:2], scalar2=None, op0=ALU.mult),
                       reads=["e4", "s4"], writes=["e4"])
                sc.add("dve", lambda e: e.tensor_tensor(out=gate_all[:, i * 4:(i + 1) * 4], in0=e4[:], in1=okk[:], op=ALU.mult),
                       reads=["e4", "okk"], writes=[("gate", i)])
                sc.add("dve", lambda e: e.tensor_copy(out=idx_all[:, i * 4:(i + 1) * 4], in_=slotk[:]),
                       reads=["slotk"], writes=[("idx", i)])
                for k in range(4):
                    sc.add("pool", lambda e, k=k: e.indirect_dma_start(
                        out=xs_d[:, :], out_offset=bass.IndirectOffsetOnAxis(ap=idx_all[:, i * 4 + k:i * 4 + k + 1], axis=0),
                        in_=ybf[b][:, :], in_offset=None, bounds_check=bcreg(e), oob_is_err=False),
                        reads=[("idx", i), ("ybf", b)], dma=True)

            for s_ in range(NT + 2):
                a, bt, ct = s_, s_ - 1, s_ - 2
                hasA, hasB, hasC = a < NT, 0 <= bt < NT, 0 <= ct < NT
                if hasA:
                    A_loads(a)
                if hasB:
                    B_stats(bt)
                if hasA:
                    A_gates(a, 0)
                    A_proj(a, 0)
                if hasC:
                    C_routerT(ct)
                if hasB:
                    B_norm(bt)
                if hasA:
                    A_m(a, 0)
                    A_gates(a, 1)
                if hasC:
                    C_logits(ct)
                if hasA:
                    A_proj(a, 1)
                    A_m(a, 1)
                    A_tail(a)
                if hasC:
                    C_pos(ct)
            sc.barrier()
        pX.close()

        if stage == "y":
            sc.emit(es)
            return nc


        NB = CAP // 128
        SG = CAP // 2
        with ExitStack() as p4:
            wgt = [sb("wgt%d" % i, [128, 8, D], BF16, p4) for i in range(2)]
            wup = [sb("wup%d" % i, [128, 8, D], BF16, p4) for i in range(2)]
            wdn = [sb("wdn%d" % i, [128, 8, D], BF16, p4) for i in range(2)]
            bdr = [sb("bdr%d" % i, [1, D], BF16, p4) for i in range(2)]
            bgu = sb("bgu", [128, 512], F32, p4)
            onesr = sb("onesr", [1, 128], BF16, p4)
            NXS = 5
            xs = [sb("xs%d" % i, [128, D], BF16, p4) for i in range(NXS)]
            xsT = [sb("xsT%d" % i, [128, 8, CAP], BF16, p4) for i in range(2)]
            hT = [sb("hT%d" % i, [128, 8, CAP], BF16, p4) for i in range(2)]
            gtt = [sb("gtt%d" % i, [128, SG], F32, p4) for i in range(2)]
            sgm = [sb("sgm%d" % i, [128, SG], F32, p4) for i in range(2)]
            ubt = [sb("ubt%d" % i, [128, SG], F32, p4) for i in range(2)]
            ysb = [sb("ysb%d" % i, [128, D], BF16, p4) for i in range(2)]
            pT = [ps("pT%d" % i, [128, 1024], BF16, p4) for i in range(2)]
            pG = [ps("pG%d" % i, [128, 512], F32, p4) for i in range(2)]
            pU = [ps("pU%d" % i, [128, 512], F32, p4) for i in range(2)]
            pY = [ps("pY%d" % i, [128, 512], F32, p4) for i in range(2)]
            sc.add("sp", lambda e: e.dma_start(out=bgu[:], in_=bgu_d), writes=["bgu"], dma=True)
            sc.add("dve", lambda e: e.memset(onesr[:], 1.0), writes=["onesr"])
            c4 = {"xs": 0, "xl": 0, "g": 0, "y": 0, "ev": 0}

            wdst = sb("wdst", [128, 8, D], F32, p4)

            def load_expert(e_):
                par = e_ % 2
                for (dst, src, nm) in ((wgt, wgate_d, "wgt"), (wup, wup_d, "wup")):
                    for hf in range(2):
                        sc.add("pool", lambda e, dst=dst, src=src, hf=hf: e.dma_start(
                            out=dst[par][:, :, hf * 512:(hf + 1) * 512],
                            in_=src[e_].rearrange("(dc p) f -> p dc f", p=128)[:, :, hf * 512:(hf + 1) * 512]),
                            writes=[(nm, par)], dma=True)
                sc.add("pool", lambda e: e.dma_start(out=bdr[par][:], in_=bdn_d[e_:e_ + 1, :]), writes=[("bdr", par)], dma=True)
                for hf in range(2):
                    sc.add("sp", lambda e, hf=hf: e.dma_start(
                        out=wdst[:, :, hf * 512:(hf + 1) * 512],
                        in_=wdown_d[e_].rearrange("(dc p) f -> p dc f", p=128)[:, :, hf * 512:(hf + 1) * 512]),
                        writes=[("wdst", hf)], dma=True)

            def cast_dn(e_):
                par = e_ % 2
                for dc in range(8):
                    sc.add("act", lambda e, dc=dc: e.copy(out=wdn[par][:, dc, :], in_=wdst[:, dc, :]),
                           reads=[("wdst", 0), ("wdst", 1)], writes=[("wdn", par)])

            n_exp = NE if stage != "moe1" else 2
            load_expert(0)
            cast_dn(0)
            def load_xs(e_):
                for blk in range(NB):
                    xb = c4["xl"] % NXS
                    c4["xl"] += 1
                    r0 = e_ * CAP + blk * 128
                    sc.add("sp", lambda e, xb=xb, r0=r0: e.dma_start(out=xs[xb][:], in_=xs_d[r0:r0 + 128, :]),
                           writes=[("xs", xb)], dma=True)

            def stage_T(e_):
                par = e_ % 2
                for blk in range(NB):
                    xb = c4["xs"] % NXS
                    tb = c4["xs"] % 2
                    c4["xs"] += 1
                    for dc in range(8):
                        sc.add("pe", lambda e, xb=xb, tb=tb, dc=dc: e.transpose(
                            out=pT[tb][:, dc * 128:(dc + 1) * 128], in_=xs[xb][:, dc * 128:(dc + 1) * 128], identity=identb[:]),
                            reads=[("xs", xb)], writes=[("pT", tb)])
                    sc.add("act", lambda e, blk=blk, tb=tb: e.copy(out=xsT[par][:, :, blk * 128:(blk + 1) * 128],
                                                                 in_=pT[tb][:].rearrange("p (c t) -> p c t", c=8)),
                           reads=[("pT", tb)], writes=[("xsT", par, blk)])

            def stage_GU(e_):
                par = e_ % 2
                xkeys = [("xsT", par, blk) for blk in range(NB)]
                groups = [(sgi, fc) for sgi in range(2) for fc in range(8)]

                def A(n):
                    sgi, fc = groups[n]
                    ss = slice(sgi * SG, (sgi + 1) * SG)
                    gb = n % 2
                    for (pp_, w_, nm, wn) in ((pG, wgt, "pG", "wgt"), (pU, wup, "pU", "wup")):
                        for dc in range(8):
                            sc.add("pe", lambda e, pp_=pp_, w_=w_, dc=dc: e.matmul(
                                out=pp_[gb][:, 0:SG], lhsT=w_[par][:, dc, fc * 128:(fc + 1) * 128], rhs=xsT[par][:, dc, ss],
                                start=(dc == 0), stop=(dc == 7)),
                                reads=xkeys + [(wn, par)], writes=[(nm, gb)])
                    col = e_ * 8 + fc
                    sc.add("dve", lambda e: e.tensor_scalar(
                        out=gtt[gb][:], in0=pG[gb][:, 0:SG], scalar1=bgu[:, col:col + 1], scalar2=7.0,
                        op0=ALU.add, op1=ALU.min), reads=[("pG", gb), "bgu"], writes=[("gtt", gb)])
                    sc.add("act", lambda e: e.activation(out=sgm[gb][:], in_=gtt[gb][:], func=AF.Sigmoid, scale=1.702),
                           reads=[("gtt", gb)], writes=[("sgm", gb)])
                    sc.add("act", lambda e: e.activation(
                        out=ubt[gb][:], in_=pU[gb][:, 0:SG], func=AF.Identity, bias=bgu[:, 256 + col:256 + col + 1], scale=1.0),
                        reads=[("pU", gb), "bgu"], writes=[("ubt", gb)])

                def B(n):
                    sgi, fc = groups[n]
                    ss = slice(sgi * SG, (sgi + 1) * SG)
                    gb = n % 2
                    sc.add("pool", lambda e: e.tensor_scalar(out=ubt[gb][:], in0=ubt[gb][:], scalar1=7.0, scalar2=-7.0,
                                                            op0=ALU.min, op1=ALU.max),
                           reads=[("ubt", gb)], writes=[("ubt", gb)])
                    sc.add("pool", lambda e: e.tensor_tensor(out=gtt[gb][:], in0=gtt[gb][:], in1=sgm[gb][:], op=ALU.mult),
                           reads=[("gtt", gb), ("sgm", gb)], writes=[("gtt", gb)])
                    sc.add("dve", lambda e: e.scalar_tensor_tensor(
                        out=hT[par][:, fc, ss], in0=ubt[gb][:], scalar=1.0, in1=gtt[gb][:], op0=ALU.add, op1=ALU.mult),
                        reads=[("ubt", gb), ("gtt", gb)], writes=[("hT", par, sgi, fc)])

                for n in range(len(groups) + 1):
                    if n < len(groups):
                        A(n)
                    if n >= 1:
                        B(n - 1)

            def stage_DN(e_):
                par = e_ % 2
                hkeys = [("hT", par, sgi, fc) for sgi in range(2) for fc in range(8)]
                for blk in range(NB):
                    yb = c4["y"] % 2
                    c4["y"] += 1
                    for hf in range(2):
                        for fc in range(8):
                            sc.add("pe", lambda e, hf=hf, fc=fc, blk=blk: e.matmul(
                                out=pY[hf][:], lhsT=hT[par][:, fc, blk * 128:(blk + 1) * 128],
                                rhs=wdn[par][:, fc, hf * 512:(hf + 1) * 512], start=(fc == 0), stop=False),
                                reads=hkeys + [("wdn", par)], writes=[("pY", hf)])
                        sc.add("pe", lambda e, hf=hf: e.matmul(
                            out=pY[hf][:], lhsT=onesr[0:1, :], rhs=bdr[par][0:1, hf * 512:(hf + 1) * 512], start=False, stop=True),
                            reads=["onesr", ("bdr", par)], writes=[("pY", hf)])
                        fa = lambda e, hf=hf, yb=yb: e.copy(out=ysb[yb][:, hf * 512:(hf + 1) * 512], in_=pY[hf][:])
                        fd = lambda e, hf=hf, yb=yb: e.tensor_copy(out=ysb[yb][:, hf * 512:(hf + 1) * 512], in_=pY[hf][:])
                        sc.add("act" if hf == 0 else "dve", fa if hf == 0 else fd, reads=[("pY", hf)], writes=[("ysb", yb, hf)])
                    r0 = e_ * CAP + blk * 128
                    sc.add("sp", lambda e, yb=yb, r0=r0: e.dma_start(out=ys_d[r0:r0 + 128, :], in_=ysb[yb][:]),
                           reads=[("ysb", yb, 0), ("ysb", yb, 1)], dma=True)

            load_xs(0)
            stage_T(0)
            for e_ in range(n_exp):
                if e_ + 1 < n_exp and not (NOLOAD and e_ + 1 >= 2):
                    load_expert(e_ + 1)
                if e_ + 1 < n_exp:
                    load_xs(e_ + 1)
                stage_GU(e_)
                if e_ + 1 < n_exp:
                    stage_T(e_ + 1)
                stage_DN(e_)
                if e_ + 1 < n_exp and not (NOLOAD and e_ + 1 >= 2):
                    cast_dn(e_ + 1)
            sc.barrier()

        with ExitStack() as p5:
            ln2 = sb("ln2", [128, 2, 1024], F32, p5)
            G = [[sb("G%d_%d" % (k, i), [128, D], BF16, p5) for i in range(2)] for k in range(4)]
            yr = [sb("yr%d" % i, [128, D], F32, p5) for i in range(2)]
            acc = sb("acc5", [128, D], F32, p5)
            z2 = sb("z2", [128, D], F32, p5)
            tn = sb("tn", [128, D], F32, p5)
            ot = [sb("ot%d" % i, [128, D], F32, p5) for i in range(2)]
            stats2 = sb("stats2", [128, 12], F32, p5)
            mv2 = sb("mv2", [128, 8], F32, p5)
            dg = [sb("dg%d" % i, [128, 4, 128], BF16, p5) for i in range(2)]
            pF = [[ps("pF%d_%d" % (i, h), [128, 512], F32, p5) for h in range(2)] for i in range(2)]
            sc.add("sp", lambda e: e.dma_start(out=ln2[:].rearrange("p a d -> p (a d)"), in_=lnp_d[:, 2048:4096]), writes=["ln2"], dma=True)
            for k in range(4):
                for i in range(2):
                    sc.add("pool", lambda e, k=k, i=i: e.memset(G[k][i][:], 0.0), writes=[("G", k, i)])
            def loads5(i):
                b = i % 2
                tsl = slice(i * 128, (i + 1) * 128)
                sc.add("sp", lambda e: e.dma_start(out=yr[b][:], in_=y32_d[tsl, :]), writes=[("yr", b)], dma=True)
                for k in range(4):
                    sc.add("pool", lambda e, k=k: e.indirect_dma_start(
                        out=G[k][b][:, :], out_offset=None, in_=ys_d[:, :],
                        in_offset=bass.IndirectOffsetOnAxis(ap=idx_all[:, i * 4 + k:i * 4 + k + 1], axis=0),
                        bounds_check=bcreg(e), oob_is_err=False),
                        writes=[("G", k, b)], dma=True)

            def combine5(i):
                b = i % 2
                tsl = slice(i * 128, (i + 1) * 128)
                for k in range(4):
                    sc.add("dve", lambda e, k=k: e.tensor_scalar(
                        out=dg[b][:, k, :], in0=identb[:], scalar1=gate_all[:, i * 4 + k:i * 4 + k + 1], scalar2=None,
                        op0=ALU.mult), writes=[("dg", b)])
                for hf in range(2):
                    for k in range(4):
                        sc.add("pe", lambda e, hf=hf, k=k: e.matmul(
                            out=pF[b][hf][:], lhsT=dg[b][:, k, :], rhs=G[k][b][:, hf * 512:(hf + 1) * 512],
                            start=(k == 0), stop=(k == 3)),
                            reads=[("dg", b), ("G", k, b)], writes=[("pF", b, hf)])
                    sc.add("dve", lambda e, hf=hf: e.scalar_tensor_tensor(
                        out=z2[:, hf * 512:(hf + 1) * 512], in0=yr[b][:, hf * 512:(hf + 1) * 512], scalar=float(ALPHA),
                        in1=pF[b][hf][:], op0=ALU.mult, op1=ALU.add),
                        reads=[("yr", b), ("pF", b, hf)], writes=["z2"])
                layer_norm("b", z2, "z2", ln2[:, 0, :], ln2[:, 1, :], ot[b][:], ("ot", b), stats2, mv2, tn)
                sc.add("sp", lambda e: e.dma_start(out=out[tsl, :], in_=ot[b][:]), reads=[("ot", b)], dma=True)

            loads5(0)
            for i in range(NT):
                if i + 1 < NT:
                    loads5(i + 1)
                combine5(i)

        sc.emit(es)
    return nc


_NC_CACHE = {}


def kernel(**inputs):
    stage = inputs.pop("_stage", os.environ.get("KSTAGE", "full"))
    if stage not in _NC_CACHE:
        _NC_CACHE[stage] = build(stage)
    nc = _NC_CACHE[stage]
    f = lambda k: np.ascontiguousarray(np.asarray(inputs[k], dtype=np.float32)[0])
    x = np.ascontiguousarray(inputs["x"], dtype=np.float32)
    ident, negd = host_consts()
    sinks = f("sinks").reshape(16)
    sinkc = np.zeros((128, 8), dtype=np.float32)
    for p in range(8):
        sinkc[:64, p] = sinks[2 * p]
        sinkc[64:, p] = sinks[2 * p + 1]
    lnp = np.concatenate([np.broadcast_to(f(k)[None, :], (128, D)) for k in ("ln1_g", "ln1_b", "ln2_g", "ln2_b")], axis=1)
    rb = np.broadcast_to(f("router_b")[None, :], (128, 32))
    bgu = np.concatenate([f("b_gate").reshape(32, 8, 128).transpose(2, 0, 1).reshape(128, 256),
                          f("b_up").reshape(32, 8, 128).transpose(2, 0, 1).reshape(128, 256)], axis=1)
    cst = np.zeros((128, 320), dtype=np.float32)
    cst[:, 0:128] = np.triu(np.ones((128, 128), dtype=np.float32), 1)
    cst[:, 128:256] = 1.0
    cst[:, 256:288] = np.arange(32, dtype=np.float32)[None, :]
    cst[:, 288:320] = (np.arange(32, dtype=np.float32) * CAP)[None, :]
    shared = {"ident": ident, "negd": negd, "sinkc": sinkc, "w_in": f("w_in"),
              "w_proj_a": f("w_proj_a"), "w_proj_b": f("w_proj_b"), "w_out": f("w_out"),
              "lnp": np.ascontiguousarray(lnp), "router_w": f("router_w"), "rb": np.ascontiguousarray(rb),
              "cst": cst, "w_gate": f("w_gate"), "w_up": f("w_up"), "w_down": f("w_down"),
              "bgu": np.ascontiguousarray(bgu), "b_down": f("b_down")}
    names = set(_IN_NAMES.get(stage, shared.keys()))
    in_maps = []
    for c in range(NCORES):
        m = {k: v for k, v in shared.items() if k in names}
        m["x"] = x[c]
        in_maps.append(m)
    res = run_bass_kernel_spmd(nc, in_maps, core_ids=list(range(NCORES)))
    if "KSTAGE" in os.environ and stage != "full":
        return np.zeros((NCORES, S, D), dtype=np.float32)
    if stage != "full":
        return res
    return np.stack([np.asarray(r["out"]) for r in res.results], axis=0)


_IN_NAMES = {}
```

```python
import numpy as np
from contextlib import ExitStack
import concourse.bass as bass
import concourse.mybir as mybir
from concourse.bass_utils import run_bass_kernel_spmd

F32 = mybir.dt.float32
BF16 = mybir.dt.bfloat16
I32 = mybir.dt.int32
U32 = mybir.dt.uint32
AF = mybir.ActivationFunctionType
ALU = mybir.AluOpType
AX = mybir.AxisListType

S = 4096
D = 1024
NT = S // 128
NCORES = 8
ALPHA = 2.0 ** 0.25
LN_EPS = 1e-5
NEG = -1.0e30
NE = 32
CAP = 640
NSLOT = NE * CAP
IN_COLS = 5632
C_AQ, C_AK, C_AV, C_BQ, C_BK, C_BV, C_GA, C_GB = 0, 768, 1536, 2304, 3328, 3456, 3584, 4608
SLOPES = [2.0 ** (-8.0 * (h + 1) / 28.0) for h in range(28)]
MASKS = [(127, 1), (128, 1), (128, 4), (128, 16)]
DIL = [1, 1, 4, 16]

import os
ATT = int(os.environ.get("ATT", "9"))
NOLOAD = os.environ.get("KPROBE", "") == "noload"
COMPUTE = ("pe", "act", "dve", "pool")
QUEUES = ("sp", "pool", "act")
KDMA = 16


class Op:
    __slots__ = ("eng", "fn", "dma", "deps", "inc", "sem", "semval", "slotwait", "name", "idx")


class Sched:
    def __init__(self, nc, es):
        self.nc = nc
        self.streams = {e: [] for e in ("pe", "act", "dve", "pool", "sp")}
        self.last_w = {}
        self.readers = {}
        self.csem = {e: es.enter_context(nc.semaphore("s_" + e)) for e in COMPUTE}
        self.dsem = {q: [es.enter_context(nc.semaphore("d_%s%d" % (q, i))) for i in range(KDMA)]
                     for q in QUEUES}
        self.dma_n = {q: 0 for q in QUEUES}
        self.dma_last = {}
        self.last_c = {}

    def add(self, eng, fn, reads=(), writes=(), dma=False, name=None):
        op = Op()
        op.eng, op.fn, op.dma, op.inc, op.name = eng, fn, dma, False, name
        op.sem = None
        op.semval = 0
        op.slotwait = None
        deps = set()
        for r in reads:
            w = self.last_w.get(r)
            if w is not None:
                deps.add(w)
        for k in writes:
            w = self.last_w.get(k)
            if w is not None:
                deps.add(w)
            for rd in self.readers.get(k, ()):
                deps.add(rd)
        if eng == "pe":
            deps = {d for d in deps if not (d.eng == "pe" and not d.dma)}
        latest = {}
        red = set()
        for d in deps:
            if d.dma:
                red.add(d)
            elif d.eng not in latest or latest[d.eng].idx < d.idx:
                latest[d.eng] = d
        red.update(latest.values())
        op.deps = red
        op.idx = len(self.streams[eng])
        for r in reads:
            self.readers.setdefault(r, []).append(op)
        for k in writes:
            self.last_w[k] = op
            self.readers[k] = []
        if dma:
            n = self.dma_n[eng]
            self.dma_n[eng] = n + 1
            op.sem = self.dsem[eng][n % KDMA]
            op.semval = 16 * (n // KDMA + 1)
            if n >= KDMA:
                op.slotwait = (op.sem, 16 * (n // KDMA))
            self.dma_last[(eng, n % KDMA)] = op
        else:
            self.last_c[eng] = op
        self.streams[eng].append(op)
        return op

    def barrier(self):
        snap = set(self.last_c.values()) | set(self.dma_last.values())
        for e in self.streams:
            op = Op()
            op.eng, op.fn, op.dma, op.inc, op.name = e, None, False, False, "barrier"
            op.sem, op.semval, op.slotwait = None, 0, None
            op.deps = set(snap)
            op.idx = len(self.streams[e])
            self.streams[e].append(op)
        self.last_w = {}
        self.readers = {}

    def emit(self, es):
        nc = self.nc
        for st in self.streams.values():
            for op in st:
                for d in op.deps:
                    d.inc = True
        for e, st in self.streams.items():
            cnt = 0
            for op in st:
                if (not op.dma) and op.inc and op.fn is not None:
                    cnt += 1
                    op.sem = self.csem[e]
                    op.semval = cnt
        block = es.enter_context(nc.Block())

        def body(ename):
            def run(e):
                seen = {}
                for op in self.streams[ename]:
                    waits = {}
                    if op.slotwait is not None:
                        waits[id(op.slotwait[0])] = op.slotwait
                    for d in op.deps:
                        if d.sem is None:
                            continue
                        k = id(d.sem)
                        if k not in waits or waits[k][1] < d.semval:
                            waits[k] = (d.sem, d.semval)
                    for k, (s, v) in waits.items():
                        if seen.get(k, 0) < v:
                            e.wait_ge(s, v)
                            seen[k] = v
                    if op.fn is None:
                        continue
                    inst = op.fn(e)
                    if op.dma:
                        inst.then_inc(op.sem, 16)
                    elif op.inc:
                        inst.then_inc(op.sem, 1)
                for (q, i), dop in self.dma_last.items():
                    if q == ename and seen.get(id(dop.sem), 0) < dop.semval:
                        e.wait_ge(dop.sem, dop.semval)
            return run

        block.tensor(body("pe"))
        block.scalar(body("act"))
        block.vector(body("dve"))
        block.gpsimd(body("pool"))
        block.sync(body("sp"))


def host_consts():
    ident = np.eye(128, dtype=np.float32)
    nd = np.zeros((128, 4, 2, 128), dtype=np.float32)
    k = np.arange(128)[:, None]
    q = np.arange(128)[None, :]
    for m, (maxd, scale) in enumerate(MASKS):
        for kb in range(2):
            diff = q - k + (128 if kb == 0 else 0)
            valid = (diff >= 0) & (diff <= maxd)
            nd[:, m, kb, :] = np.where(valid, -8.0 * scale * diff, NEG)
    return ident, nd.reshape(128, 4 * 256)


def build(stage="full"):
    nc = bass.Bass("TRN2", target_bir_lowering=False)
    x = nc.dram_tensor("x", [S, D], F32, kind="ExternalInput").ap()
    w_in = nc.dram_tensor("w_in", [D, IN_COLS], F32, kind="ExternalInput").ap()
    ident_d = nc.dram_tensor("ident", [128, 128], F32, kind="ExternalInput").ap()
    negd_d = nc.dram_tensor("negd", [128, 1024], F32, kind="ExternalInput").ap()
    sinkc_d = nc.dram_tensor("sinkc", [128, 8], F32, kind="ExternalInput").ap()
    out = nc.dram_tensor("out", [S, D], F32, kind="ExternalOutput").ap()
    oT_d = nc.dram_tensor("oT_scratch", [10, 128, S], BF16, kind="ExternalOutput").ap()
    w_in_v = w_in.rearrange("(dc p) c -> p dc c", p=128)
    wpa_d = nc.dram_tensor("w_proj_a", [256, D], F32, kind="ExternalInput").ap()
    wpb_d = nc.dram_tensor("w_proj_b", [D, D], F32, kind="ExternalInput").ap()
    wo_d = nc.dram_tensor("w_out", [D, D], F32, kind="ExternalInput").ap()
    lnp_d = nc.dram_tensor("lnp", [128, 4 * D], F32, kind="ExternalInput").ap()
    rw_d = nc.dram_tensor("router_w", [D, 32], F32, kind="ExternalInput").ap()
    rb_d = nc.dram_tensor("rb", [128, 32], F32, kind="ExternalInput").ap()
    cst_d = nc.dram_tensor("cst", [128, 320], F32, kind="ExternalInput").ap()
    wgate_d = nc.dram_tensor("w_gate", [32, D, D], F32, kind="ExternalInput").ap()
    wup_d = nc.dram_tensor("w_up", [32, D, D], F32, kind="ExternalInput").ap()
    wdown_d = nc.dram_tensor("w_down", [32, D, D], F32, kind="ExternalInput").ap()
    bgu_d = nc.dram_tensor("bgu", [128, 512], F32, kind="ExternalInput").ap()
    bdn_d = nc.dram_tensor("b_down", [32, D], F32, kind="ExternalInput").ap()
    y32_d = nc.dram_tensor("y32_scratch", [S, D], F32, kind="ExternalOutput").ap()
    xs_d = nc.dram_tensor("xs_scratch", [NSLOT, D], BF16, kind="ExternalOutput").ap()
    ys_d = nc.dram_tensor("ys_scratch", [NSLOT, D], BF16, kind="ExternalOutput").ap()
    es = ExitStack()
    with es:
        sc = Sched(nc, es)

        def sb(name, shape, dt, st=es):
            return st.enter_context(nc.sbuf_tensor("sb_" + name, shape, dt))

        def ps(name, shape, dt, st=es):
            return st.enter_context(nc.psum_tensor("ps_" + name, shape, dt))

        ident = sb("ident", [128, 128], F32)
        identb = sb("identb", [128, 128], BF16)
        negd = sb("negd", [128, 4, 256], F32)
        onesb = sb("onesb", [128, 64], BF16)
        esink = sb("esink", [128, 8], F32)
        idx_all = sb("idx_all", [128, NT * 4], I32)
        gate_all = sb("gate_all", [128, NT * 4], F32)
        cst = sb("cst", [128, 320], F32)
        utri, ones_f, iota_e, ebase = cst[:, 0:128], cst[:, 128:256], cst[:, 256:288], cst[:, 288:320]
        zfill = sb("zfill", [128, 2048], BF16)
        sc.add("dve", lambda e: e.memset(zfill[:], 0.0), writes=["zfill"])
        pX = ExitStack()
        xT = sb("xT", [128, 8, S], BF16, pX)

        sc.add("sp", lambda e: e.dma_start(out=ident[:], in_=ident_d), writes=["ident"], dma=True)
        sc.add("sp", lambda e: e.dma_start(out=negd[:].rearrange("p m c -> p (m c)"), in_=negd_d),
               writes=["negd"], dma=True)
        sc.add("sp", lambda e: e.dma_start(out=esink[:], in_=sinkc_d), writes=["esink"], dma=True)
        sc.add("dve", lambda e: e.tensor_copy(out=identb[:], in_=ident[:]), reads=["ident"], writes=["identb"])
        sc.add("dve", lambda e: e.memset(onesb[:], 1.0), writes=["onesb"])
        sc.add("act", lambda e: e.activation(out=esink[:], in_=esink[:], func=AF.Exp),
               reads=["esink"], writes=["esink"])

        with ExitStack() as p0:
            xin = [sb("xin%d" % i, [128, D], F32, p0) for i in range(2)]
            pst = [ps("pst%d" % i, [128, 512], F32, p0) for i in range(4)]
            for i in range(NT):
                b = i % 2
                sc.add("sp", lambda e, i=i, b=b: e.dma_start(out=xin[b][:], in_=x[i * 128:(i + 1) * 128, :]),
                       writes=[("xin", b)], dma=True)
                for h in range(2):
                    pb = (2 * i + h) % 4
                    for c4 in range(4):
                        c = h * 4 + c4
                        sc.add("pe", lambda e, b=b, c=c, c4=c4, pb=pb: e.transpose(
                            out=pst[pb][:, c4 * 128:(c4 + 1) * 128], in_=xin[b][:, c * 128:(c + 1) * 128],
                            identity=ident[:]),
                            reads=[("xin", b), "ident"], writes=[("pst", pb)])
                    if h == 0:
                        fn = lambda e, i=i, h=h, pb=pb: e.copy(
                            out=xT[:, h * 4:(h + 1) * 4, i * 128:(i + 1) * 128],
                            in_=pst[pb][:].rearrange("p (c t) -> p c t", c=4))
                    else:
                        fn = lambda e, i=i, h=h, pb=pb: e.tensor_copy(
                            out=xT[:, h * 4:(h + 1) * 4, i * 128:(i + 1) * 128],
                            in_=pst[pb][:].rearrange("p (c t) -> p c t", c=4))
                    sc.add("act" if h == 0 else "dve", fn, reads=[("pst", pb)], writes=[("xT", i)])
            sc.barrier()

        with ExitStack() as p1:
            QT = [sb("QT%d" % i, [128, S], BF16, p1) for i in range(2)]
            KTd = sb("KTd", [128, S], BF16, p1)
            Vd = sb("Vd", [128, NT, 128], BF16, p1)
            KTs = [sb("KTs%d" % i, [128, S], BF16, p1) for i in range(2)]
            Vs = sb("Vs", [128, NT, 128], BF16, p1)
            accN = sb("accN", [128, S], F32, p1)
            accD = sb("accD", [128, S], F32, p1)
            wq = [sb("wq%d" % i, [128, 8, 128], BF16, p1) for i in range(2)]
            wk = [sb("wk%d" % i, [128, 8, 128], BF16, p1) for i in range(2)]
            wv = [sb("wv%d" % i, [128, 8, 128], BF16, p1) for i in range(2)]
            tmp = [sb("tmp%d" % i, [128, 512], F32, p1) for i in range(3)]
            PT = [sb("PT%d" % i, [128, 512], BF16, p1) for i in range(3)]
            bias2 = [sb("bias2_%d" % i, [128, 2, 256], F32, p1) for i in range(2)]
            stg = [sb("stg%d" % i, [128, 512], BF16, p1) for i in range(2)]
            t1 = [sb("t1_%d" % i, [128, 512], F32, p1) for i in range(2)]
            pSall = ps("pSall", [128, 6, 512], F32, p1)
            pND = [ps("pND%d" % i, [128, 512], F32, p1) for i in range(2)]
            pj = pND
            cnt = {"pj": 0, "ev": 0, "blk": 0, "stg": 0, "zf": 0}

            def load_w(dst, key, c0, ncols=128, d0=0):
                sc.add("pool", lambda e: e.dma_start(out=dst[:, :, d0:d0 + ncols], in_=w_in_v[:, :, c0:c0 + ncols]),
                       writes=[key], dma=True)

            def evac(fn_act, fn_dve, reads, writes):
                k = cnt["ev"]
                cnt["ev"] += 1
                if k % 2 == 0:
                    sc.add("act", fn_act, reads=reads, writes=writes)
                else:
                    sc.add("dve", fn_dve, reads=reads, writes=writes)

            def proj_fm(w, wkey, dst, dkey, r):
                for tg in range(8):
                    b = cnt["pj"] % 2
                    cnt["pj"] += 1
                    for dc in range(8):
                        sc.add("pe", lambda e, b=b, dc=dc, tg=tg: e.matmul(
                            out=pj[b][:], lhsT=w[:, dc, :], rhs=xT[:, dc, tg * 512:(tg + 1) * 512],
                            start=(dc == 0), stop=(dc == 7)),
                            reads=[wkey], writes=[("pND", b)])
                    if r == 1:
                        o_ap = lambda tg=tg: dst[:, tg * 512:(tg + 1) * 512]
                        i_ap = lambda b=b: pj[b][:]
                    else:
                        n = 512 // r
                        o_ap = lambda tg=tg, n=n: dst[:].rearrange("p (r m) -> p r m", r=r)[:, :, tg * n:(tg + 1) * n]
                        i_ap = lambda b=b: pj[b][:].rearrange("p (m r) -> p r m", r=r)
                    evac(lambda e, o_ap=o_ap, i_ap=i_ap: e.copy(out=o_ap(), in_=i_ap()),
                         lambda e, o_ap=o_ap, i_ap=i_ap: e.tensor_copy(out=o_ap(), in_=i_ap()),
                         reads=[("pND", b)], writes=[(dkey, tg)])

            def proj_tm(w, wkey, dst, dkey, r):
                nbs = NT // r
                for b4 in range(NT // 4):
                    b = cnt["pj"] % 2
                    cnt["pj"] += 1
                    for k4 in range(4):
                        qb = b4 * 4 + k4
                        res, j = qb // nbs, qb % nbs
                        t0 = res + r * 128 * j
                        for dc in range(8):
                            sc.add("pe", lambda e, b=b, dc=dc, k4=k4, t0=t0: e.matmul(
                                out=pj[b][:, k4 * 128:(k4 + 1) * 128],
                                lhsT=xT[:, dc, t0:t0 + 127 * r + 1:r], rhs=w[:, dc, :],
                                start=(dc == 0), stop=(dc == 7)),
                                reads=[wkey], writes=[("pND", b)])
                    evac(lambda e, b=b, b4=b4: e.copy(out=dst[:, b4 * 4:(b4 + 1) * 4, :],
                                                     in_=pj[b][:].rearrange("p (k c) -> p k c", k=4)),
                         lambda e, b=b, b4=b4: e.tensor_copy(out=dst[:, b4 * 4:(b4 + 1) * 4, :],
                                                            in_=pj[b][:].rearrange("p (k c) -> p k c", k=4)),
                         reads=[("pND", b)], writes=[(dkey, b4)])

            def attention(g, Qb, qkeys, Kb, kkeys, Vb, vkey, vcol, slopes2, on_done, par):
                r = DIL[g]
                nbs = NT // r
                for hh in range(2):
                    sc.add("dve", lambda e, hh=hh: e.tensor_scalar(
                        out=bias2[par][:, hh, :], in0=negd[:, g, :], scalar1=float(slopes2[hh]), scalar2=None, op0=ALU.mult),
                        writes=[("bias2", par)])

                def scores(qb):
                    j = qb % nbs
                    sb_ = qb % 3
                    kbs = (0, 1) if j > 0 else (1,)
                    c0 = 0 if j > 0 else 128
                    for hh in range(2):
                        for kb in kbs:
                            kblk = qb - 1 + kb
                            sc.add("pe", lambda e, hh=hh, kb=kb, kblk=kblk: e.matmul(
                                out=pSall[:, 2 * sb_ + hh, kb * 128:(kb + 1) * 128],
                                lhsT=Kb[hh * 64:(hh + 1) * 64, kblk * 128:(kblk + 1) * 128],
                                rhs=Qb[hh * 64:(hh + 1) * 64, qb * 128:(qb + 1) * 128],
                                start=True, stop=True),
                                reads=list(qkeys) + list(kkeys), writes=[("pS", sb_, hh)])
                    t3 = lambda ap: ap[:].rearrange("p (h c) -> p h c", h=2)[:, :, c0:256]
                    sc.add("dve", lambda e: e.tensor_tensor(
                        out=t3(tmp[sb_]), in0=pSall[:, 2 * sb_:2 * sb_ + 2, c0:256], in1=bias2[par][:, :, c0:256], op=ALU.add),
                        reads=[("pS", sb_, 0), ("pS", sb_, 1), ("bias2", par)], writes=[("tmp", sb_)])
                    sc.add("act", lambda e: e.activation(out=t3(PT[sb_]), in_=t3(tmp[sb_]), func=AF.Exp, scale=0.125),
                           reads=[("tmp", sb_)], writes=[("PT", sb_)])

                def pv(qb):
                    j = qb % nbs
                    sb_ = qb % 3
                    kbs = (0, 1) if j > 0 else (1,)
                    b2i, k2 = qb // 2, qb % 2
                    pb = b2i % 2
                    for hh in range(2):
                        for (col0, isnum) in ((k2 * 128, True), (256 + k2 * 128, False)):
                            for ki, kb in enumerate(kbs):
                                kblk = qb - 1 + kb
                                lh = (lambda kblk=kblk, hh=hh: Vb[:, kblk, vcol[hh]:vcol[hh] + 64]) if isnum \
                                    else (lambda: onesb[:, 0:64])
                                rd = [("PT", sb_)] + ([(vkey, kblk // 4)] if isnum else [])
                                sc.add("pe", lambda e, hh=hh, kb=kb, ki=ki, col0=col0, lh=lh: e.matmul(
                                    out=pND[pb][hh * 64:(hh + 1) * 64, col0:col0 + 128],
                                    lhsT=lh(),
                                    rhs=PT[sb_][:, (hh * 2 + kb) * 128:(hh * 2 + kb + 1) * 128],
                                    start=(ki == 0), stop=(ki == len(kbs) - 1)),
                                    reads=rd, writes=[("pND", pb)])
                    if k2 == 1:
                        on_done(b2i, pb)

                for qb in range(NT + 2):
                    if qb < NT:
                        scores(qb)
                    if qb >= 2:
                        pv(qb - 2)

            ZCH = (NSLOT // 128) * D // 2048
            xs_flat = xs_d.rearrange("(p r) d -> p (r d)", p=128)

            def store_stage(chunk, t0, sbuf_i):
                sc.add("sp", lambda e: e.dma_start(out=oT_d[chunk, :, t0:t0 + 512], in_=stg[sbuf_i][:]),
                       reads=[("stg", sbuf_i)], dma=True)
                zi = cnt["zf"]
                if zi < ZCH:
                    cnt["zf"] += 1
                    sc.add("sp", lambda e: e.dma_start(out=xs_flat[:, zi * 2048:(zi + 1) * 2048], in_=zfill[:]),
                           reads=["zfill"], dma=True)

            for kv in range(2):
                load_w(wk[kv], ("wk", kv), C_BK + kv * 64, 64, 0)
                load_w(wk[kv], ("wk", kv), C_BK + kv * 64, 64, 64)
            load_w(wv[0], ("wv", 0), C_BV, 128)
            for kv in range(2):
                proj_fm(wk[kv], ("wk", kv), KTs[kv], ("KTs", kv), 1)
            proj_tm(wv[0], ("wv", 0), Vs, "Vs", 1)

            if stage == "p1a":
                dbg = sb("dbg", [128, S], F32, p1)
                for ci, (src, keys) in enumerate([(KTs[0][:], [(("KTs", 0), tg) for tg in range(8)]),
                                                  (KTs[1][:], [(("KTs", 1), tg) for tg in range(8)]),
                                                  (Vs[:].rearrange("p b c -> p (b c)"), [("Vs", b4) for b4 in range(8)])]):
                    sc.add("dve", lambda e, src=src: e.tensor_copy(out=dbg[:], in_=src), reads=keys, writes=["dbg"])
                    sc.add("sp", lambda e, ci=ci: e.dma_start(
                        out=out.rearrange("(r q) d -> r (q d)", q=4)[ci * 128:(ci + 1) * 128, :], in_=dbg[:]),
                        reads=["dbg"], dma=True)
            jobs = [(0, p) for p in range(8)] + [(g, pp) for pp in range(2) for g in (1, 2, 3)]
            if stage == "p1a":
                jobs = []
            if stage == "swa0":
                jobs = [(0, 0), (0, 5)]
            if stage == "dil0":
                jobs = [(1, 0), (2, 0), (3, 0)]
            for ji, (g, p) in enumerate(jobs):
                par = ji % 2
                r = DIL[g]
                if g == 0:
                    load_w(wq[par], ("wq", par), C_BQ + p * 128)
                    proj_fm(wq[par], ("wq", par), QT[par], ("Q", par), 1)
                    kv = p // 4
                    sl = (SLOPES[2 * p], SLOPES[2 * p + 1])

                    def done(b2i, pb, p=p):
                        b4, half = b2i // 2, b2i % 2
                        si = b4 % 2
                        hs = slice(half * 256, (half + 1) * 256)
                        sc.add("dve", lambda e: e.tensor_scalar(
                            out=t1[si][:, hs], in0=pND[pb][:, 256:512], scalar1=esink[:, p:p + 1], scalar2=None, op0=ALU.add),
                            reads=[("pND", pb)], writes=[("t1", si, half)])
                        sc.add("dve", lambda e: e.reciprocal(out=t1[si][:, hs], in_=t1[si][:, hs]),
                               reads=[("t1", si, half)], writes=[("t1", si, half)])
                        sc.add("dve", lambda e: e.tensor_tensor(
                            out=stg[si][:, hs], in0=pND[pb][:, 0:256], in1=t1[si][:, hs], op=ALU.mult),
                            reads=[("pND", pb), ("t1", si, half)], writes=[("stg", si)])
                        if half == 1:
                            store_stage(p, b4 * 512, si)

                    attention(0, QT[par], [(("Q", par), tg) for tg in range(8)],
                              KTs[kv], [(("KTs", kv), tg) for tg in range(8)],
                              Vs, "Vs", (kv * 64, kv * 64), sl, done, par)
                else:
                    c0 = (g - 1) * 256 + p * 128
                    load_w(wq[par], ("wq", par), C_AQ + c0)
                    load_w(wk[par], ("wk", par), C_AK + c0)
                    load_w(wv[par], ("wv", par), C_AV + c0)
                    proj_fm(wq[par], ("wq", par), QT[par], ("Q", par), r)
                    proj_fm(wk[par], ("wk", par), KTd, "KTd", r)
                    proj_tm(wv[par], ("wv", par), Vd, "Vd", r)
                    sl = (SLOPES[16 + (g - 1) * 4 + 2 * p], SLOPES[16 + (g - 1) * 4 + 2 * p + 1])
                    nbs = NT // r

                    def done(b2i, pb, g=g, r=r, nbs=nbs):
                        qb0 = b2i * 2
                        res, j0 = qb0 // nbs, qb0 % nbs
                        n, t0 = 256, res + r * 128 * j0
                        halves = sorted({(t0 + r * i) // 2048 for i in (0, n - 1)})
                        keys = [("acc", h) for h in halves]
                        for acc, pc in ((accN, 0), (accD, 256)):
                            if g == 1:
                                sc.add("act", lambda e, acc=acc, pc=pc: e.copy(
                                    out=acc[:, t0:t0 + (n - 1) * r + 1:r], in_=pND[pb][:, pc:pc + n]),
                                    reads=[("pND", pb)], writes=keys)
                            else:
                                sc.add("dve", lambda e, acc=acc, pc=pc: e.tensor_tensor(
                                    out=acc[:, t0:t0 + (n - 1) * r + 1:r], in0=pND[pb][:, pc:pc + n],
                                    in1=acc[:, t0:t0 + (n - 1) * r + 1:r], op=ALU.add),
                                    reads=[("pND", pb)] + keys, writes=keys)

                    attention(g, QT[par], [(("Q", par), tg) for tg in range(8)],
                              KTd, [("KTd", tg) for tg in range(8)],
                              Vd, "Vd", (0, 64), sl, done, par)
                    if g == 3:
                        for t8 in range(8):
                            si = t8 % 2
                            keys = [("acc", t8 // 4)]
                            sc.add("dve", lambda e, t8=t8, si=si: e.reciprocal(
                                out=t1[si][:], in_=accD[:, t8 * 512:(t8 + 1) * 512]),
                                reads=keys, writes=[("t1", si, 0), ("t1", si, 1)])
                            sc.add("dve", lambda e, t8=t8, si=si: e.tensor_tensor(
                                out=stg[si][:], in0=accN[:, t8 * 512:(t8 + 1) * 512], in1=t1[si][:], op=ALU.mult),
                                reads=keys + [("t1", si, 0), ("t1", si, 1)], writes=[("stg", si)])
                            store_stage(8 + p, t8 * 512, si)
            sc.barrier()


        if stage == "p01":
            pX.close()
            sc.emit(es)
            return nc
        if stage in ("swa0", "dil0", "attn"):
            with ExitStack() as pd:
                dbgb = sb("dbgb", [128, S], BF16, pd)
                dbg = sb("dbg", [128, S], F32, pd)
                chunks = {"swa0": [0, 5], "dil0": [8], "attn": list(range(8))}[stage]
                for ci, c in enumerate(chunks):
                    sc.add("sp", lambda e, c=c: e.dma_start(out=dbgb[:], in_=oT_d[c, :, :]), writes=["dbgb"], dma=True)
                    sc.add("dve", lambda e: e.tensor_copy(out=dbg[:], in_=dbgb[:]), reads=["dbgb"], writes=["dbg"])
                    sc.add("sp", lambda e, ci=ci: e.dma_start(
                        out=out.rearrange("(r q) d -> r (q d)", q=4)[ci * 128:(ci + 1) * 128, :], in_=dbg[:]),
                        reads=["dbg"], dma=True)
            sc.emit(es)
            return nc

        _regs = {}

        def bcreg(e):
            if "bc" not in _regs:
                r = e.alloc_register("bc")
                e.reg_mov(r, NSLOT - 1)
                _regs["bc"] = r
            return _regs["bc"]

        def ln_stats(p, zt, zkey, stats, mv):
            for h in range(2):
                sc.add("dve", lambda e, h=h: e.bn_stats(out=stats[:, h * 6:(h + 1) * 6], in_=zt[:, h * 512:(h + 1) * 512]),
                       reads=[zkey], writes=[("stats", p)])
            sc.add("dve", lambda e: e.bn_aggr(out=mv[:, 0:2], in_=stats[:, 0:12]), reads=[("stats", p)], writes=[("mv", p)])
            sc.add("dve", lambda e: e.tensor_scalar(out=mv[:, 2:3], in0=mv[:, 1:2], scalar1=LN_EPS, scalar2=None, op0=ALU.add),
                   reads=[("mv", p)], writes=[("mv", p)])
            sc.add("act", lambda e: e.activation(out=mv[:, 2:3], in_=mv[:, 2:3], func=AF.Sqrt),
                   reads=[("mv", p)], writes=[("mv", p)])
            sc.add("dve", lambda e: e.reciprocal(out=mv[:, 3:4], in_=mv[:, 2:3]), reads=[("mv", p)], writes=[("mv", p)])
            sc.add("dve", lambda e: e.scalar_tensor_tensor(out=mv[:, 4:5], in0=mv[:, 0:1], scalar=-1.0, in1=mv[:, 3:4],
                                                          op0=ALU.mult, op1=ALU.mult),
                   reads=[("mv", p)], writes=[("mv", p)])

        def ln_apply(p, zt, zkey, gam, bet, dst, dkey, mv, tmpn, tkeys=None):
            tk = list(tkeys) if tkeys is not None else [("tmpn", p)]
            sc.add("act", lambda e: e.activation(out=tmpn[:], in_=zt[:], func=AF.Identity, scale=mv[:, 3:4], bias=mv[:, 4:5]),
                   reads=[zkey, ("mv", p)], writes=tk)
            sc.add("dve", lambda e: e.tensor_tensor(out=tmpn[:], in0=tmpn[:], in1=gam, op=ALU.mult),
                   reads=tk, writes=tk)
            sc.add("pool", lambda e: e.tensor_tensor(out=dst, in0=tmpn[:], in1=bet, op=ALU.add),
                   reads=tk, writes=[dkey])

        def layer_norm(p, zt, zkey, gam, bet, dst, dkey, stats, mv, tmpn):
            ln_stats(p, zt, zkey, stats, mv)
            ln_apply(p, zt, zkey, gam, bet, dst, dkey, mv, tmpn)


        with ExitStack() as p2:
            wg = sb("wg", [128, 8, 2048], BF16, p2)
            wpa = sb("wpa", [128, 2, 1024], BF16, p2)
            wpb = sb("wpb", [128, 8, 1024], BF16, p2)
            wo = sb("wo", [128, 8, 1024], BF16, p2)
            rw = sb("rw", [128, 8, 32], F32, p2)
            rb = sb("rb", [128, 32], F32, p2)
            ln1 = sb("ln1", [128, 2, 1024], F32, p2)
            oTt = [sb("oTt%d" % i, [128, 10, 128], BF16, p2) for i in range(2)]
            xres = sb("xres", [128, D], F32, p2)
            sg = sb("sg", [128, 2048], F32, p2)
            m1 = sb("m1", [128, D], F32, p2)
            m2 = sb("m2", [128, D], F32, p2)
            mT = sb("mT", [128, 8, 128], BF16, p2)
            zt = sb("zt", [128, D], F32, p2)
            y32 = sb("y32", [128, D], F32, p2)
            ybf = [sb("ybf%d" % i, [128, D], BF16, p2) for i in range(2)]
            yT = sb("yT", [128, 8, 128], F32, p2)
            stats = sb("stats", [128, 12], F32, p2)
            mv = sb("mv", [128, 8], F32, p2)
            lg = sb("lg", [128, 32], F32, p2)
            top8 = sb("top8", [128, 8], F32, p2)
            idx8 = sb("idx8", [128, 8], U32, p2)
            idxf = sb("idxf", [128, 8], F32, p2)
            negmax = sb("negmax", [128, 1], F32, p2)
            e4 = sb("e4", [128, 4], F32, p2)
            s4 = sb("s4", [128, 2], F32, p2)
            mask = sb("mask", [128, 32], F32, p2)
            slotf = sb("slotf", [128, 32], F32, p2)
            ovf = sb("ovf", [128, 32], F32, p2)
            CNT = sb("CNT", [128, 32], F32, p2)
            oh = sb("oh", [128, 32], F32, p2)
            j32 = sb("j32", [128, 32], F32, p2)
            slotk = sb("slotk", [128, 4], F32, p2)
            okk = sb("okk", [128, 4], F32, p2)
            B = [ps("B%d" % i, [128, 512], F32, p2) for i in range(8)]

            sc.add("sp", lambda e: e.dma_start(out=cst[:], in_=cst_d), writes=["cst"], dma=True)
            sc.add("sp", lambda e: e.dma_start(out=rw[:], in_=rw_d.rearrange("(dc p) e -> p dc e", p=128)), writes=["rw"], dma=True)
            sc.add("sp", lambda e: e.dma_start(out=rb[:], in_=rb_d), writes=["rb"], dma=True)
            sc.add("sp", lambda e: e.dma_start(out=ln1[:].rearrange("p a d -> p (a d)"), in_=lnp_d[:, 0:2048]), writes=["ln1"], dma=True)
            for n4 in range(4):
                sc.add("pool", lambda e, n4=n4: e.dma_start(out=wg[:, :, n4 * 512:(n4 + 1) * 512],
                                                            in_=w_in_v[:, :, C_GA + n4 * 512:C_GA + (n4 + 1) * 512]),
                       writes=["wg"], dma=True)
            sc.add("pool", lambda e: e.dma_start(out=wpa[:], in_=wpa_d.rearrange("(c p) d -> p c d", p=128)), writes=["wpa"], dma=True)
            for hf in range(2):
                sc.add("pool", lambda e, hf=hf: e.dma_start(out=wpb[:, :, hf * 512:(hf + 1) * 512],
                                                            in_=wpb_d.rearrange("(c p) d -> p c d", p=128)[:, :, hf * 512:(hf + 1) * 512]),
                       writes=["wpb"], dma=True)
                sc.add("pool", lambda e, hf=hf: e.dma_start(out=wo[:, :, hf * 512:(hf + 1) * 512],
                                                            in_=wo_d.rearrange("(c p) d -> p c d", p=128)[:, :, hf * 512:(hf + 1) * 512]),
                       writes=["wo"], dma=True)
            sc.add("dve", lambda e: e.memset(CNT[:], 0.0), writes=["CNT"])
            sc.barrier()

            zts = [zt, sb("zt1", [128, D], F32, p2)]
            y32s = [y32, sb("y32b", [128, D], F32, p2)]
            Fb = B[0:5]
            CT = B[5:7]
            CL = B[7]

            def A_loads(i):
                b = i % 2
                tsl = slice(i * 128, (i + 1) * 128)
                sc.add("sp", lambda e: e.dma_start(out=oTt[b][:], in_=oT_d[:, :, tsl].rearrange("c p t -> p c t")),
                       writes=[("oTt", b)], dma=True)
                sc.add("sp", lambda e: e.dma_start(out=xres[:], in_=x[tsl, :]), writes=["xres"], dma=True)

            def A_gates(i, half):
                tsl = slice(i * 128, (i + 1) * 128)
                for n2 in range(2):
                    n4 = half * 2 + n2
                    for dc in range(8):
                        sc.add("pe", lambda e, n2=n2, n4=n4, dc=dc: e.matmul(
                            out=Fb[n2][:], lhsT=xT[:, dc, tsl], rhs=wg[:, dc, n4 * 512:(n4 + 1) * 512],
                            start=(dc == 0), stop=(dc == 7)), writes=[("F", n2)])
                    sc.add("act", lambda e, n2=n2, n4=n4: e.activation(out=sg[:, n4 * 512:(n4 + 1) * 512], in_=Fb[n2][:],
                                                                        func=AF.Sigmoid),
                           reads=[("F", n2)], writes=[("sg", n4)])

            def A_proj(i, which):
                b = i % 2
                nck, w_, c0 = (2, wpa, 8) if which == 0 else (8, wpb, 0)
                for hf in range(2):
                    for c in range(nck):
                        sc.add("pe", lambda e, hf=hf, c=c: e.matmul(
                            out=Fb[2 + hf][:], lhsT=oTt[b][:, c0 + c, :], rhs=w_[:, c, hf * 512:(hf + 1) * 512],
                            start=(c == 0), stop=(c == nck - 1)), reads=[("oTt", b)], writes=[("F", 2 + hf)])

            def A_m(i, which):
                for hf in range(2):
                    hs = slice(hf * 512, (hf + 1) * 512)
                    if which == 0:
                        sc.add("dve", lambda e, hf=hf, hs=hs: e.tensor_tensor(
                            out=m1[:, hs], in0=Fb[2 + hf][:], in1=sg[:, hs], op=ALU.mult),
                            reads=[("F", 2 + hf), ("sg", hf)], writes=[("m1", hf)])
                    else:
                        sc.add("dve", lambda e, hf=hf, hs=hs: e.tensor_tensor(
                            out=m2[:, hs], in0=Fb[2 + hf][:], in1=sg[:, 1024 + hf * 512:1024 + (hf + 1) * 512], op=ALU.mult),
                            reads=[("F", 2 + hf), ("sg", 2 + hf)], writes=[("m2", hf)])
                        sc.add("pool", lambda e, hs=hs: e.tensor_tensor(out=m1[:, hs], in0=m1[:, hs], in1=m2[:, hs], op=ALU.add),
                               reads=[("m1", hf), ("m2", hf)], writes=[("m1", hf)])

            def A_tail(i):
                zb = i % 2
                tb = (4, 0)
                for hf in range(2):
                    for c4 in range(4):
                        c = hf * 4 + c4
                        sc.add("pe", lambda e, hf=hf, c=c, c4=c4: e.transpose(
                            out=Fb[tb[hf]][:, c4 * 128:(c4 + 1) * 128], in_=m1[:, c * 128:(c + 1) * 128], identity=ident[:]),
                            reads=[("m1", hf)], writes=[("F", tb[hf])])
                    fa = lambda e, hf=hf: e.copy(out=mT[:, hf * 4:(hf + 1) * 4, :],
                                                 in_=Fb[tb[hf]][:].rearrange("p (c t) -> p c t", c=4))
                    fd = lambda e, hf=hf: e.tensor_copy(out=mT[:, hf * 4:(hf + 1) * 4, :],
                                                        in_=Fb[tb[hf]][:].rearrange("p (c t) -> p c t", c=4))
                    sc.add("act" if hf == 0 else "dve", fa if hf == 0 else fd, reads=[("F", tb[hf])], writes=[("mT", hf)])
                for hf in range(2):
                    for c in range(8):
                        sc.add("pe", lambda e, hf=hf, c=c: e.matmul(
                            out=Fb[2 + hf][:], lhsT=mT[:, c, :], rhs=wo[:, c, hf * 512:(hf + 1) * 512],
                            start=(c == 0), stop=(c == 7)), reads=[("mT", 0), ("mT", 1)], writes=[("F", 2 + hf)])
                    sc.add("dve", lambda e, hf=hf: e.scalar_tensor_tensor(
                        out=zts[zb][:, hf * 512:(hf + 1) * 512], in0=xres[:, hf * 512:(hf + 1) * 512], scalar=float(ALPHA),
                        in1=Fb[2 + hf][:], op0=ALU.mult, op1=ALU.add),
                        reads=["xres", ("F", 2 + hf)], writes=[("zt", zb)])

            def B_stats(i):
                zb = i % 2
                ln_stats("a", zts[zb], ("zt", zb), stats, mv)

            def B_norm(i):
                zb = i % 2
                tsl = slice(i * 128, (i + 1) * 128)
                ln_apply("a", zts[zb], ("zt", zb), ln1[:, 0, :], ln1[:, 1, :], y32s[zb][:], ("y32", zb), mv, m2,
                         tkeys=[("m2", 0), ("m2", 1)])
                sc.add("sp", lambda e: e.dma_start(out=y32_d[tsl, :], in_=y32s[zb][:]), reads=[("y32", zb)], dma=True)
                sc.add("act", lambda e: e.copy(out=ybf[zb][:], in_=y32s[zb][:]), reads=[("y32", zb)], writes=[("ybf", zb)])

            def C_routerT(i):
                yb = i % 2
                for hf in range(2):
                    for c4 in range(4):
                        c = hf * 4 + c4
                        sc.add("pe", lambda e, hf=hf, c=c, c4=c4: e.transpose(
                            out=CT[hf][:, c4 * 128:(c4 + 1) * 128], in_=y32s[yb][:, c * 128:(c + 1) * 128], identity=ident[:]),
                            reads=[("y32", yb)], writes=[("CT", hf)])
                    fa = lambda e, hf=hf: e.copy(out=yT[:, hf * 4:(hf + 1) * 4, :], in_=CT[hf][:].rearrange("p (c t) -> p c t", c=4))
                    fd = lambda e, hf=hf: e.tensor_copy(out=yT[:, hf * 4:(hf + 1) * 4, :], in_=CT[hf][:].rearrange("p (c t) -> p c t", c=4))
                    sc.add("act" if hf == 0 else "dve", fa if hf == 0 else fd, reads=[("CT", hf)], writes=[("yT", hf)])

            def C_logits(i):
                for dc in range(8):
                    sc.add("pe", lambda e, dc=dc: e.matmul(out=CL[:, 0:32], lhsT=yT[:, dc, :], rhs=rw[:, dc, :],
                                                           start=(dc == 0), stop=(dc == 7)),
                           reads=[("yT", 0), ("yT", 1)], writes=["CL"])
                sc.add("dve", lambda e: e.tensor_tensor(out=lg[:], in0=CL[:, 0:32], in1=rb[:], op=ALU.add),
                       reads=["CL"], writes=["lg"])
                sc.add("dve", lambda e: e.max(out=top8[:], in_=lg[:]), reads=["lg"], writes=["top8"])
                sc.add("dve", lambda e: e.max_index(out=idx8[:], in_max=top8[:], in_values=lg[:]),
                       reads=["lg", "top8"], writes=["idx8"])
                sc.add("dve", lambda e: e.tensor_copy(out=idxf[:], in_=idx8[:]), reads=["idx8"], writes=["idxf"])
                sc.add("dve", lambda e: e.tensor_scalar(out=negmax[:], in0=top8[:, 0:1], scalar1=-1.0, scalar2=None, op0=ALU.mult),
                       reads=["top8"], writes=["negmax"])
                sc.add("act", lambda e: e.activation(out=e4[:], in_=top8[:, 0:4], func=AF.Exp, bias=negmax[:, 0:1], scale=1.0,
                                                     accum_out=s4[:, 0:1]),
                       reads=["top8", "negmax"], writes=["e4", "s4"])
                sc.add("dve", lambda e: e.tensor_scalar(out=mask[:], in0=lg[:], scalar1=top8[:, 3:4], scalar2=None, op0=ALU.is_ge),
                       reads=["lg", "top8"], writes=["mask"])

            def C_pos(i):
                b = i % 2
                sc.add("pe", lambda e: e.matmul(out=CL[:, 32:64], lhsT=utri, rhs=mask[:], start=True, stop=True),
                       reads=["mask"], writes=["CL"])
                sc.add("pe", lambda e: e.matmul(out=CL[:, 64:96], lhsT=ones_f, rhs=mask[:], start=True, stop=True),
                       reads=["mask"], writes=["CL"])
                sc.add("dve", lambda e: e.reciprocal(out=s4[:, 1:2], in_=s4[:, 0:1]), reads=["s4"], writes=["s4"])
                sc.add("dve", lambda e: e.tensor_tensor(out=slotf[:], in0=CL[:, 32:64], in1=CNT[:], op=ALU.add),
                       reads=["CL", "CNT"], writes=["slotf"])
                sc.add("dve", lambda e: e.tensor_tensor(out=CNT[:], in0=CL[:, 64:96], in1=CNT[:], op=ALU.add),
                       reads=["CL", "CNT", "slotf"], writes=["CNT"])
                sc.add("dve", lambda e: e.tensor_scalar(out=ovf[:], in0=slotf[:], scalar1=float(CAP), scalar2=1.0e6,
                                                       op0=ALU.is_ge, op1=ALU.mult),
                       reads=["slotf"], writes=["ovf"])
                sc.add("dve", lambda e: e.tensor_tensor(out=slotf[:], in0=slotf[:], in1=ebase, op=ALU.add),
                       reads=["slotf"], writes=["slotf"])
                sc.add("dve", lambda e: e.tensor_tensor(out=slotf[:], in0=slotf[:], in1=ovf[:], op=ALU.add),
                       reads=["slotf", "ovf"], writes=["slotf"])
                for k in range(4):
                    sc.add("dve", lambda e, k=k: e.tensor_scalar(out=oh[:], in0=iota_e, scalar1=idxf[:, k:k + 1], scalar2=None,
                                                                op0=ALU.is_equal),
                           reads=["idxf"], writes=["oh"])
                    sc.add("dve", lambda e, k=k: e.tensor_tensor(out=j32[:], in0=oh[:], in1=slotf[:], op=ALU.mult),
                           reads=["oh", "slotf"], writes=["j32"])
                    sc.add("dve", lambda e, k=k: e.reduce_sum(out=slotk[:, k:k + 1], in_=j32[:], axis=AX.X),
                           reads=["j32"], writes=["slotk"])
                sc.add("dve", lambda e: e.tensor_scalar(out=okk[:], in0=slotk[:], scalar1=float(NSLOT), scalar2=None, op0=ALU.is_lt),
                       reads=["slotk"], writes=["okk"])
                sc.add("dve", lambda e: e.tensor_scalar(out=e4[:], in0=e4[:], scalar1=s4[:, 1:2], scalar2=None, op0=ALU.mult),
                       reads=["e4", "s4"], writes=["e4"])
                sc.add("dve", lambda e: e.tensor_tensor(out=gate_all[:, i * 4:(i + 1) * 4], in0=e4[:], in1=okk[:], op=ALU.mult),
                       reads=["e4", "okk"], writes=[("gate", i)])
                sc.add("dve", lambda e: e.tensor_copy(out=idx_all[:, i * 4:(i + 1) * 4], in_=slotk[:]),
                       reads=["slotk"], writes=[("idx", i)])
                for k in range(4):
                    sc.add("pool", lambda e, k=k: e.indirect_dma_start(
                        out=xs_d[:, :], out_offset=bass.IndirectOffsetOnAxis(ap=idx_all[:, i * 4 + k:i * 4 + k + 1], axis=0),
                        in_=ybf[b][:, :], in_offset=None, bounds_check=bcreg(e), oob_is_err=False),
                        reads=[("idx", i), ("ybf", b)], dma=True)

            for s_ in range(NT + 2):
                a, bt, ct = s_, s_ - 1, s_ - 2
                hasA, hasB, hasC = a < NT, 0 <= bt < NT, 0 <= ct < NT
                if hasA:
                    A_loads(a)
                if hasB:
                    B_stats(bt)
                if hasA:
                    A_gates(a, 0)
                    A_proj(a, 0)
                if hasC:
                    C_routerT(ct)
                if hasB:
                    B_norm(bt)
                if hasA:
                    A_m(a, 0)
                    A_gates(a, 1)
                if hasC:
                    C_logits(ct)
                if hasA:
                    A_proj(a, 1)
                    A_m(a, 1)
                    A_tail(a)
                if hasC:
                    C_pos(ct)
            sc.barrier()
        pX.close()

        if stage == "y":
            sc.emit(es)
            return nc


        NB = CAP // 128
        SG = CAP // 2
        with ExitStack() as p4:
            wgt = [sb("wgt%d" % i, [128, 8, D], BF16, p4) for i in range(2)]
            wup = [sb("wup%d" % i, [128, 8, D], BF16, p4) for i in range(2)]
            wdn = [sb("wdn%d" % i, [128, 8, D], BF16, p4) for i in range(2)]
            bdr = [sb("bdr%d" % i, [1, D], BF16, p4) for i in range(2)]
            bgu = sb("bgu", [128, 512], F32, p4)
            onesr = sb("onesr", [1, 128], BF16, p4)
            NXS = 5
            xs = [sb("xs%d" % i, [128, D], BF16, p4) for i in range(NXS)]
            xsT = [sb("xsT%d" % i, [128, 8, CAP], BF16, p4) for i in range(2)]
            hT = [sb("hT%d" % i, [128, 8, CAP], BF16, p4) for i in range(2)]
            gtt = [sb("gtt%d" % i, [128, SG], F32, p4) for i in range(2)]
            sgm = [sb("sgm%d" % i, [128, SG], F32, p4) for i in range(2)]
            ubt = [sb("ubt%d" % i, [128, SG], F32, p4) for i in range(2)]
            ysb = [sb("ysb%d" % i, [128, D], BF16, p4) for i in range(2)]
            pT = [ps("pT%d" % i, [128, 1024], BF16, p4) for i in range(2)]
            pG = [ps("pG%d" % i, [128, 512], F32, p4) for i in range(2)]
            pU = [ps("pU%d" % i, [128, 512], F32, p4) for i in range(2)]
            pY = [ps("pY%d" % i, [128, 512], F32, p4) for i in range(2)]
            sc.add("sp", lambda e: e.dma_start(out=bgu[:], in_=bgu_d), writes=["bgu"], dma=True)
            sc.add("dve", lambda e: e.memset(onesr[:], 1.0), writes=["onesr"])
            c4 = {"xs": 0, "xl": 0, "g": 0, "y": 0, "ev": 0}

            wdst = sb("wdst", [128, 8, D], F32, p4)

            def load_expert(e_):
                par = e_ % 2
                for (dst, src, nm) in ((wgt, wgate_d, "wgt"), (wup, wup_d, "wup")):
                    for hf in range(2):
                        sc.add("pool", lambda e, dst=dst, src=src, hf=hf: e.dma_start(
                            out=dst[par][:, :, hf * 512:(hf + 1) * 512],
                            in_=src[e_].rearrange("(dc p) f -> p dc f", p=128)[:, :, hf * 512:(hf + 1) * 512]),
                            writes=[(nm, par)], dma=True)
                sc.add("pool", lambda e: e.dma_start(out=bdr[par][:], in_=bdn_d[e_:e_ + 1, :]), writes=[("bdr", par)], dma=True)
                for hf in range(2):
                    sc.add("sp", lambda e, hf=hf: e.dma_start(
                        out=wdst[:, :, hf * 512:(hf + 1) * 512],
                        in_=wdown_d[e_].rearrange("(dc p) f -> p dc f", p=128)[:, :, hf * 512:(hf + 1) * 512]),
                        writes=[("wdst", hf)], dma=True)

            def cast_dn(e_):
                par = e_ % 2
                for dc in range(8):
                    sc.add("act", lambda e, dc=dc: e.copy(out=wdn[par][:, dc, :], in_=wdst[:, dc, :]),
                           reads=[("wdst", 0), ("wdst", 1)], writes=[("wdn", par)])

            n_exp = NE if stage != "moe1" else 2
            load_expert(0)
            cast_dn(0)
            def load_xs(e_):
                for blk in range(NB):
                    xb = c4["xl"] % NXS
                    c4["xl"] += 1
                    r0 = e_ * CAP + blk * 128
                    sc.add("sp", lambda e, xb=xb, r0=r0: e.dma_start(out=xs[xb][:], in_=xs_d[r0:r0 + 128, :]),
                           writes=[("xs", xb)], dma=True)

            def stage_T(e_):
                par = e_ % 2
                for blk in range(NB):
                    xb = c4["xs"] % NXS
                    tb = c4["xs"] % 2
                    c4["xs"] += 1
                    for dc in range(8):
                        sc.add("pe", lambda e, xb=xb, tb=tb, dc=dc: e.transpose(
                            out=pT[tb][:, dc * 128:(dc + 1) * 128], in_=xs[xb][:, dc * 128:(dc + 1) * 128], identity=identb[:]),
                            reads=[("xs", xb)], writes=[("pT", tb)])
                    sc.add("act", lambda e, blk=blk, tb=tb: e.copy(out=xsT[par][:, :, blk * 128:(blk + 1) * 128],
                                                                 in_=pT[tb][:].rearrange("p (c t) -> p c t", c=8)),
                           reads=[("pT", tb)], writes=[("xsT", par, blk)])

            def stage_GU(e_):
                par = e_ % 2
                xkeys = [("xsT", par, blk) for blk in range(NB)]
                groups = [(sgi, fc) for sgi in range(2) for fc in range(8)]

                def A(n):
                    sgi, fc = groups[n]
                    ss = slice(sgi * SG, (sgi + 1) * SG)
                    gb = n % 2
                    for (pp_, w_, nm, wn) in ((pG, wgt, "pG", "wgt"), (pU, wup, "pU", "wup")):
                        for dc in range(8):
                            sc.add("pe", lambda e, pp_=pp_, w_=w_, dc=dc: e.matmul(
                                out=pp_[gb][:, 0:SG], lhsT=w_[par][:, dc, fc * 128:(fc + 1) * 128], rhs=xsT[par][:, dc, ss],
                                start=(dc == 0), stop=(dc == 7)),
                                reads=xkeys + [(wn, par)], writes=[(nm, gb)])
                    col = e_ * 8 + fc
                    sc.add("dve", lambda e: e.tensor_scalar(
                        out=gtt[gb][:], in0=pG[gb][:, 0:SG], scalar1=bgu[:, col:col + 1], scalar2=7.0,
                        op0=ALU.add, op1=ALU.min), reads=[("pG", gb), "bgu"], writes=[("gtt", gb)])
                    sc.add("act", lambda e: e.activation(out=sgm[gb][:], in_=gtt[gb][:], func=AF.Sigmoid, scale=1.702),
                           reads=[("gtt", gb)], writes=[("sgm", gb)])
                    sc.add("act", lambda e: e.activation(
                        out=ubt[gb][:], in_=pU[gb][:, 0:SG], func=AF.Identity, bias=bgu[:, 256 + col:256 + col + 1], scale=1.0),
                        reads=[("pU", gb), "bgu"], writes=[("ubt", gb)])

                def B(n):
                    sgi, fc = groups[n]
                    ss = slice(sgi * SG, (sgi + 1) * SG)
                    gb = n % 2
                    sc.add("pool", lambda e: e.tensor_scalar(out=ubt[gb][:], in0=ubt[gb][:], scalar1=7.0, scalar2=-7.0,
                                                            op0=ALU.min, op1=ALU.max),
                           reads=[("ubt", gb)], writes=[("ubt", gb)])
                    sc.add("pool", lambda e: e.tensor_tensor(out=gtt[gb][:], in0=gtt[gb][:], in1=sgm[gb][:], op=ALU.mult),
                           reads=[("gtt", gb), ("sgm", gb)], writes=[("gtt", gb)])
                    sc.add("dve", lambda e: e.scalar_tensor_tensor(
                        out=hT[par][:, fc, ss], in0=ubt[gb][:], scalar=1.0, in1=gtt[gb][:], op0=ALU.add, op1=ALU.mult),
                        reads=[("ubt", gb), ("gtt", gb)], writes=[("hT", par, sgi, fc)])

                for n in range(len(groups) + 1):
                    if n < len(groups):
                        A(n)
                    if n >= 1:
                        B(n - 1)

            def stage_DN(e_):
                par = e_ % 2
                hkeys = [("hT", par, sgi, fc) for sgi in range(2) for fc in range(8)]
                for blk in range(NB):
                    yb = c4["y"] % 2
                    c4["y"] += 1
                    for hf in range(2):
                        for fc in range(8):
                            sc.add("pe", lambda e, hf=hf, fc=fc, blk=blk: e.matmul(
                                out=pY[hf][:], lhsT=hT[par][:, fc, blk * 128:(blk + 1) * 128],
                                rhs=wdn[par][:, fc, hf * 512:(hf + 1) * 512], start=(fc == 0), stop=False),
                                reads=hkeys + [("wdn", par)], writes=[("pY", hf)])
                        sc.add("pe", lambda e, hf=hf: e.matmul(
                            out=pY[hf][:], lhsT=onesr[0:1, :], rhs=bdr[par][0:1, hf * 512:(hf + 1) * 512], start=False, stop=True),
                            reads=["onesr", ("bdr", par)], writes=[("pY", hf)])
                        fa = lambda e, hf=hf, yb=yb: e.copy(out=ysb[yb][:, hf * 512:(hf + 1) * 512], in_=pY[hf][:])
                        fd = lambda e, hf=hf, yb=yb: e.tensor_copy(out=ysb[yb][:, hf * 512:(hf + 1) * 512], in_=pY[hf][:])
                        sc.add("act" if hf == 0 else "dve", fa if hf == 0 else fd, reads=[("pY", hf)], writes=[("ysb", yb, hf)])
                    r0 = e_ * CAP + blk * 128
                    sc.add("sp", lambda e, yb=yb, r0=r0: e.dma_start(out=ys_d[r0:r0 + 128, :], in_=ysb[yb][:]),
                           reads=[("ysb", yb, 0), ("ysb", yb, 1)], dma=True)

            load_xs(0)
            stage_T(0)
            for e_ in range(n_exp):
                if e_ + 1 < n_exp and not (NOLOAD and e_ + 1 >= 2):
                    load_expert(e_ + 1)
                if e_ + 1 < n_exp:
                    load_xs(e_ + 1)
                stage_GU(e_)
                if e_ + 1 < n_exp:
                    stage_T(e_ + 1)
                stage_DN(e_)
                if e_ + 1 < n_exp and not (NOLOAD and e_ + 1 >= 2):
                    cast_dn(e_ + 1)
            sc.barrier()

        with ExitStack() as p5:
            ln2 = sb("ln2", [128, 2, 1024], F32, p5)
            G = [[sb("G%d_%d" % (k, i), [128, D], BF16, p5) for i in range(2)] for k in range(4)]
            yr = [sb("yr%d" % i, [128, D], F32, p5) for i in range(2)]
            acc = sb("acc5", [128, D], F32, p5)
            z2 = sb("z2", [128, D], F32, p5)
            tn = sb("tn", [128, D], F32, p5)
            ot = [sb("ot%d" % i, [128, D], F32, p5) for i in range(2)]
            stats2 = sb("stats2", [128, 12], F32, p5)
            mv2 = sb("mv2", [128, 8], F32, p5)
            dg = [sb("dg%d" % i, [128, 4, 128], BF16, p5) for i in range(2)]
            pF = [[ps("pF%d_%d" % (i, h), [128, 512], F32, p5) for h in range(2)] for i in range(2)]
            sc.add("sp", lambda e: e.dma_start(out=ln2[:].rearrange("p a d -> p (a d)"), in_=lnp_d[:, 2048:4096]), writes=["ln2"], dma=True)
            for k in range(4):
                for i in range(2):
                    sc.add("pool", lambda e, k=k, i=i: e.memset(G[k][i][:], 0.0), writes=[("G", k, i)])
            def loads5(i):
                b = i % 2
                tsl = slice(i * 128, (i + 1) * 128)
                sc.add("sp", lambda e: e.dma_start(out=yr[b][:], in_=y32_d[tsl, :]), writes=[("yr", b)], dma=True)
                for k in range(4):
                    sc.add("pool", lambda e, k=k: e.indirect_dma_start(
                        out=G[k][b][:, :], out_offset=None, in_=ys_d[:, :],
                        in_offset=bass.IndirectOffsetOnAxis(ap=idx_all[:, i * 4 + k:i * 4 + k + 1], axis=0),
                        bounds_check=bcreg(e), oob_is_err=False),
                        writes=[("G", k, b)], dma=True)

            z2b = [z2, sb("z2b", [128, D], F32, p5)]
            tnb = [tn, sb("tnb", [128, D], F32, p5)]
            stb = [stats2, sb("stats2b", [128, 12], F32, p5)]
            mvb = [mv2, sb("mv2b", [128, 8], F32, p5)]

            def P5(i):
                b = i % 2
                for k in range(4):
                    sc.add("dve", lambda e, k=k: e.tensor_scalar(
                        out=dg[b][:, k, :], in0=identb[:], scalar1=gate_all[:, i * 4 + k:i * 4 + k + 1], scalar2=None,
                        op0=ALU.mult), writes=[("dg", b)])
                for hf in range(2):
                    for k in range(4):
                        sc.add("pe", lambda e, hf=hf, k=k: e.matmul(
                            out=pF[b][hf][:], lhsT=dg[b][:, k, :], rhs=G[k][b][:, hf * 512:(hf + 1) * 512],
                            start=(k == 0), stop=(k == 3)),
                            reads=[("dg", b), ("G", k, b)], writes=[("pF", b, hf)])
                    sc.add("dve", lambda e, hf=hf: e.scalar_tensor_tensor(
                        out=z2b[b][:, hf * 512:(hf + 1) * 512], in0=yr[b][:, hf * 512:(hf + 1) * 512], scalar=float(ALPHA),
                        in1=pF[b][hf][:], op0=ALU.mult, op1=ALU.add),
                        reads=[("yr", b), ("pF", b, hf)], writes=[("z2", b)])
                ln_stats(("b", b), z2b[b], ("z2", b), stb[b], mvb[b])

            def Q5(i):
                b = i % 2
                tsl = slice(i * 128, (i + 1) * 128)
                ln_apply(("b", b), z2b[b], ("z2", b), ln2[:, 0, :], ln2[:, 1, :], ot[b][:], ("ot", b), mvb[b], tnb[b])
                sc.add("sp", lambda e: e.dma_start(out=out[tsl, :], in_=ot[b][:]), reads=[("ot", b)], dma=True)

            loads5(0)
            for i in range(NT):
                if i + 1 < NT:
                    loads5(i + 1)
                P5(i)
                if i >= 1:
                    Q5(i - 1)
            Q5(NT - 1)

        sc.emit(es)
    return nc


_NC_CACHE = {}


def kernel(**inputs):
    stage = inputs.pop("_stage", os.environ.get("KSTAGE", "full"))
    if stage not in _NC_CACHE:
        _NC_CACHE[stage] = build(stage)
    nc = _NC_CACHE[stage]
    f = lambda k: np.ascontiguousarray(np.asarray(inputs[k], dtype=np.float32)[0])
    x = np.ascontiguousarray(inputs["x"], dtype=np.float32)
    ident, negd = host_consts()
    sinks = f("sinks").reshape(16)
    sinkc = np.zeros((128, 8), dtype=np.float32)
    for p in range(8):
        sinkc[:64, p] = sinks[2 * p]
        sinkc[64:, p] = sinks[2 * p + 1]
    lnp = np.concatenate([np.broadcast_to(f(k)[None, :], (128, D)) for k in ("ln1_g", "ln1_b", "ln2_g", "ln2_b")], axis=1)
    rb = np.broadcast_to(f("router_b")[None, :], (128, 32))
    bgu = np.concatenate([f("b_gate").reshape(32, 8, 128).transpose(2, 0, 1).reshape(128, 256),
                          f("b_up").reshape(32, 8, 128).transpose(2, 0, 1).reshape(128, 256)], axis=1)
    cst = np.zeros((128, 320), dtype=np.float32)
    cst[:, 0:128] = np.triu(np.ones((128, 128), dtype=np.float32), 1)
    cst[:, 128:256] = 1.0
    cst[:, 256:288] = np.arange(32, dtype=np.float32)[None, :]
    cst[:, 288:320] = (np.arange(32, dtype=np.float32) * CAP)[None, :]
    shared = {"ident": ident, "negd": negd, "sinkc": sinkc, "w_in": f("w_in"),
              "w_proj_a": f("w_proj_a"), "w_proj_b": f("w_proj_b"), "w_out": f("w_out"),
              "lnp": np.ascontiguousarray(lnp), "router_w": f("router_w"), "rb": np.ascontiguousarray(rb),
              "cst": cst, "w_gate": f("w_gate"), "w_up": f("w_up"), "w_down": f("w_down"),
              "bgu": np.ascontiguousarray(bgu), "b_down": f("b_down")}
    names = set(_IN_NAMES.get(stage, shared.keys()))
    in_maps = []
    for c in range(NCORES):
        m = {k: v for k, v in shared.items() if k in names}
        m["x"] = x[c]
        in_maps.append(m)
    res = run_bass_kernel_spmd(nc, in_maps, core_ids=list(range(NCORES)))
    if "KSTAGE" in os.environ and stage != "full":
        return np.zeros((NCORES, S, D), dtype=np.float32)
    if stage != "full":
        return res
    return np.stack([np.asarray(r["out"]) for r in res.results], axis=0)


_IN_NAMES = {}
```

```python
import numpy as np
from contextlib import ExitStack
import concourse.bass as bass
import concourse.mybir as mybir
from concourse.bass_utils import run_bass_kernel_spmd

F32 = mybir.dt.float32
BF16 = mybir.dt.bfloat16
I32 = mybir.dt.int32
U32 = mybir.dt.uint32
AF = mybir.ActivationFunctionType
ALU = mybir.AluOpType
AX = mybir.AxisListType

S = 4096
D = 1024
NT = S // 128
NCORES = 8
ALPHA = 2.0 ** 0.25
LN_EPS = 1e-5
NEG = -1.0e30
NE = 32
CAP = 640
NSLOT = NE * CAP
IN_COLS = 5632
C_AQ, C_AK, C_AV, C_BQ, C_BK, C_BV, C_GA, C_GB = 0, 768, 1536, 2304, 3328, 3456, 3584, 4608
SLOPES = [2.0 ** (-8.0 * (h + 1) / 28.0) for h in range(28)]
MASKS = [(127, 1), (128, 1), (128, 4), (128, 16)]
DIL = [1, 1, 4, 16]

import os
ATT = int(os.environ.get("ATT", "9"))
NOLOAD = os.environ.get("KPROBE", "") == "noload"
COMPUTE = ("pe", "act", "dve", "pool")
QUEUES = ("sp", "pool", "act")
KDMA = 16


class Op:
    __slots__ = ("eng", "fn", "dma", "deps", "inc", "sem", "semval", "slotwait", "name", "idx")


class Sched:
    def __init__(self, nc, es):
        self.nc = nc
        self.streams = {e: [] for e in ("pe", "act", "dve", "pool", "sp")}
        self.last_w = {}
        self.readers = {}
        self.csem = {e: es.enter_context(nc.semaphore("s_" + e)) for e in COMPUTE}
        self.dsem = {q: [es.enter_context(nc.semaphore("d_%s%d" % (q, i))) for i in range(KDMA)]
                     for q in QUEUES}
        self.dma_n = {q: 0 for q in QUEUES}
        self.dma_last = {}
        self.last_c = {}

    def add(self, eng, fn, reads=(), writes=(), dma=False, name=None):
        op = Op()
        op.eng, op.fn, op.dma, op.inc, op.name = eng, fn, dma, False, name
        op.sem = None
        op.semval = 0
        op.slotwait = None
        deps = set()
        for r in reads:
            w = self.last_w.get(r)
            if w is not None:
                deps.add(w)
        for k in writes:
            w = self.last_w.get(k)
            if w is not None:
                deps.add(w)
            for rd in self.readers.get(k, ()):
                deps.add(rd)
        if eng == "pe":
            deps = {d for d in deps if not (d.eng == "pe" and not d.dma)}
        latest = {}
        red = set()
        for d in deps:
            if d.dma:
                red.add(d)
            elif d.eng not in latest or latest[d.eng].idx < d.idx:
                latest[d.eng] = d
        red.update(latest.values())
        op.deps = red
        op.idx = len(self.streams[eng])
        for r in reads:
            self.readers.setdefault(r, []).append(op)
        for k in writes:
            self.last_w[k] = op
            self.readers[k] = []
        if dma:
            n = self.dma_n[eng]
            self.dma_n[eng] = n + 1
            op.sem = self.dsem[eng][n % KDMA]
            op.semval = 16 * (n // KDMA + 1)
            if n >= KDMA:
                op.slotwait = (op.sem, 16 * (n // KDMA))
            self.dma_last[(eng, n % KDMA)] = op
        else:
            self.last_c[eng] = op
        self.streams[eng].append(op)
        return op

    def barrier(self):
        snap = set(self.last_c.values()) | set(self.dma_last.values())
        for e in self.streams:
            op = Op()
            op.eng, op.fn, op.dma, op.inc, op.name = e, None, False, False, "barrier"
            op.sem, op.semval, op.slotwait = None, 0, None
            op.deps = set(snap)
            op.idx = len(self.streams[e])
            self.streams[e].append(op)
        self.last_w = {}
        self.readers = {}

    def emit(self, es):
        nc = self.nc
        for st in self.streams.values():
            for op in st:
                for d in op.deps:
                    d.inc = True
        for e, st in self.streams.items():
            cnt = 0
            for op in st:
                if (not op.dma) and op.inc and op.fn is not None:
                    cnt += 1
                    op.sem = self.csem[e]
                    op.semval = cnt
        block = es.enter_context(nc.Block())

        def body(ename):
            def run(e):
                seen = {}
                for op in self.streams[ename]:
                    waits = {}
                    if op.slotwait is not None:
                        waits[id(op.slotwait[0])] = op.slotwait
                    for d in op.deps:
                        if d.sem is None:
                            continue
                        k = id(d.sem)
                        if k not in waits or waits[k][1] < d.semval:
                            waits[k] = (d.sem, d.semval)
                    for k, (s, v) in waits.items():
                        if seen.get(k, 0) < v:
                            e.wait_ge(s, v)
                            seen[k] = v
                    if op.fn is None:
                        continue
                    inst = op.fn(e)
                    if op.dma:
                        inst.then_inc(op.sem, 16)
                    elif op.inc:
                        inst.then_inc(op.sem, 1)
                for (q, i), dop in self.dma_last.items():
                    if q == ename and seen.get(id(dop.sem), 0) < dop.semval:
                        e.wait_ge(dop.sem, dop.semval)
            return run

        block.tensor(body("pe"))
        block.scalar(body("act"))
        block.vector(body("dve"))
        block.gpsimd(body("pool"))
        block.sync(body("sp"))


def host_consts():
    ident = np.eye(128, dtype=np.float32)
    nd = np.zeros((128, 4, 2, 128), dtype=np.float32)
    k = np.arange(128)[:, None]
    q = np.arange(128)[None, :]
    for m, (maxd, scale) in enumerate(MASKS):
        for kb in range(2):
            diff = q - k + (128 if kb == 0 else 0)
            valid = (diff >= 0) & (diff <= maxd)
            nd[:, m, kb, :] = np.where(valid, -8.0 * scale * diff, NEG)
    return ident, nd.reshape(128, 4 * 256)


def build(stage="full"):
    nc = bass.Bass("TRN2", target_bir_lowering=False)
    x = nc.dram_tensor("x", [S, D], F32, kind="ExternalInput").ap()
    w_in = nc.dram_tensor("w_in", [D, IN_COLS], F32, kind="ExternalInput").ap()
    ident_d = nc.dram_tensor("ident", [128, 128], F32, kind="ExternalInput").ap()
    negd_d = nc.dram_tensor("negd", [128, 1024], F32, kind="ExternalInput").ap()
    sinkc_d = nc.dram_tensor("sinkc", [128, 8], F32, kind="ExternalInput").ap()
    out = nc.dram_tensor("out", [S, D], F32, kind="ExternalOutput").ap()
    oT_d = nc.dram_tensor("oT_scratch", [10, 128, S], BF16, kind="ExternalOutput").ap()
    w_in_v = w_in.rearrange("(dc p) c -> p dc c", p=128)
    wpa_d = nc.dram_tensor("w_proj_a", [256, D], F32, kind="ExternalInput").ap()
    wpb_d = nc.dram_tensor("w_proj_b", [D, D], F32, kind="ExternalInput").ap()
    wo_d = nc.dram_tensor("w_out", [D, D], F32, kind="ExternalInput").ap()
    lnp_d = nc.dram_tensor("lnp", [128, 4 * D], F32, kind="ExternalInput").ap()
    rw_d = nc.dram_tensor("router_w", [D, 32], F32, kind="ExternalInput").ap()
    rb_d = nc.dram_tensor("rb", [128, 32], F32, kind="ExternalInput").ap()
    cst_d = nc.dram_tensor("cst", [128, 320], F32, kind="ExternalInput").ap()
    wgate_d = nc.dram_tensor("w_gate", [32, D, D], F32, kind="ExternalInput").ap()
    wup_d = nc.dram_tensor("w_up", [32, D, D], F32, kind="ExternalInput").ap()
    wdown_d = nc.dram_tensor("w_down", [32, D, D], F32, kind="ExternalInput").ap()
    bgu_d = nc.dram_tensor("bgu", [128, 512], F32, kind="ExternalInput").ap()
    bdn_d = nc.dram_tensor("b_down", [32, D], F32, kind="ExternalInput").ap()
    y32_d = nc.dram_tensor("y32_scratch", [S, D], F32, kind="ExternalOutput").ap()
    xs_d = nc.dram_tensor("xs_scratch", [NSLOT, D], BF16, kind="ExternalOutput").ap()
    ys_d = nc.dram_tensor("ys_scratch", [NSLOT, D], BF16, kind="ExternalOutput").ap()
    es = ExitStack()
    with es:
        sc = Sched(nc, es)

        def sb(name, shape, dt, st=es):
            return st.enter_context(nc.sbuf_tensor("sb_" + name, shape, dt))

        def ps(name, shape, dt, st=es):
            return st.enter_context(nc.psum_tensor("ps_" + name, shape, dt))

        ident = sb("ident", [128, 128], F32)
        identb = sb("identb", [128, 128], BF16)
        negd = sb("negd", [128, 4, 256], F32)
        onesb = sb("onesb", [128, 64], BF16)
        esink = sb("esink", [128, 8], F32)
        idx_all = sb("idx_all", [128, NT * 4], I32)
        gate_all = sb("gate_all", [128, NT * 4], F32)
        cst = sb("cst", [128, 320], F32)
        utri, ones_f, iota_e, ebase = cst[:, 0:128], cst[:, 128:256], cst[:, 256:288], cst[:, 288:320]
        zfill = sb("zfill", [128, 2048], BF16)
        sc.add("dve", lambda e: e.memset(zfill[:], 0.0), writes=["zfill"])
        pX = ExitStack()
        xT = sb("xT", [128, 8, S], BF16, pX)

        sc.add("sp", lambda e: e.dma_start(out=ident[:], in_=ident_d), writes=["ident"], dma=True)
        sc.add("sp", lambda e: e.dma_start(out=negd[:].rearrange("p m c -> p (m c)"), in_=negd_d),
               writes=["negd"], dma=True)
        sc.add("sp", lambda e: e.dma_start(out=esink[:], in_=sinkc_d), writes=["esink"], dma=True)
        sc.add("dve", lambda e: e.tensor_copy(out=identb[:], in_=ident[:]), reads=["ident"], writes=["identb"])
        sc.add("dve", lambda e: e.memset(onesb[:], 1.0), writes=["onesb"])
        sc.add("act", lambda e: e.activation(out=esink[:], in_=esink[:], func=AF.Exp),
               reads=["esink"], writes=["esink"])

        with ExitStack() as p0:
            xin = [sb("xin%d" % i, [128, D], F32, p0) for i in range(2)]
            pst = [ps("pst%d" % i, [128, 512], F32, p0) for i in range(4)]
            for i in range(NT):
                b = i % 2
                sc.add("sp", lambda e, i=i, b=b: e.dma_start(out=xin[b][:], in_=x[i * 128:(i + 1) * 128, :]),
                       writes=[("xin", b)], dma=True)
                for h in range(2):
                    pb = (2 * i + h) % 4
                    for c4 in range(4):
                        c = h * 4 + c4
                        sc.add("pe", lambda e, b=b, c=c, c4=c4, pb=pb: e.transpose(
                            out=pst[pb][:, c4 * 128:(c4 + 1) * 128], in_=xin[b][:, c * 128:(c + 1) * 128],
                            identity=ident[:]),
                            reads=[("xin", b), "ident"], writes=[("pst", pb)])
                    if h == 0:
                        fn = lambda e, i=i, h=h, pb=pb: e.copy(
                            out=xT[:, h * 4:(h + 1) * 4, i * 128:(i + 1) * 128],
                            in_=pst[pb][:].rearrange("p (c t) -> p c t", c=4))
                    else:
                        fn = lambda e, i=i, h=h, pb=pb: e.tensor_copy(
                            out=xT[:, h * 4:(h + 1) * 4, i * 128:(i + 1) * 128],
                            in_=pst[pb][:].rearrange("p (c t) -> p c t", c=4))
                    sc.add("act" if h == 0 else "dve", fn, reads=[("pst", pb)], writes=[("xT", i)])
            sc.barrier()

        with ExitStack() as p1:
            QT = [sb("QT%d" % i, [128, S], BF16, p1) for i in range(2)]
            KTd = sb("KTd", [128, S], BF16, p1)
            Vd = sb("Vd", [128, NT, 128], BF16, p1)
            KTs = [sb("KTs%d" % i, [128, S], BF16, p1) for i in range(2)]
            Vs = sb("Vs", [128, NT, 128], BF16, p1)
            accN = sb("accN", [128, S], F32, p1)
            accD = sb("accD", [128, S], F32, p1)
            wq = [sb("wq%d" % i, [128, 8, 128], BF16, p1) for i in range(2)]
            wk = [sb("wk%d" % i, [128, 8, 128], BF16, p1) for i in range(2)]
            wv = [sb("wv%d" % i, [128, 8, 128], BF16, p1) for i in range(2)]
            tmp = [sb("tmp%d" % i, [128, 512], F32, p1) for i in range(3)]
            PT = [sb("PT%d" % i, [128, 512], BF16, p1) for i in range(3)]
            bias2 = [sb("bias2_%d" % i, [128, 2, 256], F32, p1) for i in range(2)]
            stg = [sb("stg%d" % i, [128, 512], BF16, p1) for i in range(2)]
            t1 = [sb("t1_%d" % i, [128, 512], F32, p1) for i in range(2)]
            pSall = ps("pSall", [128, 6, 512], F32, p1)
            pND = [ps("pND%d" % i, [128, 512], F32, p1) for i in range(2)]
            pj = pND
            cnt = {"pj": 0, "ev": 0, "blk": 0, "stg": 0, "zf": 0}

            def load_w(dst, key, c0, ncols=128, d0=0):
                sc.add("pool", lambda e: e.dma_start(out=dst[:, :, d0:d0 + ncols], in_=w_in_v[:, :, c0:c0 + ncols]),
                       writes=[key], dma=True)

            def evac(fn_act, fn_dve, reads, writes):
                k = cnt["ev"]
                cnt["ev"] += 1
                if k % 2 == 0:
                    sc.add("act", fn_act, reads=reads, writes=writes)
                else:
                    sc.add("dve", fn_dve, reads=reads, writes=writes)

            def proj_fm(w, wkey, dst, dkey, r):
                for tg in range(8):
                    b = cnt["pj"] % 2
                    cnt["pj"] += 1
                    for dc in range(8):
                        sc.add("pe", lambda e, b=b, dc=dc, tg=tg: e.matmul(
                            out=pj[b][:], lhsT=w[:, dc, :], rhs=xT[:, dc, tg * 512:(tg + 1) * 512],
                            start=(dc == 0), stop=(dc == 7)),
                            reads=[wkey], writes=[("pND", b)])
                    if r == 1:
                        o_ap = lambda tg=tg: dst[:, tg * 512:(tg + 1) * 512]
                        i_ap = lambda b=b: pj[b][:]
                    else:
                        n = 512 // r
                        o_ap = lambda tg=tg, n=n: dst[:].rearrange("p (r m) -> p r m", r=r)[:, :, tg * n:(tg + 1) * n]
                        i_ap = lambda b=b: pj[b][:].rearrange("p (m r) -> p r m", r=r)
                    evac(lambda e, o_ap=o_ap, i_ap=i_ap: e.copy(out=o_ap(), in_=i_ap()),
                         lambda e, o_ap=o_ap, i_ap=i_ap: e.tensor_copy(out=o_ap(), in_=i_ap()),
                         reads=[("pND", b)], writes=[(dkey, tg)])

            def proj_tm(w, wkey, dst, dkey, r):
                nbs = NT // r
                for b4 in range(NT // 4):
                    b = cnt["pj"] % 2
                    cnt["pj"] += 1
                    for k4 in range(4):
                        qb = b4 * 4 + k4
                        res, j = qb // nbs, qb % nbs
                        t0 = res + r * 128 * j
                        for dc in range(8):
                            sc.add("pe", lambda e, b=b, dc=dc, k4=k4, t0=t0: e.matmul(
                                out=pj[b][:, k4 * 128:(k4 + 1) * 128],
                                lhsT=xT[:, dc, t0:t0 + 127 * r + 1:r], rhs=w[:, dc, :],
                                start=(dc == 0), stop=(dc == 7)),
                                reads=[wkey], writes=[("pND", b)])
                    evac(lambda e, b=b, b4=b4: e.copy(out=dst[:, b4 * 4:(b4 + 1) * 4, :],
                                                     in_=pj[b][:].rearrange("p (k c) -> p k c", k=4)),
                         lambda e, b=b, b4=b4: e.tensor_copy(out=dst[:, b4 * 4:(b4 + 1) * 4, :],
                                                            in_=pj[b][:].rearrange("p (k c) -> p k c", k=4)),
                         reads=[("pND", b)], writes=[(dkey, b4)])

            def attention(g, Qb, qkeys, Kb, kkeys, Vb, vkey, vcol, slopes2, on_done, par):
                r = DIL[g]
                nbs = NT // r
                for hh in range(2):
                    sc.add("dve", lambda e, hh=hh: e.tensor_scalar(
                        out=bias2[par][:, hh, :], in0=negd[:, g, :], scalar1=float(slopes2[hh]), scalar2=None, op0=ALU.mult),
                        writes=[("bias2", par)])

                def scores(qb):
                    j = qb % nbs
                    sb_ = qb % 3
                    kbs = (0, 1) if j > 0 else (1,)
                    c0 = 0 if j > 0 else 128
                    for hh in range(2):
                        for kb in kbs:
                            kblk = qb - 1 + kb
                            sc.add("pe", lambda e, hh=hh, kb=kb, kblk=kblk: e.matmul(
                                out=pSall[:, 2 * sb_ + hh, kb * 128:(kb + 1) * 128],
                                lhsT=Kb[hh * 64:(hh + 1) * 64, kblk * 128:(kblk + 1) * 128],
                                rhs=Qb[hh * 64:(hh + 1) * 64, qb * 128:(qb + 1) * 128],
                                start=True, stop=True),
                                reads=list(qkeys) + list(kkeys), writes=[("pS", sb_, hh)])
                    t3 = lambda ap: ap[:].rearrange("p (h c) -> p h c", h=2)[:, :, c0:256]
                    sc.add("dve", lambda e: e.tensor_tensor(
                        out=t3(tmp[sb_]), in0=pSall[:, 2 * sb_:2 * sb_ + 2, c0:256], in1=bias2[par][:, :, c0:256], op=ALU.add),
                        reads=[("pS", sb_, 0), ("pS", sb_, 1), ("bias2", par)], writes=[("tmp", sb_)])
                    sc.add("act", lambda e: e.activation(out=t3(PT[sb_]), in_=t3(tmp[sb_]), func=AF.Exp, scale=0.125),
                           reads=[("tmp", sb_)], writes=[("PT", sb_)])

                def pv(qb):
                    j = qb % nbs
                    sb_ = qb % 3
                    kbs = (0, 1) if j > 0 else (1,)
                    b2i, k2 = qb // 2, qb % 2
                    pb = b2i % 2
                    for hh in range(2):
                        for (col0, isnum) in ((k2 * 128, True), (256 + k2 * 128, False)):
                            for ki, kb in enumerate(kbs):
                                kblk = qb - 1 + kb
                                lh = (lambda kblk=kblk, hh=hh: Vb[:, kblk, vcol[hh]:vcol[hh] + 64]) if isnum \
                                    else (lambda: onesb[:, 0:64])
                                rd = [("PT", sb_)] + ([(vkey, kblk // 4)] if isnum else [])
                                sc.add("pe", lambda e, hh=hh, kb=kb, ki=ki, col0=col0, lh=lh: e.matmul(
                                    out=pND[pb][hh * 64:(hh + 1) * 64, col0:col0 + 128],
                                    lhsT=lh(),
                                    rhs=PT[sb_][:, (hh * 2 + kb) * 128:(hh * 2 + kb + 1) * 128],
                                    start=(ki == 0), stop=(ki == len(kbs) - 1)),
                                    reads=rd, writes=[("pND", pb)])
                    if k2 == 1:
                        on_done(b2i, pb)

                for qb in range(NT + 2):
                    if qb < NT:
                        scores(qb)
                    if qb >= 2:
                        pv(qb - 2)

            ZCH = (NSLOT // 128) * D // 2048
            xs_flat = xs_d.rearrange("(p r) d -> p (r d)", p=128)

            def store_stage(chunk, t0, sbuf_i):
                sc.add("sp", lambda e: e.dma_start(out=oT_d[chunk, :, t0:t0 + 512], in_=stg[sbuf_i][:]),
                       reads=[("stg", sbuf_i)], dma=True)
                zi = cnt["zf"]
                if zi < ZCH:
                    cnt["zf"] += 1
                    sc.add("sp", lambda e: e.dma_start(out=xs_flat[:, zi * 2048:(zi + 1) * 2048], in_=zfill[:]),
                           reads=["zfill"], dma=True)

            for kv in range(2):
                load_w(wk[kv], ("wk", kv), C_BK + kv * 64, 64, 0)
                load_w(wk[kv], ("wk", kv), C_BK + kv * 64, 64, 64)
            load_w(wv[0], ("wv", 0), C_BV, 128)
            for kv in range(2):
                proj_fm(wk[kv], ("wk", kv), KTs[kv], ("KTs", kv), 1)
            proj_tm(wv[0], ("wv", 0), Vs, "Vs", 1)

            if stage == "p1a":
                dbg = sb("dbg", [128, S], F32, p1)
                for ci, (src, keys) in enumerate([(KTs[0][:], [(("KTs", 0), tg) for tg in range(8)]),
                                                  (KTs[1][:], [(("KTs", 1), tg) for tg in range(8)]),
                                                  (Vs[:].rearrange("p b c -> p (b c)"), [("Vs", b4) for b4 in range(8)])]):
                    sc.add("dve", lambda e, src=src: e.tensor_copy(out=dbg[:], in_=src), reads=keys, writes=["dbg"])
                    sc.add("sp", lambda e, ci=ci: e.dma_start(
                        out=out.rearrange("(r q) d -> r (q d)", q=4)[ci * 128:(ci + 1) * 128, :], in_=dbg[:]),
                        reads=["dbg"], dma=True)
            jobs = [(0, p) for p in range(8)] + [(g, pp) for pp in range(2) for g in (1, 2, 3)]
            if stage == "p1a":
                jobs = []
            if stage == "swa0":
                jobs = [(0, 0), (0, 5)]
            if stage == "dil0":
                jobs = [(1, 0), (2, 0), (3, 0)]
            for ji, (g, p) in enumerate(jobs):
                par = ji % 2
                r = DIL[g]
                if g == 0:
                    load_w(wq[par], ("wq", par), C_BQ + p * 128)
                    proj_fm(wq[par], ("wq", par), QT[par], ("Q", par), 1)
                    kv = p // 4
                    sl = (SLOPES[2 * p], SLOPES[2 * p + 1])

                    def done(b2i, pb, p=p):
                        b4, half = b2i // 2, b2i % 2
                        si = b4 % 2
                        hs = slice(half * 256, (half + 1) * 256)
                        sc.add("dve", lambda e: e.tensor_scalar(
                            out=t1[si][:, hs], in0=pND[pb][:, 256:512], scalar1=esink[:, p:p + 1], scalar2=None, op0=ALU.add),
                            reads=[("pND", pb)], writes=[("t1", si, half)])
                        sc.add("dve", lambda e: e.reciprocal(out=t1[si][:, hs], in_=t1[si][:, hs]),
                               reads=[("t1", si, half)], writes=[("t1", si, half)])
                        sc.add("dve", lambda e: e.tensor_tensor(
                            out=stg[si][:, hs], in0=pND[pb][:, 0:256], in1=t1[si][:, hs], op=ALU.mult),
                            reads=[("pND", pb), ("t1", si, half)], writes=[("stg", si)])
                        if half == 1:
                            store_stage(p, b4 * 512, si)

                    attention(0, QT[par], [(("Q", par), tg) for tg in range(8)],
                              KTs[kv], [(("KTs", kv), tg) for tg in range(8)],
                              Vs, "Vs", (kv * 64, kv * 64), sl, done, par)
                else:
                    c0 = (g - 1) * 256 + p * 128
                    load_w(wq[par], ("wq", par), C_AQ + c0)
                    load_w(wk[par], ("wk", par), C_AK + c0)
                    load_w(wv[par], ("wv", par), C_AV + c0)
                    proj_fm(wq[par], ("wq", par), QT[par], ("Q", par), r)
                    proj_fm(wk[par], ("wk", par), KTd, "KTd", r)
                    proj_tm(wv[par], ("wv", par), Vd, "Vd", r)
                    sl = (SLOPES[16 + (g - 1) * 4 + 2 * p], SLOPES[16 + (g - 1) * 4 + 2 * p + 1])
                    nbs = NT // r

                    def done(b2i, pb, g=g, r=r, nbs=nbs):
                        qb0 = b2i * 2
                        res, j0 = qb0 // nbs, qb0 % nbs
                        n, t0 = 256, res + r * 128 * j0
                        halves = sorted({(t0 + r * i) // 2048 for i in (0, n - 1)})
                        keys = [("acc", h) for h in halves]
                        for acc, pc in ((accN, 0), (accD, 256)):
                            if g == 1:
                                sc.add("act", lambda e, acc=acc, pc=pc: e.copy(
                                    out=acc[:, t0:t0 + (n - 1) * r + 1:r], in_=pND[pb][:, pc:pc + n]),
                                    reads=[("pND", pb)], writes=keys)
                            else:
                                sc.add("dve", lambda e, acc=acc, pc=pc: e.tensor_tensor(
                                    out=acc[:, t0:t0 + (n - 1) * r + 1:r], in0=pND[pb][:, pc:pc + n],
                                    in1=acc[:, t0:t0 + (n - 1) * r + 1:r], op=ALU.add),
                                    reads=[("pND", pb)] + keys, writes=keys)

                    attention(g, QT[par], [(("Q", par), tg) for tg in range(8)],
                              KTd, [("KTd", tg) for tg in range(8)],
                              Vd, "Vd", (0, 64), sl, done, par)
                    if g == 3:
                        for t8 in range(8):
                            si = t8 % 2
                            keys = [("acc", t8 // 4)]
                            sc.add("dve", lambda e, t8=t8, si=si: e.reciprocal(
                                out=t1[si][:], in_=accD[:, t8 * 512:(t8 + 1) * 512]),
                                reads=keys, writes=[("t1", si, 0), ("t1", si, 1)])
                            sc.add("dve", lambda e, t8=t8, si=si: e.tensor_tensor(
                                out=stg[si][:], in0=accN[:, t8 * 512:(t8 + 1) * 512], in1=t1[si][:], op=ALU.mult),
                                reads=keys + [("t1", si, 0), ("t1", si, 1)], writes=[("stg", si)])
                            store_stage(8 + p, t8 * 512, si)
            sc.barrier()


        if stage == "p01":
            pX.close()
            sc.emit(es)
            return nc
        if stage in ("swa0", "dil0", "attn"):
            with ExitStack() as pd:
                dbgb = sb("dbgb", [128, S], BF16, pd)
                dbg = sb("dbg", [128, S], F32, pd)
                chunks = {"swa0": [0, 5], "dil0": [8], "attn": list(range(8))}[stage]
                for ci, c in enumerate(chunks):
                    sc.add("sp", lambda e, c=c: e.dma_start(out=dbgb[:], in_=oT_d[c, :, :]), writes=["dbgb"], dma=True)
                    sc.add("dve", lambda e: e.tensor_copy(out=dbg[:], in_=dbgb[:]), reads=["dbgb"], writes=["dbg"])
                    sc.add("sp", lambda e, ci=ci: e.dma_start(
                        out=out.rearrange("(r q) d -> r (q d)", q=4)[ci * 128:(ci + 1) * 128, :], in_=dbg[:]),
                        reads=["dbg"], dma=True)
            sc.emit(es)
            return nc

        _regs = {}

        def bcreg(e):
            if "bc" not in _regs:
                r = e.alloc_register("bc")
                e.reg_mov(r, NSLOT - 1)
                _regs["bc"] = r
            return _regs["bc"]

        def ln_stats(p, zt, zkey, stats, mv):
            for h in range(2):
                sc.add("dve", lambda e, h=h: e.bn_stats(out=stats[:, h * 6:(h + 1) * 6], in_=zt[:, h * 512:(h + 1) * 512]),
                       reads=[zkey], writes=[("stats", p)])
            sc.add("dve", lambda e: e.bn_aggr(out=mv[:, 0:2], in_=stats[:, 0:12]), reads=[("stats", p)], writes=[("mv", p)])
            sc.add("dve", lambda e: e.tensor_scalar(out=mv[:, 2:3], in0=mv[:, 1:2], scalar1=LN_EPS, scalar2=None, op0=ALU.add),
                   reads=[("mv", p)], writes=[("mv", p)])
            sc.add("act", lambda e: e.activation(out=mv[:, 2:3], in_=mv[:, 2:3], func=AF.Sqrt),
                   reads=[("mv", p)], writes=[("mv", p)])
            sc.add("dve", lambda e: e.reciprocal(out=mv[:, 3:4], in_=mv[:, 2:3]), reads=[("mv", p)], writes=[("mv", p)])
            sc.add("dve", lambda e: e.scalar_tensor_tensor(out=mv[:, 4:5], in0=mv[:, 0:1], scalar=-1.0, in1=mv[:, 3:4],
                                                          op0=ALU.mult, op1=ALU.mult),
                   reads=[("mv", p)], writes=[("mv", p)])

        def ln_apply(p, zt, zkey, gam, bet, dst, dkey, mv, tmpn, tkeys=None, bias_eng="pool"):
            tk = list(tkeys) if tkeys is not None else [("tmpn", p)]
            sc.add("act", lambda e: e.activation(out=tmpn[:], in_=zt[:], func=AF.Identity, scale=mv[:, 3:4], bias=mv[:, 4:5]),
                   reads=[zkey, ("mv", p)], writes=tk)
            sc.add("dve", lambda e: e.tensor_tensor(out=tmpn[:], in0=tmpn[:], in1=gam, op=ALU.mult),
                   reads=tk, writes=tk)
            sc.add(bias_eng, lambda e: e.tensor_tensor(out=dst, in0=tmpn[:], in1=bet, op=ALU.add),
                   reads=tk, writes=[dkey])

        def layer_norm(p, zt, zkey, gam, bet, dst, dkey, stats, mv, tmpn):
            ln_stats(p, zt, zkey, stats, mv)
            ln_apply(p, zt, zkey, gam, bet, dst, dkey, mv, tmpn)


        with ExitStack() as p2:
            wg = sb("wg", [128, 8, 2048], BF16, p2)
            wpa = sb("wpa", [128, 2, 1024], BF16, p2)
            wpb = sb("wpb", [128, 8, 1024], BF16, p2)
            wo = sb("wo", [128, 8, 1024], BF16, p2)
            rw = sb("rw", [128, 8, 32], F32, p2)
            rb = sb("rb", [128, 32], F32, p2)
            ln1 = sb("ln1", [128, 2, 1024], F32, p2)
            oTt = [sb("oTt%d" % i, [128, 10, 128], BF16, p2) for i in range(2)]
            xres = sb("xres", [128, D], F32, p2)
            sg = sb("sg", [128, 2048], F32, p2)
            m1 = sb("m1", [128, D], F32, p2)
            m2 = sb("m2", [128, D], F32, p2)
            mT = sb("mT", [128, 8, 128], BF16, p2)
            zt = sb("zt", [128, D], F32, p2)
            y32 = sb("y32", [128, D], F32, p2)
            ybf = [sb("ybf%d" % i, [128, D], BF16, p2) for i in range(2)]
            yT = sb("yT", [128, 8, 128], F32, p2)
            stats = sb("stats", [128, 12], F32, p2)
            mv = sb("mv", [128, 8], F32, p2)
            lg = sb("lg", [128, 32], F32, p2)
            top8 = sb("top8", [128, 8], F32, p2)
            idx8 = sb("idx8", [128, 8], U32, p2)
            idxf = sb("idxf", [128, 8], F32, p2)
            negmax = sb("negmax", [128, 1], F32, p2)
            e4 = sb("e4", [128, 4], F32, p2)
            s4 = sb("s4", [128, 2], F32, p2)
            mask = sb("mask", [128, 32], F32, p2)
            slotf = sb("slotf", [128, 32], F32, p2)
            ovf = sb("ovf", [128, 32], F32, p2)
            CNT = sb("CNT", [128, 32], F32, p2)
            oh = sb("oh", [128, 32], F32, p2)
            j32 = sb("j32", [128, 32], F32, p2)
            slotk = sb("slotk", [128, 4], F32, p2)
            okk = sb("okk", [128, 4], F32, p2)
            B = [ps("B%d" % i, [128, 512], F32, p2) for i in range(8)]

            sc.add("sp", lambda e: e.dma_start(out=cst[:], in_=cst_d), writes=["cst"], dma=True)
            sc.add("sp", lambda e: e.dma_start(out=rw[:], in_=rw_d.rearrange("(dc p) e -> p dc e", p=128)), writes=["rw"], dma=True)
            sc.add("sp", lambda e: e.dma_start(out=rb[:], in_=rb_d), writes=["rb"], dma=True)
            sc.add("sp", lambda e: e.dma_start(out=ln1[:].rearrange("p a d -> p (a d)"), in_=lnp_d[:, 0:2048]), writes=["ln1"], dma=True)
            for n4 in range(4):
                sc.add("pool", lambda e, n4=n4: e.dma_start(out=wg[:, :, n4 * 512:(n4 + 1) * 512],
                                                            in_=w_in_v[:, :, C_GA + n4 * 512:C_GA + (n4 + 1) * 512]),
                       writes=["wg"], dma=True)
            sc.add("pool", lambda e: e.dma_start(out=wpa[:], in_=wpa_d.rearrange("(c p) d -> p c d", p=128)), writes=["wpa"], dma=True)
            for hf in range(2):
                sc.add("pool", lambda e, hf=hf: e.dma_start(out=wpb[:, :, hf * 512:(hf + 1) * 512],
                                                            in_=wpb_d.rearrange("(c p) d -> p c d", p=128)[:, :, hf * 512:(hf + 1) * 512]),
                       writes=["wpb"], dma=True)
                sc.add("pool", lambda e, hf=hf: e.dma_start(out=wo[:, :, hf * 512:(hf + 1) * 512],
                                                            in_=wo_d.rearrange("(c p) d -> p c d", p=128)[:, :, hf * 512:(hf + 1) * 512]),
                       writes=["wo"], dma=True)
            sc.add("dve", lambda e: e.memset(CNT[:], 0.0), writes=["CNT"])
            sc.barrier()

            zts = [zt, sb("zt1", [128, D], F32, p2)]
            y32s = [y32, sb("y32b", [128, D], F32, p2)]
            Fb = B[0:5]
            CT = B[5:7]
            CL = B[7]

            def A_loads(i):
                b = i % 2
                tsl = slice(i * 128, (i + 1) * 128)
                sc.add("sp", lambda e: e.dma_start(out=oTt[b][:], in_=oT_d[:, :, tsl].rearrange("c p t -> p c t")),
                       writes=[("oTt", b)], dma=True)
                sc.add("sp", lambda e: e.dma_start(out=xres[:], in_=x[tsl, :]), writes=["xres"], dma=True)

            def A_gates(i, half):
                tsl = slice(i * 128, (i + 1) * 128)
                for n2 in range(2):
                    n4 = half * 2 + n2
                    for dc in range(8):
                        sc.add("pe", lambda e, n2=n2, n4=n4, dc=dc: e.matmul(
                            out=Fb[n2][:], lhsT=xT[:, dc, tsl], rhs=wg[:, dc, n4 * 512:(n4 + 1) * 512],
                            start=(dc == 0), stop=(dc == 7)), writes=[("F", n2)])
                    sc.add("act", lambda e, n2=n2, n4=n4: e.activation(out=sg[:, n4 * 512:(n4 + 1) * 512], in_=Fb[n2][:],
                                                                        func=AF.Sigmoid),
                           reads=[("F", n2)], writes=[("sg", n4)])

            def A_proj(i, which):
                b = i % 2
                nck, w_, c0 = (2, wpa, 8) if which == 0 else (8, wpb, 0)
                for hf in range(2):
                    for c in range(nck):
                        sc.add("pe", lambda e, hf=hf, c=c: e.matmul(
                            out=Fb[2 + hf][:], lhsT=oTt[b][:, c0 + c, :], rhs=w_[:, c, hf * 512:(hf + 1) * 512],
                            start=(c == 0), stop=(c == nck - 1)), reads=[("oTt", b)], writes=[("F", 2 + hf)])

            def A_m(i, which):
                for hf in range(2):
                    hs = slice(hf * 512, (hf + 1) * 512)
                    if which == 0:
                        sc.add("dve", lambda e, hf=hf, hs=hs: e.tensor_tensor(
                            out=m1[:, hs], in0=Fb[2 + hf][:], in1=sg[:, hs], op=ALU.mult),
                            reads=[("F", 2 + hf), ("sg", hf)], writes=[("m1", hf)])
                    else:
                        sc.add("dve", lambda e, hf=hf, hs=hs: e.tensor_tensor(
                            out=m2[:, hs], in0=Fb[2 + hf][:], in1=sg[:, 1024 + hf * 512:1024 + (hf + 1) * 512], op=ALU.mult),
                            reads=[("F", 2 + hf), ("sg", 2 + hf)], writes=[("m2", hf)])
                        sc.add("pool", lambda e, hs=hs: e.tensor_tensor(out=m1[:, hs], in0=m1[:, hs], in1=m2[:, hs], op=ALU.add),
                               reads=[("m1", hf), ("m2", hf)], writes=[("m1", hf)])

            def A_tail(i):
                zb = i % 2
                tb = (4, 0)
                for hf in range(2):
                    for c4 in range(4):
                        c = hf * 4 + c4
                        sc.add("pe", lambda e, hf=hf, c=c, c4=c4: e.transpose(
                            out=Fb[tb[hf]][:, c4 * 128:(c4 + 1) * 128], in_=m1[:, c * 128:(c + 1) * 128], identity=ident[:]),
                            reads=[("m1", hf)], writes=[("F", tb[hf])])
                    fa = lambda e, hf=hf: e.copy(out=mT[:, hf * 4:(hf + 1) * 4, :],
                                                 in_=Fb[tb[hf]][:].rearrange("p (c t) -> p c t", c=4))
                    fd = lambda e, hf=hf: e.tensor_copy(out=mT[:, hf * 4:(hf + 1) * 4, :],
                                                        in_=Fb[tb[hf]][:].rearrange("p (c t) -> p c t", c=4))
                    sc.add("act" if hf == 0 else "dve", fa if hf == 0 else fd, reads=[("F", tb[hf])], writes=[("mT", hf)])
                for hf in range(2):
                    for c in range(8):
                        sc.add("pe", lambda e, hf=hf, c=c: e.matmul(
                            out=Fb[2 + hf][:], lhsT=mT[:, c, :], rhs=wo[:, c, hf * 512:(hf + 1) * 512],
                            start=(c == 0), stop=(c == 7)), reads=[("mT", 0), ("mT", 1)], writes=[("F", 2 + hf)])
                    sc.add("dve", lambda e, hf=hf: e.scalar_tensor_tensor(
                        out=zts[zb][:, hf * 512:(hf + 1) * 512], in0=xres[:, hf * 512:(hf + 1) * 512], scalar=float(ALPHA),
                        in1=Fb[2 + hf][:], op0=ALU.mult, op1=ALU.add),
                        reads=["xres", ("F", 2 + hf)], writes=[("zt", zb)])

            def B_stats(i):
                zb = i % 2
                ln_stats("a", zts[zb], ("zt", zb), stats, mv)

            def B_norm(i):
                zb = i % 2
                tsl = slice(i * 128, (i + 1) * 128)
                ln_apply("a", zts[zb], ("zt", zb), ln1[:, 0, :], ln1[:, 1, :], y32s[zb][:], ("y32", zb), mv, m2,
                         tkeys=[("m2", 0), ("m2", 1)])
                sc.add("sp", lambda e: e.dma_start(out=y32_d[tsl, :], in_=y32s[zb][:]), reads=[("y32", zb)], dma=True)
                sc.add("act", lambda e: e.copy(out=ybf[zb][:], in_=y32s[zb][:]), reads=[("y32", zb)], writes=[("ybf", zb)])

            def C_routerT(i):
                yb = i % 2
                for hf in range(2):
                    for c4 in range(4):
                        c = hf * 4 + c4
                        sc.add("pe", lambda e, hf=hf, c=c, c4=c4: e.transpose(
                            out=CT[hf][:, c4 * 128:(c4 + 1) * 128], in_=y32s[yb][:, c * 128:(c + 1) * 128], identity=ident[:]),
                            reads=[("y32", yb)], writes=[("CT", hf)])
                    fa = lambda e, hf=hf: e.copy(out=yT[:, hf * 4:(hf + 1) * 4, :], in_=CT[hf][:].rearrange("p (c t) -> p c t", c=4))
                    fd = lambda e, hf=hf: e.tensor_copy(out=yT[:, hf * 4:(hf + 1) * 4, :], in_=CT[hf][:].rearrange("p (c t) -> p c t", c=4))
                    sc.add("act" if hf == 0 else "dve", fa if hf == 0 else fd, reads=[("CT", hf)], writes=[("yT", hf)])

            def C_logits(i):
                for dc in range(8):
                    sc.add("pe", lambda e, dc=dc: e.matmul(out=CL[:, 0:32], lhsT=yT[:, dc, :], rhs=rw[:, dc, :],
                                                           start=(dc == 0), stop=(dc == 7)),
                           reads=[("yT", 0), ("yT", 1)], writes=["CL"])
                sc.add("dve", lambda e: e.tensor_tensor(out=lg[:], in0=CL[:, 0:32], in1=rb[:], op=ALU.add),
                       reads=["CL"], writes=["lg"])
                sc.add("dve", lambda e: e.max(out=top8[:], in_=lg[:]), reads=["lg"], writes=["top8"])
                sc.add("dve", lambda e: e.max_index(out=idx8[:], in_max=top8[:], in_values=lg[:]),
                       reads=["lg", "top8"], writes=["idx8"])
                sc.add("dve", lambda e: e.tensor_copy(out=idxf[:], in_=idx8[:]), reads=["idx8"], writes=["idxf"])
                sc.add("dve", lambda e: e.tensor_scalar(out=negmax[:], in0=top8[:, 0:1], scalar1=-1.0, scalar2=None, op0=ALU.mult),
                       reads=["top8"], writes=["negmax"])
                sc.add("act", lambda e: e.activation(out=e4[:], in_=top8[:, 0:4], func=AF.Exp, bias=negmax[:, 0:1], scale=1.0,
                                                     accum_out=s4[:, 0:1]),
                       reads=["top8", "negmax"], writes=["e4", "s4"])
                sc.add("dve", lambda e: e.tensor_scalar(out=mask[:], in0=lg[:], scalar1=top8[:, 3:4], scalar2=None, op0=ALU.is_ge),
                       reads=["lg", "top8"], writes=["mask"])

            def C_pos(i):
                b = i % 2
                sc.add("pe", lambda e: e.matmul(out=CL[:, 32:64], lhsT=utri, rhs=mask[:], start=True, stop=True),
                       reads=["mask"], writes=["CL"])
                sc.add("pe", lambda e: e.matmul(out=CL[:, 64:96], lhsT=ones_f, rhs=mask[:], start=True, stop=True),
                       reads=["mask"], writes=["CL"])
                sc.add("dve", lambda e: e.reciprocal(out=s4[:, 1:2], in_=s4[:, 0:1]), reads=["s4"], writes=["s4"])
                sc.add("dve", lambda e: e.tensor_tensor(out=slotf[:], in0=CL[:, 32:64], in1=CNT[:], op=ALU.add),
                       reads=["CL", "CNT"], writes=["slotf"])
                sc.add("dve", lambda e: e.tensor_tensor(out=CNT[:], in0=CL[:, 64:96], in1=CNT[:], op=ALU.add),
                       reads=["CL", "CNT", "slotf"], writes=["CNT"])
                sc.add("dve", lambda e: e.tensor_scalar(out=ovf[:], in0=slotf[:], scalar1=float(CAP), scalar2=1.0e6,
                                                       op0=ALU.is_ge, op1=ALU.mult),
                       reads=["slotf"], writes=["ovf"])
                sc.add("dve", lambda e: e.tensor_tensor(out=slotf[:], in0=slotf[:], in1=ebase, op=ALU.add),
                       reads=["slotf"], writes=["slotf"])
                sc.add("dve", lambda e: e.tensor_tensor(out=slotf[:], in0=slotf[:], in1=ovf[:], op=ALU.add),
                       reads=["slotf", "ovf"], writes=["slotf"])
                for k in range(4):
                    sc.add("dve", lambda e, k=k: e.tensor_scalar(out=oh[:], in0=iota_e, scalar1=idxf[:, k:k + 1], scalar2=None,
                                                                op0=ALU.is_equal),
                           reads=["idxf"], writes=["oh"])
                    sc.add("dve", lambda e, k=k: e.tensor_tensor(out=j32[:], in0=oh[:], in1=slotf[:], op=ALU.mult),
                           reads=["oh", "slotf"], writes=["j32"])
                    sc.add("dve", lambda e, k=k: e.reduce_sum(out=slotk[:, k:k + 1], in_=j32[:], axis=AX.X),
                           reads=["j32"], writes=["slotk"])
                sc.add("dve", lambda e: e.tensor_scalar(out=okk[:], in0=slotk[:], scalar1=float(NSLOT), scalar2=None, op0=ALU.is_lt),
                       reads=["slotk"], writes=["okk"])
                sc.add("dve", lambda e: e.tensor_scalar(out=e4[:], in0=e4[:], scalar1=s4[:, 1:2], scalar2=None, op0=ALU.mult),
                       reads=["e4", "s4"], writes=["e4"])
                sc.add("dve", lambda e: e.tensor_tensor(out=gate_all[:, i * 4:(i + 1) * 4], in0=e4[:], in1=okk[:], op=ALU.mult),
                       reads=["e4", "okk"], writes=[("gate", i)])
                sc.add("dve", lambda e: e.tensor_copy(out=idx_all[:, i * 4:(i + 1) * 4], in_=slotk[:]),
                       reads=["slotk"], writes=[("idx", i)])
                for k in range(4):
                    sc.add("pool", lambda e, k=k: e.indirect_dma_start(
                        out=xs_d[:, :], out_offset=bass.IndirectOffsetOnAxis(ap=idx_all[:, i * 4 + k:i * 4 + k + 1], axis=0),
                        in_=ybf[b][:, :], in_offset=None, bounds_check=bcreg(e), oob_is_err=False),
                        reads=[("idx", i), ("ybf", b)], dma=True)

            for s_ in range(NT + 2):
                a, bt, ct = s_, s_ - 1, s_ - 2
                hasA, hasB, hasC = a < NT, 0 <= bt < NT, 0 <= ct < NT
                if hasA:
                    A_loads(a)
                if hasB:
                    B_stats(bt)
                if hasA:
                    A_gates(a, 0)
                    A_proj(a, 0)
                if hasC:
                    C_routerT(ct)
                if hasB:
                    B_norm(bt)
                if hasA:
                    A_m(a, 0)
                    A_gates(a, 1)
                if hasC:
                    C_logits(ct)
                if hasA:
                    A_proj(a, 1)
                    A_m(a, 1)
                    A_tail(a)
                if hasC:
                    C_pos(ct)
            sc.barrier()
        pX.close()

        if stage == "y":
            sc.emit(es)
            return nc


        NB = CAP // 128
        SG = CAP // 2
        with ExitStack() as p4:
            wgt = [sb("wgt%d" % i, [128, 8, D], BF16, p4) for i in range(2)]
            wup = [sb("wup%d" % i, [128, 8, D], BF16, p4) for i in range(2)]
            wdn = [sb("wdn%d" % i, [128, 8, D], BF16, p4) for i in range(2)]
            bdr = [sb("bdr%d" % i, [1, D], BF16, p4) for i in range(2)]
            bgu = sb("bgu", [128, 512], F32, p4)
            onesr = sb("onesr", [1, 128], BF16, p4)
            NXS = 5
            xs = [sb("xs%d" % i, [128, D], BF16, p4) for i in range(NXS)]
            xsT = [sb("xsT%d" % i, [128, 8, CAP], BF16, p4) for i in range(2)]
            hT = [sb("hT%d" % i, [128, 8, CAP], BF16, p4) for i in range(2)]
            gtt = [sb("gtt%d" % i, [128, SG], F32, p4) for i in range(2)]
            sgm = [sb("sgm%d" % i, [128, SG], F32, p4) for i in range(2)]
            ubt = [sb("ubt%d" % i, [128, SG], F32, p4) for i in range(2)]
            ysb = [sb("ysb%d" % i, [128, D], BF16, p4) for i in range(2)]
            pT = [ps("pT%d" % i, [128, 1024], BF16, p4) for i in range(2)]
            pG = [ps("pG%d" % i, [128, 512], F32, p4) for i in range(2)]
            pU = [ps("pU%d" % i, [128, 512], F32, p4) for i in range(2)]
            pY = [ps("pY%d" % i, [128, 512], F32, p4) for i in range(2)]
            sc.add("sp", lambda e: e.dma_start(out=bgu[:], in_=bgu_d), writes=["bgu"], dma=True)
            sc.add("dve", lambda e: e.memset(onesr[:], 1.0), writes=["onesr"])
            c4 = {"xs": 0, "xl": 0, "g": 0, "y": 0, "ev": 0}

            wdst = sb("wdst", [128, 8, D], F32, p4)

            def load_expert(e_):
                par = e_ % 2
                for (dst, src, nm) in ((wgt, wgate_d, "wgt"), (wup, wup_d, "wup")):
                    for hf in range(2):
                        sc.add("pool", lambda e, dst=dst, src=src, hf=hf: e.dma_start(
                            out=dst[par][:, :, hf * 512:(hf + 1) * 512],
                            in_=src[e_].rearrange("(dc p) f -> p dc f", p=128)[:, :, hf * 512:(hf + 1) * 512]),
                            writes=[(nm, par)], dma=True)
                sc.add("pool", lambda e: e.dma_start(out=bdr[par][:], in_=bdn_d[e_:e_ + 1, :]), writes=[("bdr", par)], dma=True)
                for hf in range(2):
                    sc.add("sp", lambda e, hf=hf: e.dma_start(
                        out=wdst[:, :, hf * 512:(hf + 1) * 512],
                        in_=wdown_d[e_].rearrange("(dc p) f -> p dc f", p=128)[:, :, hf * 512:(hf + 1) * 512]),
                        writes=[("wdst", hf)], dma=True)

            def cast_dn(e_):
                par = e_ % 2
                for dc in range(8):
                    sc.add("act", lambda e, dc=dc: e.copy(out=wdn[par][:, dc, :], in_=wdst[:, dc, :]),
                           reads=[("wdst", 0), ("wdst", 1)], writes=[("wdn", par)])

            n_exp = NE if stage != "moe1" else 2
            load_expert(0)
            cast_dn(0)
            def load_xs(e_):
                for blk in range(NB):
                    xb = c4["xl"] % NXS
                    c4["xl"] += 1
                    r0 = e_ * CAP + blk * 128
                    sc.add("sp", lambda e, xb=xb, r0=r0: e.dma_start(out=xs[xb][:], in_=xs_d[r0:r0 + 128, :]),
                           writes=[("xs", xb)], dma=True)

            def stage_T(e_):
                par = e_ % 2
                for blk in range(NB):
                    xb = c4["xs"] % NXS
                    tb = c4["xs"] % 2
                    c4["xs"] += 1
                    for dc in range(8):
                        sc.add("pe", lambda e, xb=xb, tb=tb, dc=dc: e.transpose(
                            out=pT[tb][:, dc * 128:(dc + 1) * 128], in_=xs[xb][:, dc * 128:(dc + 1) * 128], identity=identb[:]),
                            reads=[("xs", xb)], writes=[("pT", tb)])
                    sc.add("act", lambda e, blk=blk, tb=tb: e.copy(out=xsT[par][:, :, blk * 128:(blk + 1) * 128],
                                                                 in_=pT[tb][:].rearrange("p (c t) -> p c t", c=8)),
                           reads=[("pT", tb)], writes=[("xsT", par, blk)])

            def stage_GU(e_):
                par = e_ % 2
                xkeys = [("xsT", par, blk) for blk in range(NB)]
                groups = [(sgi, fc) for sgi in range(2) for fc in range(8)]

                def A(n):
                    sgi, fc = groups[n]
                    ss = slice(sgi * SG, (sgi + 1) * SG)
                    gb = n % 2
                    for (pp_, w_, nm, wn) in ((pG, wgt, "pG", "wgt"), (pU, wup, "pU", "wup")):
                        for dc in range(8):
                            sc.add("pe", lambda e, pp_=pp_, w_=w_, dc=dc: e.matmul(
                                out=pp_[gb][:, 0:SG], lhsT=w_[par][:, dc, fc * 128:(fc + 1) * 128], rhs=xsT[par][:, dc, ss],
                                start=(dc == 0), stop=(dc == 7)),
                                reads=xkeys + [(wn, par)], writes=[(nm, gb)])
                    col = e_ * 8 + fc
                    sc.add("dve", lambda e: e.tensor_scalar(
                        out=gtt[gb][:], in0=pG[gb][:, 0:SG], scalar1=bgu[:, col:col + 1], scalar2=7.0,
                        op0=ALU.add, op1=ALU.min), reads=[("pG", gb), "bgu"], writes=[("gtt", gb)])
                    sc.add("act", lambda e: e.activation(out=sgm[gb][:], in_=gtt[gb][:], func=AF.Sigmoid, scale=1.702),
                           reads=[("gtt", gb)], writes=[("sgm", gb)])
                    sc.add("act", lambda e: e.activation(
                        out=ubt[gb][:], in_=pU[gb][:, 0:SG], func=AF.Identity, bias=bgu[:, 256 + col:256 + col + 1], scale=1.0),
                        reads=[("pU", gb), "bgu"], writes=[("ubt", gb)])

                def B(n):
                    sgi, fc = groups[n]
                    ss = slice(sgi * SG, (sgi + 1) * SG)
                    gb = n % 2
                    sc.add("pool", lambda e: e.tensor_scalar(out=ubt[gb][:], in0=ubt[gb][:], scalar1=7.0, scalar2=-7.0,
                                                            op0=ALU.min, op1=ALU.max),
                           reads=[("ubt", gb)], writes=[("ubt", gb)])
                    sc.add("pool", lambda e: e.tensor_tensor(out=gtt[gb][:], in0=gtt[gb][:], in1=sgm[gb][:], op=ALU.mult),
                           reads=[("gtt", gb), ("sgm", gb)], writes=[("gtt", gb)])
                    sc.add("dve", lambda e: e.scalar_tensor_tensor(
                        out=hT[par][:, fc, ss], in0=ubt[gb][:], scalar=1.0, in1=gtt[gb][:], op0=ALU.add, op1=ALU.mult),
                        reads=[("ubt", gb), ("gtt", gb)], writes=[("hT", par, sgi, fc)])

                for n in range(len(groups) + 1):
                    if n < len(groups):
                        A(n)
                    if n >= 1:
                        B(n - 1)

            def stage_DN(e_):
                par = e_ % 2
                hkeys = [("hT", par, sgi, fc) for sgi in range(2) for fc in range(8)]
                for blk in range(NB):
                    yb = c4["y"] % 2
                    c4["y"] += 1
                    for hf in range(2):
                        for fc in range(8):
                            sc.add("pe", lambda e, hf=hf, fc=fc, blk=blk: e.matmul(
                                out=pY[hf][:], lhsT=hT[par][:, fc, blk * 128:(blk + 1) * 128],
                                rhs=wdn[par][:, fc, hf * 512:(hf + 1) * 512], start=(fc == 0), stop=False),
                                reads=hkeys + [("wdn", par)], writes=[("pY", hf)])
                        sc.add("pe", lambda e, hf=hf: e.matmul(
                            out=pY[hf][:], lhsT=onesr[0:1, :], rhs=bdr[par][0:1, hf * 512:(hf + 1) * 512], start=False, stop=True),
                            reads=["onesr", ("bdr", par)], writes=[("pY", hf)])
                        fa = lambda e, hf=hf, yb=yb: e.copy(out=ysb[yb][:, hf * 512:(hf + 1) * 512], in_=pY[hf][:])
                        fd = lambda e, hf=hf, yb=yb: e.tensor_copy(out=ysb[yb][:, hf * 512:(hf + 1) * 512], in_=pY[hf][:])
                        sc.add("act" if hf == 0 else "dve", fa if hf == 0 else fd, reads=[("pY", hf)], writes=[("ysb", yb, hf)])
                    r0 = e_ * CAP + blk * 128
                    sc.add("sp", lambda e, yb=yb, r0=r0: e.dma_start(out=ys_d[r0:r0 + 128, :], in_=ysb[yb][:]),
                           reads=[("ysb", yb, 0), ("ysb", yb, 1)], dma=True)

            load_xs(0)
            stage_T(0)
            for e_ in range(n_exp):
                if e_ + 1 < n_exp and not (NOLOAD and e_ + 1 >= 2):
                    load_expert(e_ + 1)
                if e_ + 1 < n_exp:
                    load_xs(e_ + 1)
                stage_GU(e_)
                if e_ + 1 < n_exp:
                    stage_T(e_ + 1)
                stage_DN(e_)
                if e_ + 1 < n_exp and not (NOLOAD and e_ + 1 >= 2):
                    cast_dn(e_ + 1)
            sc.barrier()

        with ExitStack() as p5:
            ln2 = sb("ln2", [128, 2, 1024], F32, p5)
            G = [[sb("G%d_%d" % (k, i), [128, D], BF16, p5) for i in range(2)] for k in range(4)]
            yr = [sb("yr%d" % i, [128, D], F32, p5) for i in range(2)]
            acc = sb("acc5", [128, D], F32, p5)
            z2 = sb("z2", [128, D], F32, p5)
            tn = sb("tn", [128, D], F32, p5)
            ot = [sb("ot%d" % i, [128, D], F32, p5) for i in range(2)]
            stats2 = sb("stats2", [128, 12], F32, p5)
            mv2 = sb("mv2", [128, 8], F32, p5)
            dg = [sb("dg%d" % i, [128, 4, 128], BF16, p5) for i in range(2)]
            pF = [[ps("pF%d_%d" % (i, h), [128, 512], F32, p5) for h in range(2)] for i in range(2)]
            sc.add("sp", lambda e: e.dma_start(out=ln2[:].rearrange("p a d -> p (a d)"), in_=lnp_d[:, 2048:4096]), writes=["ln2"], dma=True)
            for k in range(4):
                for i in range(2):
                    sc.add("pool", lambda e, k=k, i=i: e.memset(G[k][i][:], 0.0), writes=[("G", k, i)])
            def loads5(i):
                b = i % 2
                tsl = slice(i * 128, (i + 1) * 128)
                sc.add("sp", lambda e: e.dma_start(out=yr[b][:], in_=y32_d[tsl, :]), writes=[("yr", b)], dma=True)
                for k in range(4):
                    sc.add("pool", lambda e, k=k: e.indirect_dma_start(
                        out=G[k][b][:, :], out_offset=None, in_=ys_d[:, :],
                        in_offset=bass.IndirectOffsetOnAxis(ap=idx_all[:, i * 4 + k:i * 4 + k + 1], axis=0),
                        bounds_check=bcreg(e), oob_is_err=False),
                        writes=[("G", k, b)], dma=True)

            z2b = [z2, sb("z2b", [128, D], F32, p5)]
            tnb = [tn, sb("tnb", [128, D], F32, p5)]
            stb = [stats2, sb("stats2b", [128, 12], F32, p5)]
            mvb = [mv2, sb("mv2b", [128, 8], F32, p5)]

            def P5(i):
                b = i % 2
                for k in range(4):
                    sc.add("dve", lambda e, k=k: e.tensor_scalar(
                        out=dg[b][:, k, :], in0=identb[:], scalar1=gate_all[:, i * 4 + k:i * 4 + k + 1], scalar2=None,
                        op0=ALU.mult), writes=[("dg", b)])
                for hf in range(2):
                    for k in range(4):
                        sc.add("pe", lambda e, hf=hf, k=k: e.matmul(
                            out=pF[b][hf][:], lhsT=dg[b][:, k, :], rhs=G[k][b][:, hf * 512:(hf + 1) * 512],
                            start=(k == 0), stop=(k == 3)),
                            reads=[("dg", b), ("G", k, b)], writes=[("pF", b, hf)])
                    sc.add("dve", lambda e, hf=hf: e.scalar_tensor_tensor(
                        out=z2b[b][:, hf * 512:(hf + 1) * 512], in0=yr[b][:, hf * 512:(hf + 1) * 512], scalar=float(ALPHA),
                        in1=pF[b][hf][:], op0=ALU.mult, op1=ALU.add),
                        reads=[("yr", b), ("pF", b, hf)], writes=[("z2", b)])
                ln_stats(("b", b), z2b[b], ("z2", b), stb[b], mvb[b])

            def Q5(i):
                b = i % 2
                tsl = slice(i * 128, (i + 1) * 128)
                ln_apply(("b", b), z2b[b], ("z2", b), ln2[:, 0, :], ln2[:, 1, :], ot[b][:], ("ot", b), mvb[b], tnb[b],
                         bias_eng="dve")
                sc.add("sp", lambda e: e.dma_start(out=out[tsl, :], in_=ot[b][:]), reads=[("ot", b)], dma=True)

            loads5(0)
            for i in range(NT):
                if i + 1 < NT:
                    loads5(i + 1)
                P5(i)
                if i >= 1:
                    Q5(i - 1)
            Q5(NT - 1)

        sc.emit(es)
    return nc


_NC_CACHE = {}


def kernel(**inputs):
    stage = inputs.pop("_stage", os.environ.get("KSTAGE", "full"))
    if stage not in _NC_CACHE:
        _NC_CACHE[stage] = build(stage)
    nc = _NC_CACHE[stage]
    f = lambda k: np.ascontiguousarray(np.asarray(inputs[k], dtype=np.float32)[0])
    x = np.ascontiguousarray(inputs["x"], dtype=np.float32)
    ident, negd = host_consts()
    sinks = f("sinks").reshape(16)
    sinkc = np.zeros((128, 8), dtype=np.float32)
    for p in range(8):
        sinkc[:64, p] = sinks[2 * p]
        sinkc[64:, p] = sinks[2 * p + 1]
    lnp = np.concatenate([np.broadcast_to(f(k)[None, :], (128, D)) for k in ("ln1_g", "ln1_b", "ln2_g", "ln2_b")], axis=1)
    rb = np.broadcast_to(f("router_b")[None, :], (128, 32))
    bgu = np.concatenate([f("b_gate").reshape(32, 8, 128).transpose(2, 0, 1).reshape(128, 256),
                          f("b_up").reshape(32, 8, 128).transpose(2, 0, 1).reshape(128, 256)], axis=1)
    cst = np.zeros((128, 320), dtype=np.float32)
    cst[:, 0:128] = np.triu(np.ones((128, 128), dtype=np.float32), 1)
    cst[:, 128:256] = 1.0
    cst[:, 256:288] = np.arange(32, dtype=np.float32)[None, :]
    cst[:, 288:320] = (np.arange(32, dtype=np.float32) * CAP)[None, :]
    shared = {"ident": ident, "negd": negd, "sinkc": sinkc, "w_in": f("w_in"),
              "w_proj_a": f("w_proj_a"), "w_proj_b": f("w_proj_b"), "w_out": f("w_out"),
              "lnp": np.ascontiguousarray(lnp), "router_w": f("router_w"), "rb": np.ascontiguousarray(rb),
              "cst": cst, "w_gate": f("w_gate"), "w_up": f("w_up"), "w_down": f("w_down"),
              "bgu": np.ascontiguousarray(bgu), "b_down": f("b_down")}
    names = set(_IN_NAMES.get(stage, shared.keys()))
    in_maps = []
    for c in range(NCORES):
        m = {k: v for k, v in shared.items() if k in names}
        m["x"] = x[c]
        in_maps.append(m)
    res = run_bass_kernel_spmd(nc, in_maps, core_ids=list(range(NCORES)))
    if "KSTAGE" in os.environ and stage != "full":
        return np.zeros((NCORES, S, D), dtype=np.float32)
    if stage != "full":
        return res
    return np.stack([np.asarray(r["out"]) for r in res.results], axis=0)


_IN_NAMES = {}
```
